# Optimizing a Trainium2 kernel written in Bass

```python
import math
import jax
import jax.numpy as jnp
from jax import lax
import numpy as np

D_MODEL = 1024
BATCH = 8
SEQ = 4096
DEPTH = 2

CTX_LEN = 256
GRID_W = 64

LRU_WIDTH = 512
LRU_HEADS = 8
LRU_BLOCK = LRU_WIDTH // LRU_HEADS
CONV_W = 4
LRU_C = 8.0
DA_HEADS = 4
DA_HEAD_DIM = 64
DA_V_DIM = 2 * DA_HEAD_DIM
DA_WIDTH = DA_HEADS * DA_V_DIM
Q_BLOCK = 128
ROPE_BASE = 10000.0
IN_EVEN = 2 * LRU_WIDTH + 3 * DA_WIDTH
IN_SPLITS = (LRU_WIDTH, 2 * LRU_WIDTH, 2 * LRU_WIDTH + DA_WIDTH, 2 * LRU_WIDTH + 2 * DA_WIDTH)
FNET_GROUPS = 4
FNET_GROUP_DIM = D_MODEL // FNET_GROUPS
N_GROUPS = 4
EXPERTS_PER_GROUP = 8
N_EXPERTS = N_GROUPS * EXPERTS_PER_GROUP
TOP_K = 2
EXPERT_FF = 512
MOE_BLOCK = 128
LN_EPS = 1e-6
ALPHA = (2.0 * DEPTH) ** 0.25
BETA = (8.0 * DEPTH) ** -0.25

kernel_name = "hybrid_rglru_diffattn_fnet_hmoe_dit"


def _layer_norm(x, gain=None, bias=None):
    xf = x.astype(jnp.float32)
    mu = jnp.mean(xf, axis=-1, keepdims=True)
    var = jnp.mean(jnp.square(xf - mu), axis=-1, keepdims=True)
    y = (xf - mu) * lax.rsqrt(var + LN_EPS)
    if gain is not None:
        y = y * gain.astype(jnp.float32) + bias.astype(jnp.float32)
    return y.astype(x.dtype)


def _rms_norm(x, gain):
    xf = x.astype(jnp.float32)
    y = xf * lax.rsqrt(jnp.mean(jnp.square(xf), axis=-1, keepdims=True) + LN_EPS)
    return (y * gain.astype(jnp.float32)).astype(x.dtype)


def _modulate(x, shift, scale):
    return _layer_norm(x) * (1.0 + scale) + shift


def _ada_terms(cond, w, b):
    m = jax.nn.silu(cond) @ w + b
    m = m.reshape(m.shape[:-1] + (1, 6, D_MODEL))
    return [m[..., k, :] for k in range(6)]


def _axial_rope_tables(n_tok, dtype):
    t = jnp.arange(n_tok)
    row = (t // GRID_W).astype(jnp.float32)
    col = (t % GRID_W).astype(jnp.float32)
    nf = DA_HEAD_DIM // 4
    freqs = ROPE_BASE ** (-jnp.arange(nf, dtype=jnp.float32) / nf)
    ang = jnp.stack([row[:, None] * freqs, col[:, None] * freqs], axis=1)
    return jnp.cos(ang).astype(dtype), jnp.sin(ang).astype(dtype)


def _apply_axial_rope(x, cos, sin):
    xs = x.reshape(x.shape[:-1] + (2, 2, DA_HEAD_DIM // 4))
    x1 = xs[..., 0, :]
    x2 = xs[..., 1, :]
    out = jnp.stack([x1 * cos - x2 * sin, x2 * cos + x1 * sin], axis=-2)
    return out.reshape(x.shape)


def _diff_attend(q1, q2, k1, k2, v, lam):
    scale = DA_HEAD_DIM ** -0.5
    s1 = jnp.einsum('bhqd,bhkd->bhqk', q1, k1).astype(jnp.float32) * scale
    s2 = jnp.einsum('bhqd,bhkd->bhqk', q2, k2).astype(jnp.float32) * scale
    a = jax.nn.softmax(s1, axis=-1) - lam * jax.nn.softmax(s2, axis=-1)
    return jnp.einsum('bhqk,bhkv->bhqv', a.astype(v.dtype), v)


def _diff_attn_latent(q1, q2, k1, k2, v, lam):
    b, h, n, dh = q1.shape
    nb = n // Q_BLOCK

    def to_blocks(q):
        return jnp.moveaxis(q.reshape(b, h, nb, Q_BLOCK, dh), 2, 0)

    def one_block(qs):
        return _diff_attend(qs[0], qs[1], k1, k2, v, lam)

    out = lax.map(one_block, (to_blocks(q1), to_blocks(q2)))
    return jnp.moveaxis(out, 0, 2).reshape(b, h, n, DA_V_DIM)


def _merge_heads(o, gain, lam_init):
    b, h, n, dv = o.shape
    o = _rms_norm(o, gain) * (1.0 - lam_init)
    return o.transpose(0, 2, 1, 3).reshape(b, n, h * dv)


def _short_conv(x, w, b):
    n = x.shape[1]
    left = CONV_W // 2
    xp = jnp.pad(x, ((0, 0), (left, CONV_W - 1 - left), (0, 0)))
    y = b
    for k in range(CONV_W):
        y = y + xp[:, k:k + n] * w[k]
    return y


def _lru_coeffs(xc, wa, ba, wx, bx, lam):
    b, n, w = xc.shape
    xb = xc.reshape(b, n, LRU_HEADS, LRU_BLOCK)
    r = jax.nn.sigmoid((jnp.einsum('bnhi,hij->bnhj', xb, wa).reshape(b, n, w) + ba).astype(jnp.float32))
    i = jax.nn.sigmoid((jnp.einsum('bnhi,hij->bnhj', xb, wx).reshape(b, n, w) + bx).astype(jnp.float32))
    log_a = -LRU_C * jax.nn.softplus(-lam.astype(jnp.float32)) * r
    a = jnp.exp(log_a)
    mult = jnp.sqrt(-jnp.expm1(2.0 * log_a))
    return a, mult * i * xc.astype(jnp.float32)


def _combine(left, right):
    return left[0] * right[0], right[0] * left[1] + right[1]


def _linear_scan(a, b, h0, reverse):
    if reverse:
        a = jnp.flip(a, axis=1)
        b = jnp.flip(b, axis=1)
    a_cum, b_cum = lax.associative_scan(_combine, (a, b), axis=1)
    h = a_cum * h0[:, None, :] + b_cum
    return jnp.flip(h, axis=1) if reverse else h


def _rglru_bidir(xc_ctx, xc_lat, wa, ba, wx, bx, lam):
    y_ctx = jnp.zeros(xc_ctx.shape, jnp.float32)
    y_lat = jnp.zeros(xc_lat.shape, jnp.float32)
    h_zero = jnp.zeros((xc_ctx.shape[0], xc_ctx.shape[2]), jnp.float32)
    for d, rev in ((0, False), (1, True)):
        a_c, b_c = _lru_coeffs(xc_ctx, wa[d], ba[d], wx[d], bx[d], lam[d])
        h_c = _linear_scan(a_c, b_c, h_zero, rev)
        h_final = h_c[:, 0] if rev else h_c[:, -1]
        a_l, b_l = _lru_coeffs(xc_lat, wa[d], ba[d], wx[d], bx[d], lam[d])
        h_l = _linear_scan(a_l, b_l, h_final, rev)
        y_ctx = y_ctx + h_c
        y_lat = y_lat + h_l
    return y_ctx, y_lat


def _project_even(h, w_in):
    b, n, _ = h.shape
    gate, xr, q, k, v = jnp.split(h @ w_in, IN_SPLITS, axis=-1)
    q = q.reshape(b, n, DA_HEADS, 2, DA_HEAD_DIM).transpose(3, 0, 2, 1, 4)
    k = k.reshape(b, n, DA_HEADS, 2, DA_HEAD_DIM).transpose(3, 0, 2, 1, 4)
    v = v.reshape(b, n, DA_HEADS, DA_V_DIM).transpose(0, 2, 1, 3)
    return gate, xr, q, k, v


def _even_mixer(h_lat, h_ctx, w_in, conv_w, conv_b, ga_w, ga_b, gx_w, gx_b, lru_lambda,
                da_lambda, da_subln, w_out, lam_init, ctx_out):
    g_l, xr_l, q_l, k_l, v_l = _project_even(h_lat, w_in)
    g_c, xr_c, q_c, k_c, v_c = _project_even(h_ctx, w_in)
    cos, sin = _axial_rope_tables(h_lat.shape[1], q_l.dtype)
    q_l = _apply_axial_rope(q_l, cos, sin)
    k_l = _apply_axial_rope(k_l, cos, sin)
    lf = da_lambda.astype(jnp.float32)
    lam = jnp.exp(jnp.sum(lf[0] * lf[1])) - jnp.exp(jnp.sum(lf[2] * lf[3])) + lam_init
    k_all = jnp.concatenate([k_c, k_l], axis=3)
    v_all = jnp.concatenate([v_c, v_l], axis=2)
    o_l = _diff_attn_latent(q_l[0], q_l[1], k_all[0], k_all[1], v_all, lam)
    xc_l = _short_conv(xr_l, conv_w, conv_b)
    xc_c = _short_conv(xr_c, conv_w, conv_b)
    r_c, r_l = _rglru_bidir(xc_c, xc_l, ga_w, ga_b, gx_w, gx_b, lru_lambda)
    y_lat = jnp.concatenate([r_l.astype(h_lat.dtype) * jax.nn.gelu(g_l),
                             _merge_heads(o_l, da_subln, lam_init)], axis=-1) @ w_out
    if not ctx_out:
        return y_lat, None
    o_c = _diff_attend(q_c[0], q_c[1], k_c[0], k_c[1], v_c, lam)
    y_ctx = jnp.concatenate([r_c.astype(h_ctx.dtype) * jax.nn.gelu(g_c),
                             _merge_heads(o_c, da_subln, lam_init)], axis=-1) @ w_out
    return y_lat, y_ctx


def _fourier_mix(h, w, b):
    bsz, n, d = h.shape
    hg = h.astype(jnp.float32).reshape(bsz, n, FNET_GROUPS, FNET_GROUP_DIM).transpose(0, 2, 1, 3)
    y = jnp.fft.fft2(hg, axes=(-2, -1), norm='ortho').real
    y = y.transpose(0, 2, 1, 3).reshape(bsz, n, d).astype(h.dtype)
    return y @ w + b


def _hier_moe(t, wg, bg, wf, bf, w1, w3, w2):
    n_tok, d = t.shape
    g_logits = (t @ wg + bg).astype(jnp.float32)
    g_prob = jax.nn.softmax(g_logits, axis=-1)
    g_idx = jnp.argmax(g_logits, axis=-1)
    p_g = jnp.take_along_axis(g_prob, g_idx[:, None], axis=-1)
    f_logits = (t @ wf + bf).astype(jnp.float32).reshape(n_tok, N_GROUPS, EXPERTS_PER_GROUP)
    f_sel = jnp.take_along_axis(f_logits, g_idx[:, None, None], axis=1)[:, 0]
    top_v, top_j = lax.top_k(f_sel, TOP_K)
    gate_w = (jax.nn.softmax(top_v, axis=-1) * p_g).astype(t.dtype)
    e_idx = g_idx[:, None].astype(jnp.int32) * EXPERTS_PER_GROUP + top_j.astype(jnp.int32)
    m = n_tok * TOP_K
    flat_e = e_idx.reshape(m)
    flat_w = gate_w.reshape(m)
    flat_tok = jnp.repeat(jnp.arange(n_tok, dtype=jnp.int32), TOP_K)
    order = jnp.argsort(flat_e)
    s_e = flat_e[order]
    counts = jnp.bincount(flat_e, length=N_EXPERTS).astype(jnp.int32)
    padded = ((counts + MOE_BLOCK - 1) // MOE_BLOCK) * MOE_BLOCK
    start = jnp.cumsum(counts) - counts
    pend = jnp.cumsum(padded)
    pstart = pend - padded
    dest = pstart[s_e] + jnp.arange(m, dtype=jnp.int32) - start[s_e]
    n_blocks = -(-m // MOE_BLOCK) + N_EXPERTS
    n_rows = n_blocks * MOE_BLOCK
    row_tok = jnp.full((n_rows,), n_tok, dtype=jnp.int32).at[dest].set(flat_tok[order])
    row_w = jnp.zeros((n_rows,), t.dtype).at[dest].set(flat_w[order])
    block_start = jnp.arange(n_blocks, dtype=jnp.int32) * MOE_BLOCK
    block_e = jnp.minimum(jnp.searchsorted(pend, block_start, side='right'), N_EXPERTS - 1)
    t_pad = jnp.concatenate([t, jnp.zeros((1, d), t.dtype)], axis=0)
    xb = t_pad[row_tok].reshape(n_blocks, MOE_BLOCK, d)

    def expert_block(args):
        xe, e = args
        hid = jax.nn.silu(xe @ w1[e]) * (xe @ w3[e])
        return hid @ w2[e]

    yb = lax.map(expert_block, (xb, block_e))
    y = yb.reshape(n_rows, d) * row_w[:, None]
    return jnp.zeros((n_tok + 1, d), t.dtype).at[row_tok].add(y)[:n_tok]


def setup_inputs(seed: int = 0) -> dict:
    key = jax.random.key(seed)
    ks = iter(jax.random.split(key, 40))
    n_even = (DEPTH + 1) // 2
    n_odd = DEPTH // 2
    d = D_MODEL

    def nrm(shape, scale):
        return jax.random.normal(next(ks), shape, jnp.float32) * scale

    x = nrm((BATCH, SEQ, d), 1.0)
    c = nrm((BATCH, d), 1.0)
    ctx = nrm((BATCH, CTX_LEN, d), 1.0)
    c_ctx = nrm((d,), 1.0)
    ada_w = nrm((DEPTH, d, 6 * d), 0.5 * d ** -0.5)
    ada_b = nrm((DEPTH, 6 * d), 0.02)
    ln_g = 1.0 + nrm((DEPTH, 2, d), 0.02)
    ln_b = nrm((DEPTH, 2, d), 0.02)
    ev_w_in = nrm((n_even, d, IN_EVEN), d ** -0.5)
    ev_conv_w = nrm((n_even, CONV_W, LRU_WIDTH), CONV_W ** -0.5)
    ev_conv_b = nrm((n_even, LRU_WIDTH), 0.02)
    ev_gate_a_w = nrm((n_even, 2, LRU_HEADS, LRU_BLOCK, LRU_BLOCK), LRU_BLOCK ** -0.5)
    ev_gate_a_b = nrm((n_even, 2, LRU_WIDTH), 0.02)
    ev_gate_x_w = nrm((n_even, 2, LRU_HEADS, LRU_BLOCK, LRU_BLOCK), LRU_BLOCK ** -0.5)
    ev_gate_x_b = nrm((n_even, 2, LRU_WIDTH), 0.02)
    u = jax.random.uniform(next(ks), (n_even, 2, LRU_WIDTH), jnp.float32, minval=0.9, maxval=0.999)
    a0 = u ** (1.0 / LRU_C)
    ev_lru_lambda = jnp.log(a0) - jnp.log1p(-a0)
    ev_da_lambda = nrm((n_even, 4, DA_HEAD_DIM), 0.1)
    ev_da_subln = 1.0 + nrm((n_even, DA_V_DIM), 0.02)
    ev_w_out = nrm((n_even, LRU_WIDTH + DA_WIDTH, d), BETA * (LRU_WIDTH + DA_WIDTH) ** -0.5)
    od_w_out = nrm((n_odd, d, d), BETA * d ** -0.5)
    od_b_out = nrm((n_odd, d), 0.02)
    moe_wg = nrm((DEPTH, d, N_GROUPS), d ** -0.5)
    moe_bg = nrm((DEPTH, N_GROUPS), 0.01)
    moe_wf = nrm((DEPTH, d, N_EXPERTS), d ** -0.5)
    moe_bf = nrm((DEPTH, N_EXPERTS), 0.01)
    moe_w1 = nrm((DEPTH, N_EXPERTS, d, EXPERT_FF), d ** -0.5)
    moe_w3 = nrm((DEPTH, N_EXPERTS, d, EXPERT_FF), d ** -0.5)
    moe_w2 = nrm((DEPTH, N_EXPERTS, EXPERT_FF, d), BETA * EXPERT_FF ** -0.5)
    return {"x": x, "c": c, "ctx": ctx, "c_ctx": c_ctx, "ada_w": ada_w, "ada_b": ada_b,
            "ln_g": ln_g, "ln_b": ln_b, "ev_w_in": ev_w_in, "ev_conv_w": ev_conv_w,
            "ev_conv_b": ev_conv_b, "ev_gate_a_w": ev_gate_a_w, "ev_gate_a_b": ev_gate_a_b,
            "ev_gate_x_w": ev_gate_x_w, "ev_gate_x_b": ev_gate_x_b, "ev_lru_lambda": ev_lru_lambda,
            "ev_da_lambda": ev_da_lambda, "ev_da_subln": ev_da_subln, "ev_w_out": ev_w_out,
            "od_w_out": od_w_out, "od_b_out": od_b_out, "moe_wg": moe_wg, "moe_bg": moe_bg,
            "moe_wf": moe_wf, "moe_bf": moe_bf, "moe_w1": moe_w1, "moe_w3": moe_w3, "moe_w2": moe_w2}


def reference(x, c, ctx, c_ctx, ada_w, ada_b, ln_g, ln_b, ev_w_in, ev_conv_w, ev_conv_b,
              ev_gate_a_w, ev_gate_a_b, ev_gate_x_w, ev_gate_x_b, ev_lru_lambda, ev_da_lambda,
              ev_da_subln, ev_w_out, od_w_out, od_b_out, moe_wg, moe_bg, moe_wf, moe_bf,
              moe_w1, moe_w3, moe_w2):
    bsz, n_lat, d = x.shape
    n_ctx_tok = ctx.shape[0] * ctx.shape[1]
    for l in range(DEPTH):
        ctx_live = any(m % 2 == 0 for m in range(l + 1, DEPTH))
        sh1, sc1, g1, sh2, sc2, g2 = _ada_terms(c, ada_w[l], ada_b[l])
        csh1, csc1, cg1, csh2, csc2, cg2 = _ada_terms(c_ctx, ada_w[l], ada_b[l])
        if l % 2 == 0:
            e = l // 2
            lam_init = 0.8 - 0.6 * math.exp(-0.3 * l)
            y_lat, y_ctx = _even_mixer(_modulate(x, sh1, sc1), _modulate(ctx, csh1, csc1),
                                       ev_w_in[e], ev_conv_w[e], ev_conv_b[e], ev_gate_a_w[e],
                                       ev_gate_a_b[e], ev_gate_x_w[e], ev_gate_x_b[e],
                                       ev_lru_lambda[e], ev_da_lambda[e], ev_da_subln[e],
                                       ev_w_out[e], lam_init, ctx_live)
        else:
            o = l // 2
            y_lat = _fourier_mix(_modulate(x, sh1, sc1), od_w_out[o], od_b_out[o])
            y_ctx = _fourier_mix(_modulate(ctx, csh1, csc1), od_w_out[o], od_b_out[o]) if ctx_live else None
        x = _layer_norm(ALPHA * x + g1 * y_lat, ln_g[l, 0], ln_b[l, 0])
        if ctx_live:
            ctx = _layer_norm(ALPHA * ctx + cg1 * y_ctx, ln_g[l, 0], ln_b[l, 0])
            tok = jnp.concatenate([_modulate(ctx, csh2, csc2).reshape(-1, d),
                                   _modulate(x, sh2, sc2).reshape(-1, d)], axis=0)
            mo = _hier_moe(tok, moe_wg[l], moe_bg[l], moe_wf[l], moe_bf[l], moe_w1[l], moe_w3[l], moe_w2[l])
            m_ctx = mo[:n_ctx_tok].reshape(ctx.shape)
            m_lat = mo[n_ctx_tok:].reshape(x.shape)
            ctx = _layer_norm(ALPHA * ctx + cg2 * m_ctx, ln_g[l, 1], ln_b[l, 1])
        else:
            m_lat = _hier_moe(_modulate(x, sh2, sc2).reshape(-1, d), moe_wg[l], moe_bg[l], moe_wf[l],
                              moe_bf[l], moe_w1[l], moe_w3[l], moe_w2[l]).reshape(x.shape)
        x = _layer_norm(ALPHA * x + g2 * m_lat, ln_g[l, 1], ln_b[l, 1])
    return x
```

```python
import contextlib
import math
import os
import numpy as np
import ml_dtypes
import concourse.bass as bass
import concourse.mybir as mybir
from concourse.bass_utils import run_bass_kernel_spmd

F32 = mybir.dt.float32
BF16 = mybir.dt.bfloat16
I32 = mybir.dt.int32
AF = mybir.ActivationFunctionType
ALU = mybir.AluOpType
AX = mybir.AxisListType
ENGS = ['pe', 'act', 'dve', 'pool', 'sp']
NPOOL = 86
NBG = 4

D = 1024
SEQ = 4096
NCTX = 256
NTOK = SEQ + NCTX
ALPHA = (2.0 * 2) ** 0.25
EPS = 1e-6
BLK = 256
NBLK = 2 * SEQ // BLK + 32
NSLOT = NBLK * BLK
NB = 5
NT = SEQ // 128


class Res:
    __slots__ = ('name', 'writers', 'readers')

    def __init__(self, name):
        self.name = name
        self.writers = {}
        self.readers = {}


class Prog:
    def __init__(self, nc, st):
        self.nc = nc
        self.esem = {e: st.enter_context(nc.semaphore('se_' + e)) for e in ENGS}
        self.bar = st.enter_context(nc.semaphore('sbar'))
        self.psem = [st.enter_context(nc.semaphore('sp%d' % i)) for i in range(NPOOL + NBG)]
        self.bg = {}
        self.cnt = {('e', e): 0 for e in ENGS}
        for i in range(NPOOL + NBG):
            self.cnt[('p', i)] = 0
        self.bar_cnt = 0
        self.nres = 0
        self._reset_phase()
        self.waited = {e: {} for e in ENGS}

    def _reset_phase(self):
        self.ops = {e: [] for e in ENGS}
        self.res_sem = {}
        self.free = list(range(NPOOL))
        self.touched = set()

    def sem(self, k):
        return self.esem[k[1]] if k[0] == 'e' else self.psem[k[1]]

    def res(self, name=None):
        self.nres += 1
        return Res('%s#%d' % (name or 'r', self.nres))

    def op(self, eng, fn, reads=(), writes=(), dma=None, accum=(), bg=False):
        waits = {}
        isdma = dma is not None

        def need(sk, tok):
            waits[sk] = max(waits.get(sk, 0), tok[0])

        for r in reads:
            for sk, tok in r.writers.items():
                need(sk, tok)
        for w in list(writes) + list(accum):
            is_acc = any(w is a for a in accum)
            if not is_acc:
                for sk, tok in w.writers.items():
                    if (not isdma) and tok[2] == 'c' and tok[1] == eng:
                        continue
                    need(sk, tok)
            for sk, tok in w.readers.items():
                if (not isdma) and tok[2] == 'c' and tok[1] == eng:
                    continue
                need(sk, tok)
        if isdma and bg:
            if dma.name not in self.bg:
                self.bg[dma.name] = NPOOL + len(self.bg)
            sk = ('p', self.bg[dma.name])
        elif isdma:
            if dma.name not in self.res_sem:
                self.res_sem[dma.name] = self.free.pop(0)
            sk = ('p', self.res_sem[dma.name])
        if isdma:
            self.cnt[sk] += 16
            tok = (self.cnt[sk], eng, 'd')
            inc = 16
        else:
            sk = ('e', eng)
            self.cnt[sk] += 1
            tok = (self.cnt[sk], eng, 'c')
            inc = 1
        if not bg:
            self.touched.add(sk)
        wl = []
        wd = self.waited[eng]
        for k, v in waits.items():
            if wd.get(k, 0) >= v:
                continue
            wd[k] = v
            wl.append((k, v))
        self.ops[eng].append((fn, wl, sk, inc))
        for r in reads:
            r.readers[sk] = tok
        for w in writes:
            if any(w is a for a in accum):
                continue
            w.writers = {sk: tok}
            w.readers = {}
        for w in accum:
            w.writers[sk] = tok
        return tok

    def flush(self):
        nc = self.nc
        self.bar_cnt += 1
        barv = self.bar_cnt
        finals = [(k, self.cnt[k]) for k in sorted(self.touched)]
        ops = self.ops
        P = self

        def replay(e, key):
            for fn, wl, sk, inc in ops[key]:
                for k, v in wl:
                    e.wait_ge(P.sem(k), v)
                ins = fn(e)
                ins.then_inc(P.sem(sk), inc)
            if key == 'sp':
                for k, v in finals:
                    e.wait_ge(P.sem(k), v)
                e.sem_inc(P.bar, 1)
            else:
                e.wait_ge(P.bar, barv)

        with nc.Block() as block:
            @block.tensor
            def _(e):
                replay(e, 'pe')

            @block.scalar
            def _(e):
                replay(e, 'act')

            @block.vector
            def _(e):
                replay(e, 'dve')

            @block.gpsimd
            def _(e):
                replay(e, 'pool')

            @block.sync
            def _(e):
                replay(e, 'sp')
        bgk = set(('p', i) for i in self.bg.values())
        for e in ENGS:
            for k in self.cnt:
                if k in bgk:
                    continue
                self.waited[e][k] = self.cnt[k]
        self._reset_phase()


class T:
    def __init__(self, P, t, name):
        self.t = t
        self.r = P.res(name)

    def __getitem__(self, k):
        return self.t[k]


class Ctx:
    pass


def build(dbg=()):
    nc = bass.Bass("TRN2", target_bir_lowering=False)
    gst = contextlib.ExitStack()
    P = Prog(nc, gst)

    def din(name, shape, dt=F32):
        return nc.dram_tensor(name, list(shape), dt, kind="ExternalInput").ap()

    class DR:
        def __init__(self, name, shape, dt=F32, out=False):
            kind = "ExternalOutput" if (out or name in dbg) else "Internal"
            self.ap = nc.dram_tensor(name, list(shape), dt, kind=kind).ap()
            self.r = P.res(name)

    x_in = din("x", [SEQ, D]); c_in = din("c", [D]); ctx_in = din("ctx", [NCTX, D]); cctx_in = din("c_ctx", [D])
    ada_w = din("ada_w", [2, D, 6 * D]); ada_b = din("ada_b", [2, 6 * D])
    ln_g = din("ln_g", [2, 2, D]); ln_b = din("ln_b", [2, 2, D])
    w_in = din("ev_w_in", [D, 2560]); conv_w = din("ev_conv_w", [4, 512]); conv_b = din("ev_conv_b", [512])
    ga_w = din("ev_gate_a_w", [2, 8, 64, 64]); ga_b = din("ev_gate_a_b", [2, 512])
    gx_w = din("ev_gate_x_w", [2, 8, 64, 64]); gx_b = din("ev_gate_x_b", [2, 512])
    lru_lam = din("ev_lru_lambda", [2, 512]); da_lam = din("ev_da_lambda", [4, 64]); da_sub = din("ev_da_subln", [128])
    ev_w_out = din("ev_w_out", [D, D]); od_w_out = din("od_w_out", [D, D]); od_b_out = din("od_b_out", [D])
    moe_wg = din("moe_wg", [2, D, 4]); moe_bg = din("moe_bg", [2, 4]); moe_wf = din("moe_wf", [2, D, 32]); moe_bf = din("moe_bf", [2, 32])
    moe_w1 = din("moe_w1", [2, 32, D, 512]); moe_w3 = din("moe_w3", [2, 32, D, 512]); moe_w2 = din("moe_w2", [2, 32, 512, D])
    ropec = din("k_ropec", [128, SEQ]); ropes = din("k_ropes", [128, SEQ])
    k_cc = din("k_cc", [256, 256]); k_sc = din("k_sc", [256, 256])
    k_cn = din("k_cn", [32, 128, 32 * 128], BF16); k_sn = din("k_sn", [32, 128, 32 * 128], BF16)
    out_d = DR("out", [SEQ, D], F32, out=True)

    GG = DR("GG", [512, SEQ], BF16); XR = DR("XR", [512, NTOK]); QT = DR("QT", [512, SEQ], BF16)
    KT = DR("KT", [512, NTOK], BF16); VV = DR("VV", [NTOK, 512], BF16); MT = DR("MT", [D, SEQ], BF16)
    X1 = DR("X1", [SEQ, D]); X2 = DR("X2", [SEQ, D]); X3 = DR("X3", [SEQ, D])
    XG = DR("XG", [NSLOT, D], BF16); YG = DR("YG", [NSLOT, D])
    W1B = DR("W1B", [2 * 32 * 128, 4096], BF16); W3B = DR("W3B", [2 * 32 * 128, 4096], BF16); W2B = DR("W2B", [2 * 32 * 128, 4096], BF16)
    UU = DR("UU", [SEQ, D], BF16); VW = DR("VW", [SEQ, D], BF16); Y1 = DR("Y1", [SEQ, D])
    ADAS = DR("ADAS", [2, 4, D])

    def sbp(name, shape, dt=F32):
        return T(P, gst.enter_context(nc.sbuf_tensor(name, list(shape), dt)), name)

    ident = sbp("ident", [128, 128]); ones_f = sbp("ones_f", [128, 128]); ones_b = sbp("ones_b", [128, 128], BF16)
    ustrict = sbp("ustrict", [128, 128]); ustrict_b = sbp("ustrict_b", [128, 128], BF16); ident_b = sbp("ident_b", [128, 128], BF16)
    iota_p = sbp("iota_p", [128, 1])
    adac = sbp("adac", [128, 2, 4, 8])
    adap = sbp("adap", [128, 2, 2, 8])
    adacx = sbp("adacx", [128, 2, 8])
    gbc = sbp("gbc", [128, 2, 2, D])
    lamc = sbp("lamc", [128, 2])
    eps_c = sbp("eps_c", [128, 1])

    K = Ctx()

    def dma(q, out, in_, reads, writes, key, accum=(), slow=False):
        P.op(q, lambda e: e.dma_start(out=out, in_=in_, allow_slow_non_contiguous=slow), reads=reads, writes=writes, dma=key, accum=accum)

    conv_res = [P.res("conv0"), P.res("conv1")]
    w1v_ = moe_w1.rearrange("l e (p r) n -> (l e p) (r n)", p=128)
    w3v_ = moe_w3.rearrange("l e (p r) n -> (l e p) (r n)", p=128)
    w2v_ = moe_w2.rearrange("l e (p r) n -> (l e p) (r n)", p=128)
    conv_list = []
    for l_ in range(2):
        for g in range(8):
            for (src, dst) in ((w1v_, W1B), (w3v_, W3B), (w2v_, W2B)):
                conv_list.append((l_, g, src, dst))
    conv_pos = [0, 0]

    def conv_issue(l_, n):
        for _ in range(n):
            items = [c for c in conv_list if c[0] == l_]
            if conv_pos[l_] >= len(items):
                return
            (_, g, src, dst) = items[conv_pos[l_]]; conv_pos[l_] += 1
            r0 = l_ * 4096 + g * 512
            P.op('pool', lambda e, src=src, dst=dst, r0=r0: e.dma_start(out=dst.ap[r0:r0 + 512, :], in_=src[r0:r0 + 512, :]), reads=[], writes=[], dma=conv_res[l_], accum=[dst.r], bg=True)

    phno = [0]

    def phase(fn):
        phno[0] += 1
        pfx = "p%d_" % phno[0]
        with contextlib.ExitStack() as st:
            def sb(name, shape, dt=F32):
                return T(P, st.enter_context(nc.sbuf_tensor(pfx + name, list(shape), dt)), name)

            def ps(name, shape=(128, 512), dt=F32):
                return T(P, st.enter_context(nc.psum_tensor(pfx + name, list(shape), dt)), name)
            fn(sb, ps)
            P.flush()

    def ln_stats(xt_ap, stats, mv, rstd, nmr, reads):
        P.op('dve', lambda e: e.bn_stats(out=stats[:, 0:6], in_=xt_ap[:, 0:512]), reads=reads, writes=[stats.r])
        P.op('dve', lambda e: e.bn_stats(out=stats[:, 6:12], in_=xt_ap[:, 512:1024]), reads=reads, writes=[stats.r])
        P.op('dve', lambda e: e.bn_aggr(out=mv[:, :], in_=stats[:, :]), reads=[stats.r], writes=[mv.r])
        P.op('act', lambda e: e.activation(out=rstd[:, :], in_=mv[:, 1:2], func=AF.Sqrt, bias=eps_c[:, 0:1], scale=1.0), reads=[mv.r, eps_c.r], writes=[rstd.r])
        P.op('dve', lambda e: e.reciprocal(out=rstd[:, :], in_=rstd[:, :]), reads=[rstd.r], writes=[rstd.r])
        P.op('dve', lambda e: e.scalar_tensor_tensor(out=nmr[:, :], in0=mv[:, 0:1], scalar=-1.0, in1=rstd[:, :], op0=ALU.mult, op1=ALU.mult), reads=[mv.r, rstd.r], writes=[nmr.r])

    def ph_prologue(sb, ps):
        P.op('pool', lambda e: e.memset(ident[:, :], 0.0), writes=[ident.r])
        P.op('pool', lambda e: e.affine_select(out=ident[:, :], in_=ident[:, :], pattern=[[-1, 128]], compare_op=ALU.not_equal, fill=1.0, base=0, channel_multiplier=1), reads=[ident.r], writes=[ident.r])
        P.op('pool', lambda e: e.memset(ones_f[:, :], 1.0), writes=[ones_f.r])
        P.op('pool', lambda e: e.memset(ones_b[:, :], 1.0), writes=[ones_b.r])
        P.op('pool', lambda e: e.memset(eps_c[:, :], EPS), writes=[eps_c.r])
        P.op('pool', lambda e: e.memset(ustrict[:, :], 1.0), writes=[ustrict.r])
        P.op('pool', lambda e: e.affine_select(out=ustrict[:, :], in_=ustrict[:, :], pattern=[[1, 128]], compare_op=ALU.is_gt, fill=0.0, base=0, channel_multiplier=-1), reads=[ustrict.r], writes=[ustrict.r])
        P.op('pool', lambda e: e.tensor_copy(out=ustrict_b[:, :], in_=ustrict[:, :]), reads=[ustrict.r], writes=[ustrict_b.r])
        P.op('pool', lambda e: e.tensor_copy(out=ident_b[:, :], in_=ident[:, :]), reads=[ident.r], writes=[ident_b.r])
        P.op('pool', lambda e: e.iota(iota_p[:, :], pattern=[[0, 1]], base=0, channel_multiplier=1, allow_small_or_imprecise_dtypes=True), writes=[iota_p.r])
        cc = sb("cc", [128, 2, 8]); sc = sb("sc", [128, 8, 2]); srep = sb("srep", [128, 8, 128])
        dma('sp', cc[:, 0, :], c_in.rearrange("(k p) -> p k", p=128), [], [cc.r], cc.r, slow=True)
        dma('sp', cc[:, 1, :], cctx_in.rearrange("(k p) -> p k", p=128), [], [cc.r], cc.r, slow=True)
        P.op('act', lambda e: e.activation(out=sc[:, :, :].rearrange("p k t -> p t k"), in_=cc[:, :, :], func=AF.Silu), reads=[cc.r], writes=[sc.r])
        for kc in range(8):
            P.op('dve', lambda e, kc=kc: e.tensor_scalar(out=srep[:, kc, :], in0=ones_f[:, :], scalar1=sc[:, kc, 0:1], scalar2=None, op0=ALU.mult), reads=[sc.r, ones_f.r], writes=[srep.r])
        wsl = [sb("wsl%d" % i, [128, 8, D]) for i in range(2)]
        badd = sb("badd", [128, 2, 6, 8]); bbc = sb("bbc", [128, D])
        dma('sp', badd[:, :, :, :], ada_b.rearrange("l (t n p) -> p l t n", p=128, n=8), [], [badd.r], badd.r, slow=True)
        pc = ps("pc", [128, 512]); pb = [ps("pb%d" % i, [128, 512]) for i in range(2)]
        colmap = {0: 0, 1: 1, 3: 2, 4: 3}
        i = 0
        for l in range(2):
            for term in range(6):
                w = wsl[i % 2]; i += 1
                dma('sp', w[:, :, :], ada_w[l, :, term * D:(term + 1) * D].rearrange("(k p) n -> p k n", p=128), [], [w.r], w.r)
                if term in colmap:
                    ci = colmap[term]
                    for n in range(8):
                        for kc in range(8):
                            P.op('pe', lambda e, w=w, n=n, kc=kc: e.matmul(pc[:, n * 2:n * 2 + 2], lhsT=w[:, kc, n * 128:(n + 1) * 128], rhs=sc[:, kc, :], start=(kc == 0), stop=(kc == 7)),
                                 reads=[w.r, sc.r], writes=[pc.r])
                    addc = 1.0 if term in (1, 4) else 0.0
                    P.op('dve', lambda e, l=l, term=term, ci=ci, addc=addc: e.scalar_tensor_tensor(out=adac[:, l, ci, :], in0=pc[:, 0:16:2], scalar=addc, in1=badd[:, l, term, :], op0=ALU.add, op1=ALU.add),
                         reads=[pc.r, badd.r], writes=[adac.r])
                    if l == 0 and term in (0, 1):
                        P.op('dve', lambda e, term=term, addc=addc: e.scalar_tensor_tensor(out=adacx[:, term, :], in0=pc[:, 1:16:2], scalar=addc, in1=badd[:, 0, term, :], op0=ALU.add, op1=ALU.add),
                             reads=[pc.r, badd.r], writes=[adacx.r])
                else:
                    gi = 0 if term == 2 else 1
                    dma('sp', bbc[:, :], ada_b[l, term * D:(term + 1) * D].partition_broadcast(128), [], [bbc.r], bbc.r)
                    for h in range(2):
                        for kc in range(8):
                            P.op('pe', lambda e, w=w, h=h, kc=kc: e.matmul(pb[h][:, :], lhsT=srep[:, kc, :], rhs=w[:, kc, h * 512:(h + 1) * 512], start=(kc == 0), stop=(kc == 7)),
                                 reads=[w.r, srep.r], writes=[pb[h].r])
                        P.op('dve', lambda e, l=l, gi=gi, h=h: e.tensor_tensor(out=gbc[:, l, gi, h * 512:(h + 1) * 512], in0=pb[h][:, :], in1=bbc[:, h * 512:(h + 1) * 512], op=ALU.add),
                             reads=[pb[h].r, bbc.r], writes=[gbc.r])
        for l in range(2):
            dma('sp', ADAS.ap[l].rearrange("t (n p) -> p t n", p=128), adac[:, l, :, :], [adac.r], [], adac.r, accum=[ADAS.r], slow=True)
        for l in range(2):
            dma('sp', adap[:, l, :, :], ADAS.ap[l, 2:4, :].rearrange("t (p n) -> p t n", n=8), [ADAS.r], [adap.r], adap.r, slow=True)
        dl = sb("dl", [1, 4, 64]); dp = sb("dp", [1, 2, 64]); dsum = sb("dsum", [1, 2]); lv = sb("lv", [1, 2])
        dma('sp', dl[:, :, :], da_lam.rearrange("(o a) d -> o a d", o=1), [], [dl.r], dl.r)
        P.op('dve', lambda e: e.tensor_tensor(out=dp[:, :, :], in0=dl[:, 0:4:2, :], in1=dl[:, 1:4:2, :], op=ALU.mult), reads=[dl.r], writes=[dp.r])
        P.op('dve', lambda e: e.reduce_sum(out=dsum[:, :], in_=dp[:, :, :], axis=AX.X), reads=[dp.r], writes=[dsum.r])
        P.op('act', lambda e: e.activation(out=dsum[:, :], in_=dsum[:, :], func=AF.Exp), reads=[dsum.r], writes=[dsum.r])
        P.op('dve', lambda e: e.scalar_tensor_tensor(out=lv[:, 1:2], in0=dsum[:, 0:1], scalar=0.2, in1=dsum[:, 1:2], op0=ALU.add, op1=ALU.subtract), reads=[dsum.r], writes=[lv.r])
        P.op('dve', lambda e: e.tensor_scalar(out=lv[:, 0:1], in0=lv[:, 1:2], scalar1=-1.0, scalar2=None, op0=ALU.mult), reads=[lv.r], writes=[lv.r])
        P.op('pe', lambda e: e.matmul(pc[:, 32:34], lhsT=ones_f[0:1, :], rhs=lv[:, :], start=True, stop=True), reads=[ones_f.r, lv.r], writes=[pc.r])
        P.op('dve', lambda e: e.tensor_copy(out=lamc[:, :], in_=pc[:, 32:34]), reads=[pc.r], writes=[lamc.r])

    phase(ph_prologue)

    def ph_s1(sb, ps):
        wb = sb("wb", [128, 8, 2560], BF16); wp = sb("wp", [128, 8, 1024], BF16)
        wst = [sb("wst%d" % i, [128, 8, 512]) for i in range(2)]
        for ch in range(5):
            w = wst[ch % 2]
            dma('sp', w[:, :, :], w_in[:, ch * 512:(ch + 1) * 512].rearrange("(k p) n -> p k n", p=128), [], [w.r], w.r)
            P.op('pool', lambda e, w=w, ch=ch: e.tensor_copy(out=wb[:, :, ch * 512:(ch + 1) * 512], in_=w[:, :, :]), reads=[w.r], writes=[wb.r])
        for kc in range(8):
            src = wb[:, kc, 1024:2048].rearrange("p (b h f) -> p b h f", h=2, f=16)
            dst = wp[:, kc, :].rearrange("p (b h f) -> p b h f", h=2, f=16)
            P.op('pool', lambda e, src=src, dst=dst: e.tensor_copy(out=dst[:, :, 0, :], in_=src[:, :, 1, :]), reads=[wb.r], writes=[wp.r])
            P.op('pool', lambda e, src=src, dst=dst: e.tensor_copy(out=dst[:, :, 1, :], in_=src[:, :, 0, :]), reads=[wb.r], writes=[wp.r])
        xt = [sb("xt%d" % i, [128, D]) for i in range(3)]
        xn = sb("xn", [128, 4, D])
        hT = [sb("hT%d" % i, [128, 8, 512], BF16) for i in range(2)]
        rc = [sb("rc%d" % i, [128, 512]) for i in range(2)]; rs = [sb("rs%d" % i, [128, 512]) for i in range(2)]
        stats = sb("stats", [128, 12]); mv = sb("mv", [128, 2]); rstd = sb("rstd", [128, 1]); nmr = sb("nmr", [128, 1])
        ogg = [sb("ogg%d" % i, [128, 512], BF16) for i in range(2)]
        oxr = [sb("oxr%d" % i, [128, 512]) for i in range(2)]
        oqk = [sb("oqk%d" % i, [128, 512], BF16) for i in range(2)]
        t1 = sb("t1", [128, 512]); t2 = sb("t2", [128, 512])
        ov = [sb("ov%d" % i, [128, 512], BF16) for i in range(2)]
        pT = [ps("pT%d" % i) for i in range(2)]
        pM = [ps("pM%d" % i) for i in range(4)]
        cnt = {'x': 0, 'm': 0, 'gg': 0, 'xr': 0, 'qk': 0, 'v': 0}

        def nxt(k, n):
            v = cnt[k]; cnt[k] += 1
            return v % n

        for s in range(9):
            isctx = (s == 0)
            ntile = 2 if isctx else 4
            ntk = ntile * 128
            tok0 = 0 if isctx else NCTX + (s - 1) * 512
            lat0 = (s - 1) * 512
            h = hT[s % 2]
            for j in range(ntile):
                x = xt[nxt('x', 3)]
                src = ctx_in[j * 128:(j + 1) * 128, :] if isctx else x_in[lat0 + j * 128: lat0 + (j + 1) * 128, :]
                dma('sp', x[:, :], src, [], [x.r], x.r)
                ln_stats(x, stats, mv, rstd, nmr, [x.r])
                P.op('act', lambda e, x=x, j=j: e.activation(out=xn[:, j, :], in_=x[:, :], func=AF.Identity, scale=rstd[:, 0:1], bias=nmr[:, 0:1]), reads=[x.r, rstd.r, nmr.r], writes=[xn.r])
            for kc in range(8):
                p = pT[kc % 2]
                for j in range(ntile):
                    P.op('pe', lambda e, p=p, j=j, kc=kc: e.transpose(p[:, j * 128:(j + 1) * 128], xn[:, j, kc * 128:(kc + 1) * 128], ident[:, :]), reads=[xn.r, ident.r], writes=[p.r])
                scl = adacx[:, 1, kc:kc + 1] if isctx else adac[:, 0, 1, kc:kc + 1]
                bia = adacx[:, 0, kc:kc + 1] if isctx else adac[:, 0, 0, kc:kc + 1]
                P.op('act', lambda e, p=p, kc=kc, scl=scl, bia=bia, h=h, ntk=ntk: e.activation(out=h[:, kc, 0:ntk], in_=p[:, 0:ntk], func=AF.Identity, scale=scl, bias=bia),
                     reads=[p.r, adac.r, adacx.r], writes=[h.r])
            if not isctx:
                rcb = rc[s % 2]; rsb = rs[s % 2]
                dma('sp', rcb[:, :], ropec[:, lat0:lat0 + 512], [], [rcb.r], rcb.r)
                dma('sp', rsb[:, :], ropes[:, lat0:lat0 + 512], [], [rsb.r], rsb.r)

            def mm_feat(pm, wt, c0, h=h, ntk=ntk):
                for kc in range(8):
                    P.op('pe', lambda e, kc=kc: e.matmul(pm[:, 0:ntk], lhsT=wt[:, kc, c0:c0 + 128], rhs=h[:, kc, 0:ntk], start=(kc == 0), stop=(kc == 7)), reads=[wt.r, h.r], writes=[pm.r])

            if not isctx:
                for c4 in range(4):
                    pm = pM[nxt('m', 4)]
                    mm_feat(pm, wb, c4 * 128)
                    o = ogg[nxt('gg', 2)]
                    P.op('act', lambda e, pm=pm, o=o: e.activation(out=o[:, :], in_=pm[:, :], func=AF.Gelu_apprx_tanh), reads=[pm.r], writes=[o.r])
                    dma('act', GG.ap[c4 * 128:(c4 + 1) * 128, lat0:lat0 + 512], o[:, :], [o.r], [], o.r, accum=[GG.r])
            for c4 in range(4):
                pm = pM[nxt('m', 4)]
                mm_feat(pm, wb, 512 + c4 * 128)
                o = oxr[nxt('xr', 2)]
                P.op('act', lambda e, pm=pm, o=o, ntk=ntk: e.copy(out=o[:, 0:ntk], in_=pm[:, 0:ntk]), reads=[pm.r], writes=[o.r])
                dma('act', XR.ap[c4 * 128:(c4 + 1) * 128, tok0:tok0 + ntk], o[:, 0:ntk], [o.r], [], o.r, accum=[XR.r])
            for which in range(2):
                if which == 0 and isctx:
                    continue
                for hd in range(4):
                    c0 = 1024 + which * 512 + hd * 128
                    pm = pM[nxt('m', 4)]
                    mm_feat(pm, wb, c0)
                    o = oqk[nxt('qk', 2)]
                    if isctx:
                        P.op('dve', lambda e, pm=pm, o=o, ntk=ntk: e.tensor_copy(out=o[:, 0:ntk], in_=pm[:, 0:ntk]), reads=[pm.r], writes=[o.r])
                    else:
                        pm2 = pM[nxt('m', 4)]
                        mm_feat(pm2, wp, which * 512 + hd * 128)
                        P.op('dve', lambda e, pm=pm, rcb=rcb: e.tensor_tensor(out=t1[:, :], in0=pm[:, :], in1=rcb[:, :], op=ALU.mult), reads=[pm.r, rcb.r], writes=[t1.r])
                        P.op('dve', lambda e, pm2=pm2, rsb=rsb: e.tensor_tensor(out=t2[:, :], in0=pm2[:, :], in1=rsb[:, :], op=ALU.mult), reads=[pm2.r, rsb.r], writes=[t2.r])
                        P.op('pool', lambda e, o=o: e.tensor_tensor(out=o[:, :], in0=t1[:, :], in1=t2[:, :], op=ALU.add), reads=[t1.r, t2.r], writes=[o.r])
                    if which == 0:
                        dma('pool', QT.ap[hd * 128:(hd + 1) * 128, lat0:lat0 + 512], o[:, :], [o.r], [], o.r, accum=[QT.r])
                    else:
                        dma('pool', KT.ap[hd * 128:(hd + 1) * 128, tok0:tok0 + ntk], o[:, 0:ntk], [o.r], [], o.r, accum=[KT.r])
            for j in range(ntile):
                pm = pM[nxt('m', 4)]
                for kc in range(8):
                    P.op('pe', lambda e, pm=pm, kc=kc, j=j, h=h: e.matmul(pm[:, :], lhsT=h[:, kc, j * 128:(j + 1) * 128], rhs=wb[:, kc, 2048:2560], start=(kc == 0), stop=(kc == 7)), reads=[wb.r, h.r], writes=[pm.r])
                o = ov[nxt('v', 2)]
                P.op('act', lambda e, pm=pm, o=o: e.copy(out=o[:, :], in_=pm[:, :]), reads=[pm.r], writes=[o.r])
                dma('act', VV.ap[tok0 + j * 128: tok0 + (j + 1) * 128, :], o[:, :], [o.r], [], o.r, accum=[VV.r])

    phase(ph_s1)
    if 's1' in dbg:
        return nc, gst

    def ph_s2(sb, ps):
        LB = NTOK + 6
        xr = sb("xr", [128, LB]); xc = sb("xc", [128, NTOK]); rr = sb("rr", [128, NTOK]); ii = sb("ii", [128, NTOK])
        a2 = sb("a2", [128, NTOK]); hf = sb("hf", [128, NTOK]); hb = sb("hb", [128, NTOK])
        xcb = sb("xcb", [128, NTOK], BF16); gg = sb("gg", [128, SEQ], BF16); yo = sb("yo", [128, SEQ], BF16)
        cw = sb("cw", [128, 4, 4]); cb = sb("cb", [128, 4]); gba = sb("gba", [128, 2, 4]); gbx = sb("gbx", [128, 2, 4])
        lam = sb("lam", [128, 2, 4]); cA = sb("cA", [128, 2, 4]); cA2 = sb("cA2", [128, 2, 4])
        wgs = sb("wgs", [128, 16, 128]); wg = sb("wg", [128, 16, 128], BF16)
        pm = [ps("pm%d" % i) for i in range(4)]
        P.op('pool', lambda e: e.memset(xr[:, :], 0.0), writes=[xr.r])
        P.op('pool', lambda e: e.memset(wgs[:, :, :], 0.0), writes=[wgs.r])
        for k in range(4):
            dma('sp', cw[:, :, k], conv_w[k].rearrange("(c p) -> p c", p=128), [], [], cw.r, accum=[cw.r], slow=True)
        dma('sp', cb[:, :], conv_b.rearrange("(c p) -> p c", p=128), [], [cb.r], cb.r, slow=True)
        dma('sp', gba[:, :, :], ga_b.rearrange("d (c p) -> p d c", p=128), [], [gba.r], gba.r, slow=True)
        dma('sp', gbx[:, :, :], gx_b.rearrange("d (c p) -> p d c", p=128), [], [gbx.r], gbx.r, slow=True)
        dma('sp', lam[:, :, :], lru_lam.rearrange("d (c p) -> p d c", p=128), [], [lam.r], lam.r, slow=True)
        for typ, gw in enumerate((ga_w, gx_w)):
            for d in range(2):
                for cc in range(4):
                    mi = (typ * 2 + d) * 4 + cc
                    for hh in range(2):
                        dma('sp', wgs[hh * 64:(hh + 1) * 64, mi, hh * 64:(hh + 1) * 64], gw[d, 2 * cc + hh], [wgs.r], [], wgs.r, accum=[wgs.r])
        P.op('pool', lambda e: e.tensor_copy(out=wg[:, :, :], in_=wgs[:, :, :]), reads=[wgs.r], writes=[wg.r])
        P.op('act', lambda e: e.activation(out=cA[:, :, :], in_=lam[:, :, :], func=AF.Exp, scale=-1.0), reads=[lam.r], writes=[cA.r])
        P.op('act', lambda e: e.activation(out=cA[:, :, :], in_=cA[:, :, :], func=AF.Ln, bias=ones_f[:, 0:1], scale=1.0), reads=[cA.r, ones_f.r], writes=[cA.r])
        P.op('dve', lambda e: e.tensor_scalar(out=cA2[:, :, :], in0=cA[:, :, :], scalar1=-16.0, scalar2=None, op0=ALU.mult), reads=[cA.r], writes=[cA2.r])
        P.op('dve', lambda e: e.tensor_scalar(out=cA[:, :, :], in0=cA[:, :, :], scalar1=-8.0, scalar2=None, op0=ALU.mult), reads=[cA.r], writes=[cA.r])
        segs = [(0, NCTX, 0), (NCTX, SEQ, 259)]
        chunks = [(c0, min(512, NTOK - c0)) for c0 in range(0, NTOK, 512)]
        mcnt = [0]
        for cc in range(4):
            conv_issue(0, 6)
            dma('sp', xr[:, 2:2 + NCTX], XR.ap[cc * 128:(cc + 1) * 128, 0:NCTX], [XR.r], [xr.r], xr.r)
            dma('sp', xr[:, 261:261 + SEQ], XR.ap[cc * 128:(cc + 1) * 128, NCTX:NTOK], [XR.r], [], xr.r, accum=[xr.r])
            dma('sp', gg[:, :], GG.ap[cc * 128:(cc + 1) * 128, :], [GG.r], [gg.r], gg.r)
            for (o0, ln, b0) in segs:
                P.op('dve', lambda e, o0=o0, ln=ln, b0=b0, cc=cc: e.tensor_scalar(out=xc[:, o0:o0 + ln], in0=xr[:, b0:b0 + ln], scalar1=cw[:, cc, 0:1], scalar2=cb[:, cc:cc + 1], op0=ALU.mult, op1=ALU.add),
                     reads=[xr.r, cw.r, cb.r], writes=[xc.r])
                for k in range(1, 4):
                    P.op('dve', lambda e, o0=o0, ln=ln, b0=b0, cc=cc, k=k: e.scalar_tensor_tensor(out=xc[:, o0:o0 + ln], in0=xr[:, b0 + k:b0 + k + ln], scalar=cw[:, cc, k:k + 1], in1=xc[:, o0:o0 + ln], op0=ALU.mult, op1=ALU.add),
                         reads=[xr.r, cw.r, xc.r], writes=[xc.r])
            P.op('pool', lambda e: e.tensor_copy(out=xcb[:, :], in_=xc[:, :]), reads=[xc.r], writes=[xcb.r])
            for d in range(2):
                for (c0, cl) in chunks:
                    for typ, dst, bb in ((0, rr, gba), (1, ii, gbx)):
                        p = pm[mcnt[0] % 4]; mcnt[0] += 1
                        mi = (typ * 2 + d) * 4 + cc
                        P.op('pe', lambda e, p=p, mi=mi, c0=c0, cl=cl: e.matmul(p[:, 0:cl], lhsT=wg[:, mi, :], rhs=xcb[:, c0:c0 + cl], start=True, stop=True), reads=[wg.r, xcb.r], writes=[p.r])
                        P.op('act', lambda e, p=p, dst=dst, bb=bb, c0=c0, cl=cl, d=d, cc=cc: e.activation(out=dst[:, c0:c0 + cl], in_=p[:, 0:cl], func=AF.Sigmoid, bias=bb[:, d, cc:cc + 1], scale=1.0),
                             reads=[p.r, bb.r], writes=[dst.r])
                P.op('act', lambda e, d=d, cc=cc: e.activation(out=a2[:, :], in_=rr[:, :], func=AF.Exp, scale=cA2[:, d, cc:cc + 1]), reads=[rr.r, cA2.r], writes=[a2.r])
                P.op('act', lambda e, d=d, cc=cc: e.activation(out=rr[:, :], in_=rr[:, :], func=AF.Exp, scale=cA[:, d, cc:cc + 1]), reads=[rr.r, cA.r], writes=[rr.r])
                P.op('dve', lambda e: e.tensor_scalar(out=a2[:, :], in0=a2[:, :], scalar1=-1.0, scalar2=1.0, op0=ALU.mult, op1=ALU.add), reads=[a2.r], writes=[a2.r])
                P.op('act', lambda e: e.activation(out=a2[:, :], in_=a2[:, :], func=AF.Sqrt), reads=[a2.r], writes=[a2.r])
                P.op('dve', lambda e: e.tensor_tensor(out=a2[:, :], in0=a2[:, :], in1=ii[:, :], op=ALU.mult), reads=[a2.r, ii.r], writes=[a2.r])
                P.op('dve', lambda e: e.tensor_tensor(out=a2[:, :], in0=a2[:, :], in1=xc[:, :], op=ALU.mult), reads=[a2.r, xc.r], writes=[a2.r])
                if d == 0:
                    P.op('dve', lambda e: e.tensor_tensor_scan(out=hf[:, :], data0=rr[:, :], data1=a2[:, :], initial=0.0, op0=ALU.mult, op1=ALU.add), reads=[rr.r, a2.r], writes=[hf.r])
                else:
                    P.op('dve', lambda e: e.tensor_tensor_scan(out=hb[:, NCTX - 1::-1], data0=rr[:, NCTX - 1::-1], data1=a2[:, NCTX - 1::-1], initial=0.0, op0=ALU.mult, op1=ALU.add), reads=[rr.r, a2.r], writes=[hb.r])
                    P.op('dve', lambda e: e.tensor_tensor_scan(out=hb[:, NTOK - 1:NCTX - 1:-1], data0=rr[:, NTOK - 1:NCTX - 1:-1], data1=a2[:, NTOK - 1:NCTX - 1:-1], initial=hb[:, 0:1], op0=ALU.mult, op1=ALU.add),
                         reads=[rr.r, a2.r, hb.r], writes=[hb.r])
            P.op('pool', lambda e: e.tensor_tensor(out=hf[:, NCTX:NTOK], in0=hf[:, NCTX:NTOK], in1=hb[:, NCTX:NTOK], op=ALU.add), reads=[hf.r, hb.r], writes=[hf.r])
            P.op('pool', lambda e: e.tensor_tensor(out=yo[:, :], in0=hf[:, NCTX:NTOK], in1=gg[:, :], op=ALU.mult), reads=[hf.r, gg.r], writes=[yo.r])
            dma('pool', MT.ap[cc * 128:(cc + 1) * 128, :], yo[:, :], [yo.r], [], yo.r, accum=[MT.r])

    phase(ph_s2)

    def ph_s3(sb, ps):
        NKT = NTOK // 128
        vt = sb("vt", [128, NKT, 512], BF16)
        kt_ = [sb("kt%d" % i, [128, NTOK], BF16) for i in range(2)]
        qt_ = [sb("qt%d" % i, [128, SEQ], BF16) for i in range(2)]
        pt = [sb("pt%d" % i, [128, 1024], BF16) for i in range(2)]
        r1 = sb("r1", [128, 512]); o1 = sb("o1", [128, 512]); r2 = sb("r2", [128, 512]); o2 = sb("o2", [128, 512])
        sq = sb("sq", [128, 512], BF16); rsd = sb("rsd", [128, 512]); ob = [sb("ob%d" % i, [128, 512], BF16) for i in range(2)]
        g08 = sb("g08", [128, 1])
        psS = [ps("psS%d" % i, [128, 1024]) for i in range(2)]
        psO = [ps("psO%d" % i) for i in range(2)]; psD = [ps("psD%d" % i) for i in range(2)]
        accA = [sb("accA%d" % i, [128, 768]) for i in range(2)]; accB = [sb("accB%d" % i, [128, 256]) for i in range(2)]
        dsb = [sb("dsb%d" % i, [128, 512]) for i in range(2)]
        dma('sp', g08[:, :], da_sub.rearrange("(p o) -> p o", o=1), [], [g08.r], g08.r, slow=True)
        P.op('dve', lambda e: e.tensor_scalar(out=g08[:, :], in0=g08[:, :], scalar1=0.8, scalar2=None, op0=ALU.mult), reads=[g08.r], writes=[g08.r])
        dma('sp', vt[:, :, :], VV.ap.rearrange("(t p) n -> p t n", p=128), [VV.r], [vt.r], vt.r)
        oc = [0]
        for hd in range(4):
            kt = kt_[hd % 2]; qt = qt_[hd % 2]
            dma('sp', kt[:, :], KT.ap[hd * 128:(hd + 1) * 128, :], [KT.r], [kt.r], kt.r)
            dma('sp', qt[:, :], QT.ap[hd * 128:(hd + 1) * 128, :], [QT.r], [qt.r], qt.r)
            for qc in range(8):
                q0 = qc * 512

                def qk(t, kt=kt, qt=qt, q0=q0):
                    S = psS[t % 2]
                    for i in range(2):
                        P.op('pe', lambda e, S=S, i=i, t=t: e.matmul(S[:, i * 512:(i + 1) * 512], lhsT=kt[i * 64:(i + 1) * 64, t * 128:(t + 1) * 128], rhs=qt[i * 64:(i + 1) * 64, q0:q0 + 512], start=True, stop=True),
                             reads=[kt.r, qt.r], writes=[S.r])
                qk(0)
                for t in range(NKT):
                    S = psS[t % 2]; p = pt[t % 2]
                    P.op('act', lambda e, S=S, p=p: e.activation(out=p[:, :], in_=S[:, :], func=AF.Exp, scale=0.125), reads=[S.r], writes=[p.r])
                    if t + 1 < NKT:
                        qk(t + 1)
                    for i in range(2):
                        P.op('pe', lambda e, p=p, i=i, t=t, hd=hd: e.matmul(psO[i][:, :], lhsT=vt[:, t, hd * 128:(hd + 1) * 128], rhs=p[:, i * 512:(i + 1) * 512], start=(t == 0), stop=(t == NKT - 1)),
                             reads=[vt.r, p.r], writes=[psO[i].r])
                    for (eng, ac, c0, c1) in (('dve', accA[qc % 2], 0, 768), ('pool', accB[qc % 2], 768, 1024)):
                        if t == 0:
                            P.op(eng, lambda e, p=p, ac=ac, c0=c0, c1=c1: e.tensor_copy(out=ac[:, :], in_=p[:, c0:c1]), reads=[p.r], writes=[ac.r])
                        else:
                            P.op(eng, lambda e, p=p, ac=ac, c0=c0, c1=c1: e.tensor_tensor(out=ac[:, :], in0=ac[:, :], in1=p[:, c0:c1], op=ALU.add), reads=[p.r, ac.r], writes=[ac.r])
                aA = accA[qc % 2]; aB = accB[qc % 2]
                P.op('pe', lambda e, aA=aA: e.matmul(psD[0][:, :], lhsT=ones_f[:, :], rhs=aA[:, 0:512], start=True, stop=True), reads=[ones_f.r, aA.r], writes=[psD[0].r])
                P.op('pe', lambda e, aA=aA: e.matmul(psD[1][:, 0:256], lhsT=ones_f[:, :], rhs=aA[:, 512:768], start=True, stop=True), reads=[ones_f.r, aA.r], writes=[psD[1].r])
                P.op('pe', lambda e, aB=aB: e.matmul(psD[1][:, 256:512], lhsT=ones_f[:, :], rhs=aB[:, :], start=True, stop=True), reads=[ones_f.r, aB.r], writes=[psD[1].r])
                for i in range(2):
                    P.op('act', lambda e, i=i: e.activation(out=dsb[i][:, :], in_=psD[i][:, :], func=AF.Ln), reads=[psD[i].r], writes=[dsb[i].r])
                    P.op('act', lambda e, i=i: e.activation(out=dsb[i][:, :], in_=dsb[i][:, :], func=AF.Exp, scale=-1.0), reads=[dsb[i].r], writes=[dsb[i].r])
                P.op('dve', lambda e: e.tensor_tensor(out=o1[:, :], in0=psO[0][:, :], in1=dsb[0][:, :], op=ALU.mult), reads=[psO[0].r, dsb[0].r], writes=[o1.r])
                P.op('dve', lambda e: e.tensor_tensor(out=o2[:, :], in0=psO[1][:, :], in1=dsb[1][:, :], op=ALU.mult), reads=[psO[1].r, dsb[1].r], writes=[o2.r])
                P.op('dve', lambda e: e.scalar_tensor_tensor(out=o1[:, :], in0=o2[:, :], scalar=lamc[:, 0:1], in1=o1[:, :], op0=ALU.mult, op1=ALU.add), reads=[o2.r, o1.r, lamc.r], writes=[o1.r])
                P.op('pool', lambda e: e.tensor_tensor(out=sq[:, :], in0=o1[:, :], in1=o1[:, :], op=ALU.mult), reads=[o1.r], writes=[sq.r])
                S = psS[0]
                P.op('pe', lambda e, S=S: e.matmul(S[:, 0:512], lhsT=ones_b[:, :], rhs=sq[:, :], start=True, stop=True), reads=[ones_b.r, sq.r], writes=[S.r])
                P.op('act', lambda e, S=S: e.activation(out=rsd[:, :], in_=S[:, 0:512], func=AF.Ln, bias=eps_c[:, 0:1], scale=1.0 / 128.0), reads=[S.r, eps_c.r], writes=[rsd.r])
                P.op('act', lambda e: e.activation(out=rsd[:, :], in_=rsd[:, :], func=AF.Exp, scale=-0.5), reads=[rsd.r], writes=[rsd.r])
                P.op('dve', lambda e: e.tensor_tensor(out=o1[:, :], in0=o1[:, :], in1=rsd[:, :], op=ALU.mult), reads=[o1.r, rsd.r], writes=[o1.r])
                o = ob[oc[0] % 2]; oc[0] += 1
                P.op('act', lambda e, o=o: e.activation(out=o[:, :], in_=o1[:, :], func=AF.Identity, scale=g08[:, 0:1]), reads=[o1.r, g08.r], writes=[o.r])
                dma('act', MT.ap[512 + hd * 128:512 + (hd + 1) * 128, q0:q0 + 512], o[:, :], [o.r], [], o.r, accum=[MT.r])

    phase(ph_s3)

    def load_bc(sb, l, j):
        lg = sb("lgbc", [128, D]); lb = sb("lbbc", [128, D])
        dma('sp', lg[:, :], ln_g[l, j].partition_broadcast(128), [], [lg.r], lg.r)
        dma('sp', lb[:, :], ln_b[l, j].partition_broadcast(128), [], [lb.r], lb.r)
        return lg, lb

    def resid_A(x, t, z, lnt, l, gi):
        stats, mv, rstd, nmr = lnt
        P.op('dve', lambda e: e.tensor_tensor(out=t[:, :], in0=t[:, :], in1=gbc[:, l, gi, :], op=ALU.mult), reads=[t.r, gbc.r], writes=[t.r])
        P.op('dve', lambda e: e.scalar_tensor_tensor(out=z[:, :], in0=x[:, :], scalar=ALPHA, in1=t[:, :], op0=ALU.mult, op1=ALU.add), reads=[x.r, t.r], writes=[z.r])
        ln_stats(z, stats, mv, rstd, nmr, [z.r])

    def resid_B(z, lnt, lg, lb, o):
        stats, mv, rstd, nmr = lnt
        P.op('act', lambda e: e.activation(out=z[:, :], in_=z[:, :], func=AF.Identity, scale=rstd[:, 0:1], bias=nmr[:, 0:1]), reads=[z.r, rstd.r, nmr.r], writes=[z.r])
        P.op('pool', lambda e: e.tensor_tensor(out=z[:, :], in0=z[:, :], in1=lg[:, :], op=ALU.mult), reads=[z.r, lg.r], writes=[z.r])
        P.op('dve', lambda e: e.tensor_tensor(out=o[:, :], in0=z[:, :], in1=lb[:, :], op=ALU.add), reads=[z.r, lb.r], writes=[o.r])

    def ph_s4(sb, ps):
        wo = sb("wo", [128, 8, D], BF16)
        wst = [sb("wst%d" % i, [128, 8, 512]) for i in range(2)]
        for ch in range(2):
            w = wst[ch]
            dma('sp', w[:, :, :], ev_w_out[:, ch * 512:(ch + 1) * 512].rearrange("(k p) n -> p k n", p=128), [], [w.r], w.r)
            P.op('pool', lambda e, w=w, ch=ch: e.tensor_copy(out=wo[:, :, ch * 512:(ch + 1) * 512], in_=w[:, :, :]), reads=[w.r], writes=[wo.r])
        lg, lb = load_bc(sb, 0, 0)
        mt = [sb("mt%d" % i, [128, 8, 512], BF16) for i in range(2)]
        xt = [sb("xt%d" % i, [128, D]) for i in range(NB)]; tt = [sb("tt%d" % i, [128, D]) for i in range(NB)]
        zz = [sb("zz%d" % i, [128, D]) for i in range(NB)]; oo = [sb("oo%d" % i, [128, D]) for i in range(NB)]
        lnts = [(sb("stats%d" % i, [128, 12]), sb("mv%d" % i, [128, 2]), sb("rstd%d" % i, [128, 1]), sb("nmr%d" % i, [128, 1])) for i in range(NB)]
        py = [ps("py%d" % i, [128, 1024]) for i in range(3)]
        def stB(ti):
            z = zz[ti % NB]; o = oo[ti % NB]
            resid_B(z, lnts[ti % NB], lg, lb, o)
            if ti >= 1:
                stS(ti - 1)

        def stS(ti):
            o = oo[ti % NB]
            dma('pool', X1.ap[ti * 128:(ti + 1) * 128, :], o[:, :], [o.r], [], o.r, accum=[X1.r])
        for s in range(8):
            m = mt[s % 2]
            dma('sp', m[:, :, :], MT.ap[:, s * 512:(s + 1) * 512].rearrange("(k p) t -> p k t", p=128), [MT.r], [m.r], m.r)
            for j in range(4):
                ti = s * 4 + j
                x = xt[ti % NB]; t = tt[ti % NB]; z = zz[ti % NB]; y = py[ti % 3]; lnt = lnts[ti % NB]
                dma('sp', x[:, :], x_in[ti * 128:(ti + 1) * 128, :], [], [x.r], x.r)
                for h in range(2):
                    for kc in range(8):
                        P.op('pe', lambda e, y=y, m=m, h=h, kc=kc, j=j: e.matmul(y[:, h * 512:(h + 1) * 512], lhsT=m[:, kc, j * 128:(j + 1) * 128], rhs=wo[:, kc, h * 512:(h + 1) * 512], start=(kc == 0), stop=(kc == 7)),
                             reads=[m.r, wo.r], writes=[y.r])
                P.op('act', lambda e, y=y, t=t: e.copy(out=t[:, :], in_=y[:, :]), reads=[y.r], writes=[t.r])
                resid_A(x, t, z, lnt, 0, 0)
                if ti >= 1:
                    stB(ti - 1)
        stB(NT - 1)
        stS(NT - 1)

    phase(ph_s4)
    if 's4' in dbg:
        return nc, gst

    Am = sbp("Am", [128, NT, 2, 32]); RK = sbp("RK", [128, NT, 2]); WG = sbp("WG", [128, NT, 2])
    SLOT = sbp("SLOT", [128, NT * 2], I32); carry = sbp("carry", [128, 32]); idxW = sbp("idxW", [128, NBLK], I32)
    pstart = sbp("pstart", [128, 32])

    def moe(l, Xin, Xout):
        def ph_r(sb, ps):
            wr = sb("wr", [128, 8, 36]); rb = sb("rb", [128, 36])
            dma('sp', wr[:, :, 0:4], moe_wg[l].rearrange("(k p) g -> p k g", p=128), [], [], wr.r, accum=[wr.r], slow=True)
            dma('sp', wr[:, :, 4:36], moe_wf[l].rearrange("(k p) g -> p k g", p=128), [], [], wr.r, accum=[wr.r], slow=True)
            dma('sp', rb[:, 0:4], moe_bg[l].partition_broadcast(128), [], [], rb.r, accum=[rb.r], slow=True)
            dma('sp', rb[:, 4:36], moe_bf[l].partition_broadcast(128), [], [], rb.r, accum=[rb.r], slow=True)
            LG = sb("LG", [128, NT, 36]); xnb = sb("xnb", [128, NT, D], BF16)
            xt = [sb("xt%d" % i, [128, D]) for i in range(NB)]; xn = [sb("xn%d" % i, [128, D]) for i in range(NB)]
            tokT = [sb("tokT%d" % i, [128, 8, 128]) for i in range(NB)]
            lnts = [(sb("stats%d" % i, [128, 12]), sb("mv%d" % i, [128, 2]), sb("rstd%d" % i, [128, 1]), sb("nmr%d" % i, [128, 1])) for i in range(NB)]
            pT = [ps("pT%d" % i) for i in range(4)]; plg = [ps("plg%d" % i) for i in range(2)]
            for ti in range(NT):
                x = xt[ti % NB]; n = xn[ti % NB]; tk = tokT[ti % NB]; lnt = lnts[ti % NB]; pl = plg[ti % 2]
                dma('sp', x[:, :], Xin.ap[ti * 128:(ti + 1) * 128, :], [Xin.r], [x.r], x.r)
                ln_stats(x, lnt[0], lnt[1], lnt[2], lnt[3], [x.r])
                P.op('act', lambda e, x=x, n=n, lnt=lnt: e.activation(out=n[:, :], in_=x[:, :], func=AF.Identity, scale=lnt[2][:, 0:1], bias=lnt[3][:, 0:1]), reads=[x.r, lnt[2].r, lnt[3].r], writes=[n.r])
                P.op('pool', lambda e, n=n, ti=ti: e.tensor_copy(out=xnb[:, ti, :], in_=n[:, :]), reads=[n.r], writes=[], accum=[xnb.r])
                for kc in range(8):
                    p = pT[(ti % 2) * 2 + kc // 4]
                    P.op('pe', lambda e, p=p, kc=kc, n=n: e.transpose(p[:, (kc % 4) * 128:(kc % 4 + 1) * 128], n[:, kc * 128:(kc + 1) * 128], ident[:, :]), reads=[n.r, ident.r], writes=[p.r])
                for kc in range(8):
                    p = pT[(ti % 2) * 2 + kc // 4]
                    P.op('act', lambda e, p=p, kc=kc, tk=tk: e.activation(out=tk[:, kc, :], in_=p[:, (kc % 4) * 128:(kc % 4 + 1) * 128], func=AF.Identity, scale=adac[:, l, 3, kc:kc + 1], bias=adac[:, l, 2, kc:kc + 1]),
                         reads=[p.r, adac.r], writes=[tk.r])
                for kc in range(8):
                    P.op('pe', lambda e, kc=kc, tk=tk, pl=pl: e.matmul(pl[:, 0:36], lhsT=tk[:, kc, :], rhs=wr[:, kc, :], start=(kc == 0), stop=(kc == 7)), reads=[tk.r, wr.r], writes=[pl.r])
                P.op('dve', lambda e, pl=pl, ti=ti: e.tensor_tensor(out=LG[:, ti, :], in0=pl[:, 0:36], in1=rb[:, :], op=ALU.add), reads=[pl.r, rb.r], writes=[], accum=[LG.r])
            gmax = sb("gmax", [128, NT]); dlt = sb("dlt", [128, NT, 4]); gmask = sb("gmask", [128, NT, 4]); sg = sb("sg", [128, NT])
            pen = sb("pen", [128, NT, 4]); fm = sb("fm", [128, NT, 32]); T8 = sb("T8", [128, NT, 8]); dd = sb("dd", [128, NT]); s0 = sb("s0", [128, NT])
            Asb = sb("Asb", [128, NT, 32], BF16); tmp3 = sb("tmp3", [128, NT, 32]); slk = sb("slk", [128, NT])
            ppf = ps("ppf", [128, 1024]); pcnt = plg[0]
            P.op('dve', lambda e: e.reduce_max(out=gmax[:, :], in_=LG[:, :, 0:4], axis=AX.X), reads=[LG.r], writes=[gmax.r])
            P.op('dve', lambda e: e.tensor_tensor(out=dlt[:, :, :], in0=LG[:, :, 0:4], in1=gmax[:, :].unsqueeze(2).to_broadcast([128, NT, 4]), op=ALU.subtract), reads=[LG.r, gmax.r], writes=[dlt.r])
            P.op('dve', lambda e: e.tensor_scalar(out=gmask[:, :, :], in0=dlt[:, :, :], scalar1=0.0, scalar2=None, op0=ALU.is_equal), reads=[dlt.r], writes=[gmask.r])
            P.op('act', lambda e: e.activation(out=dlt[:, :, :], in_=dlt[:, :, :], func=AF.Exp), reads=[dlt.r, gmask.r], writes=[dlt.r])
            P.op('dve', lambda e: e.reduce_sum(out=sg[:, :], in_=dlt[:, :, :], axis=AX.X), reads=[dlt.r], writes=[sg.r])
            P.op('dve', lambda e: e.reciprocal(out=sg[:, :], in_=sg[:, :]), reads=[sg.r], writes=[sg.r])
            P.op('dve', lambda e: e.tensor_scalar(out=pen[:, :, :], in0=gmask[:, :, :], scalar1=-1.0, scalar2=1e30, op0=ALU.add, op1=ALU.mult), reads=[gmask.r], writes=[pen.r])
            P.op('dve', lambda e: e.tensor_tensor(out=fm[:, :, :].rearrange("p t (g j) -> p t g j", g=4), in0=LG[:, :, 4:36].rearrange("p t (g j) -> p t g j", g=4), in1=pen[:, :, :].unsqueeze(3).to_broadcast([128, NT, 4, 8]), op=ALU.add),
                 reads=[LG.r, pen.r], writes=[fm.r])
            for ti in range(NT):
                P.op('dve', lambda e, ti=ti: e.max(out=T8[:, ti, :], in_=fm[:, ti, :]), reads=[fm.r], writes=[], accum=[T8.r])
            for k in range(2):
                P.op('dve', lambda e, k=k: e.tensor_tensor(out=Am[:, :, k, :], in0=fm[:, :, :], in1=T8[:, :, k:k + 1].to_broadcast([128, NT, 32]), op=ALU.is_equal), reads=[fm.r, T8.r], writes=[], accum=[Am.r])
            P.op('dve', lambda e: e.tensor_tensor(out=dd[:, :], in0=T8[:, :, 0], in1=T8[:, :, 1], op=ALU.subtract), reads=[T8.r], writes=[dd.r])
            P.op('act', lambda e: e.activation(out=s0[:, :], in_=dd[:, :], func=AF.Sigmoid), reads=[dd.r], writes=[s0.r])
            P.op('dve', lambda e: e.tensor_tensor(out=WG[:, :, 0], in0=sg[:, :], in1=s0[:, :], op=ALU.mult), reads=[sg.r, s0.r], writes=[WG.r])
            P.op('dve', lambda e: e.tensor_tensor(out=WG[:, :, 1], in0=sg[:, :], in1=WG[:, :, 0], op=ALU.subtract), reads=[sg.r, WG.r], writes=[WG.r])
            P.op('dve', lambda e: e.tensor_tensor(out=Asb[:, :, :], in0=Am[:, :, 0, :], in1=Am[:, :, 1, :], op=ALU.add), reads=[Am.r], writes=[Asb.r])
            for ti in range(NT):
                P.op('pe', lambda e, ti=ti: e.matmul(ppf[:, ti * 32:(ti + 1) * 32], lhsT=ustrict_b[:, :], rhs=Asb[:, ti, :], start=True, stop=(ti == 0)), reads=[ustrict_b.r, Asb.r], writes=[ppf.r])
                for tj in range(ti):
                    P.op('pe', lambda e, ti=ti, tj=tj: e.matmul(ppf[:, ti * 32:(ti + 1) * 32], lhsT=ones_b[:, :], rhs=Asb[:, tj, :], start=False, stop=(tj == ti - 1)), reads=[ones_b.r, Asb.r], writes=[ppf.r])
            for tj in range(NT):
                P.op('pe', lambda e, tj=tj: e.matmul(pcnt[:, 0:32], lhsT=ones_b[:, :], rhs=Asb[:, tj, :], start=(tj == 0), stop=(tj == NT - 1)), reads=[ones_b.r, Asb.r], writes=[pcnt.r])
            P.op('dve', lambda e: e.tensor_copy(out=carry[:, :], in_=pcnt[:, 0:32]), reads=[pcnt.r], writes=[carry.r])
            for k in range(2):
                P.op('dve', lambda e, k=k: e.tensor_tensor(out=tmp3[:, :, :], in0=Am[:, :, k, :], in1=ppf[:, :].rearrange("p (t e) -> p t e", e=32), op=ALU.mult), reads=[Am.r, ppf.r], writes=[tmp3.r])
                P.op('dve', lambda e, k=k: e.reduce_sum(out=RK[:, :, k], in_=tmp3[:, :, :], axis=AX.X), reads=[tmp3.r], writes=[RK.r])
            pad = sb("pad", [128, 32]); mm_ = sb("mm_", [128, 32]); pend = sb("pend", [128, 32]); thr = sb("thr", [128, NBLK])
            cmp = sb("cmp", [128, NBLK, 32]); be = sb("be", [128, NBLK]); idxf = sb("idxf", [128, NBLK])
            P.op('pool', lambda e: e.iota(thr[:, :], pattern=[[BLK, NBLK]], base=0, channel_multiplier=0, allow_small_or_imprecise_dtypes=True), writes=[thr.r])
            P.op('dve', lambda e: e.tensor_tensor(out=cmp[:, 0:32, :], in0=carry[:, :].unsqueeze(2).to_broadcast([128, 32, 32]), in1=thr[:, 0:32].unsqueeze(1).to_broadcast([128, 32, 32]), op=ALU.is_gt), reads=[carry.r, thr.r], writes=[cmp.r])
            P.op('dve', lambda e: e.reduce_sum(out=mm_[:, :], in_=cmp[:, 0:32, :], axis=AX.X), reads=[cmp.r], writes=[mm_.r])
            P.op('dve', lambda e: e.tensor_scalar(out=pad[:, :], in0=mm_[:, :], scalar1=float(BLK), scalar2=None, op0=ALU.mult), reads=[mm_.r], writes=[pad.r])
            P.op('dve', lambda e: e.tensor_tensor_scan(out=pend[:, :], data0=ones_f[:, 0:32], data1=pad[:, :], initial=0.0, op0=ALU.mult, op1=ALU.add), reads=[ones_f.r, pad.r], writes=[pend.r])
            P.op('dve', lambda e: e.tensor_tensor(out=pstart[:, :], in0=pend[:, :], in1=pad[:, :], op=ALU.subtract), reads=[pend.r, pad.r], writes=[pstart.r])
            P.op('dve', lambda e: e.tensor_tensor(out=cmp[:, :, :], in0=pend[:, :].unsqueeze(1).to_broadcast([128, NBLK, 32]), in1=thr[:, :].unsqueeze(2).to_broadcast([128, NBLK, 32]), op=ALU.is_le), reads=[pend.r, thr.r], writes=[cmp.r])
            P.op('dve', lambda e: e.reduce_sum(out=be[:, :], in_=cmp[:, :, :], axis=AX.X), reads=[cmp.r], writes=[be.r])
            P.op('dve', lambda e: e.tensor_scalar(out=be[:, :], in0=be[:, :], scalar1=31.0, scalar2=128.0, op0=ALU.min, op1=ALU.mult), reads=[be.r], writes=[be.r])
            P.op('dve', lambda e: e.tensor_scalar(out=idxf[:, :], in0=be[:, :], scalar1=iota_p[:, 0:1], scalar2=float(l * 4096), op0=ALU.add, op1=ALU.add), reads=[be.r, iota_p.r], writes=[idxf.r])
            P.op('dve', lambda e: e.tensor_copy(out=idxW[:, :], in_=idxf[:, :]), reads=[idxf.r], writes=[idxW.r])
            for k in range(2):
                P.op('dve', lambda e, k=k: e.tensor_tensor(out=tmp3[:, :, :], in0=Am[:, :, k, :], in1=pstart[:, :].unsqueeze(1).to_broadcast([128, NT, 32]), op=ALU.mult), reads=[Am.r, pstart.r], writes=[tmp3.r])
                P.op('dve', lambda e, k=k: e.reduce_sum(out=slk[:, :], in_=tmp3[:, :, :], axis=AX.X), reads=[tmp3.r], writes=[slk.r])
                P.op('dve', lambda e, k=k: e.tensor_tensor(out=slk[:, :], in0=slk[:, :], in1=RK[:, :, k], op=ALU.add), reads=[slk.r, RK.r], writes=[slk.r])
                P.op('dve', lambda e, k=k: e.tensor_copy(out=SLOT[:, k::2], in_=slk[:, :]), reads=[slk.r], writes=[], accum=[SLOT.r])
            for ti in range(NT):
                for k in range(2):
                    P.op('pool', lambda e, ti=ti, k=k: e.indirect_dma_start(out=XG.ap, out_offset=bass.IndirectOffsetOnAxis(ap=SLOT[:, ti * 2 + k:ti * 2 + k + 1], axis=0), in_=xnb[:, ti, :], in_offset=None),
                         reads=[xnb.r, SLOT.r], writes=[], dma=xnb.r, accum=[XG.r])

        phase(ph_r)

        def ph_e(sb, ps):
            w1b = [sb("w1b%d" % i, [128, 8, 512], BF16) for i in range(2)]
            w3b = [sb("w3b%d" % i, [128, 8, 512], BF16) for i in range(2)]
            w2b = [sb("w2b%d" % i, [128, 4, D], BF16) for i in range(2)]
            xb = [sb("xb%d" % i, [128, 2, D], BF16) for i in range(2)]
            XT = [sb("XT%d" % i, [128, 8, BLK], BF16) for i in range(2)]
            hid = [sb("hid%d" % i, [128, 4, BLK], BF16) for i in range(2)]
            sa = [sb("sa%d" % i, [128, BLK]) for i in range(2)]
            ysb = [sb("ysb%d" % i, [128, D]) for i in range(2)]
            pT = [ps("pT%d" % i, [128, 512], BF16) for i in range(2)]; pa = [ps("pa%d" % i) for i in range(2)]; pb = [ps("pb%d" % i) for i in range(2)]
            py = ps("py", [128, 1024])
            w1v = W1B; w3v = W3B; w2v = W2B
            yc = [0]
            for i in range(NBLK):
                b = i % 2
                for wt, wv in ((w1b[b], w1v), (w3b[b], w3v), (w2b[b], w2v)):
                    P.op('pool', lambda e, wt=wt, wv=wv, i=i: e.indirect_dma_start(out=wt[:, :, :].rearrange("p a b -> p (a b)"), out_offset=None, in_=wv.ap, in_offset=bass.IndirectOffsetOnAxis(ap=idxW[:, i:i + 1], axis=0)),
                         reads=[idxW.r, wv.r], writes=[wt.r], dma=wt.r)
                x = xb[b]
                dma('sp', x[:, :, :], XG.ap[i * BLK:(i + 1) * BLK, :].rearrange("(j p) d -> p j d", p=128), [XG.r], [x.r], x.r)
                xT = XT[b]
                for kc in range(8):
                    p = pT[kc % 2]
                    for j in range(2):
                        P.op('pe', lambda e, p=p, j=j, kc=kc, x=x: e.transpose(p[:, j * 128:(j + 1) * 128], x[:, j, kc::8], ident_b[:, :]), reads=[x.r, ident_b.r], writes=[p.r])
                    P.op('act', lambda e, p=p, kc=kc, xT=xT: e.activation(out=xT[:, kc, :], in_=p[:, 0:BLK], func=AF.Identity, scale=adap[:, l, 1, kc:kc + 1], bias=adap[:, l, 0, kc:kc + 1]),
                         reads=[p.r, adap.r], writes=[xT.r])
                h = hid[b]
                for fc in range(4):
                    A_ = pa[fc % 2]; B_ = pb[fc % 2]; s_ = sa[fc % 2]
                    for (pp, wt) in ((A_, w1b[b]), (B_, w3b[b])):
                        for kc in range(8):
                            P.op('pe', lambda e, pp=pp, wt=wt, kc=kc, fc=fc, xT=xT: e.matmul(pp[:, 0:BLK], lhsT=wt[:, kc, fc::4], rhs=xT[:, kc, :], start=(kc == 0), stop=(kc == 7)), reads=[wt.r, xT.r], writes=[pp.r])
                    P.op('act', lambda e, A_=A_, s_=s_: e.activation(out=s_[:, :], in_=A_[:, 0:BLK], func=AF.Silu), reads=[A_.r], writes=[s_.r])
                    P.op('dve', lambda e, B_=B_, s_=s_, h=h, fc=fc: e.tensor_tensor(out=h[:, fc, :], in0=s_[:, :], in1=B_[:, 0:BLK], op=ALU.mult), reads=[s_.r, B_.r], writes=[h.r])
                for j in range(2):
                    for hh in range(2):
                        for fc in range(4):
                            P.op('pe', lambda e, j=j, hh=hh, fc=fc, h=h, b=b: e.matmul(py[:, hh * 512:(hh + 1) * 512], lhsT=h[:, fc, j * 128:(j + 1) * 128], rhs=w2b[b][:, fc, hh * 512:(hh + 1) * 512], start=(fc == 0), stop=(fc == 3)),
                                 reads=[h.r, w2b[b].r], writes=[py.r])
                    y = ysb[yc[0] % 2]; yc[0] += 1
                    P.op('act', lambda e, y=y: e.copy(out=y[:, :], in_=py[:, :]), reads=[py.r], writes=[y.r])
                    dma('act', YG.ap[i * BLK + j * 128: i * BLK + (j + 1) * 128, :], y[:, :], [y.r], [], y.r, accum=[YG.r])

        phase(ph_e)

        def ph_c(sb, ps):
            lg, lb = load_bc(sb, l, 1)
            y0 = [sb("y0%d" % i, [128, D]) for i in range(NB)]; y1 = [sb("y1%d" % i, [128, D]) for i in range(NB)]
            xt = [sb("xt%d" % i, [128, D]) for i in range(NB)]; tt = [sb("tt%d" % i, [128, D]) for i in range(NB)]
            zz = [sb("zz%d" % i, [128, D]) for i in range(NB)]; oo = [sb("oo%d" % i, [128, D]) for i in range(NB)]
            lnts = [(sb("stats%d" % i, [128, 12]), sb("mv%d" % i, [128, 2]), sb("rstd%d" % i, [128, 1]), sb("nmr%d" % i, [128, 1])) for i in range(NB)]
            for it in range(NT + 3):
                if it < NT:
                    ti = it; b = ti % NB
                    for k, yy in ((0, y0[b]), (1, y1[b])):
                        P.op('pool', lambda e, yy=yy, ti=ti, k=k: e.indirect_dma_start(out=yy[:, :], out_offset=None, in_=YG.ap, in_offset=bass.IndirectOffsetOnAxis(ap=SLOT[:, ti * 2 + k:ti * 2 + k + 1], axis=0)),
                             reads=[YG.r, SLOT.r], writes=[yy.r], dma=yy.r)
                    dma('sp', xt[b][:, :], Xin.ap[ti * 128:(ti + 1) * 128, :], [Xin.r], [xt[b].r], xt[b].r)
                if 1 <= it <= NT:
                    ti = it - 1; b = ti % NB
                    t = tt[b]
                    P.op('act', lambda e, t=t, b=b, ti=ti: e.activation(out=t[:, :], in_=y0[b][:, :], func=AF.Identity, scale=WG[:, ti, 0:1]), reads=[y0[b].r, WG.r], writes=[t.r])
                    P.op('dve', lambda e, t=t, b=b, ti=ti: e.scalar_tensor_tensor(out=t[:, :], in0=y1[b][:, :], scalar=WG[:, ti, 1:2], in1=t[:, :], op0=ALU.mult, op1=ALU.add), reads=[y1[b].r, WG.r, t.r], writes=[t.r])
                    resid_A(xt[b], t, zz[b], lnts[b], l, 1)
                if 2 <= it <= NT + 1:
                    ti = it - 2; b = ti % NB
                    resid_B(zz[b], lnts[b], lg, lb, oo[b])
                if it >= 3:
                    ti = it - 3; b = ti % NB
                    dma('pool', Xout.ap[ti * 128:(ti + 1) * 128, :], oo[b][:, :], [oo[b].r], [], oo[b].r, accum=[Xout.r])

        phase(ph_c)

    moe(0, X1, X2)
    if 'm0' in dbg:
        return nc, gst

    def ph_f1(sb, ps):
        ccs = sb("ccs", [128, 2, 256]); scs = sb("scs", [128, 2, 256]); wod = sb("wod", [128, 8, D])
        M1 = sb("M1", [128, 8, D], BF16); M2 = sb("M2", [128, 8, D], BF16)
        dma('sp', ccs[:, :, :], k_cc.rearrange("(a p) n -> p a n", p=128), [], [ccs.r], ccs.r)
        dma('sp', scs[:, :, :], k_sc.rearrange("(a p) n -> p a n", p=128), [], [scs.r], scs.r)
        dma('sp', wod[:, :, :], od_w_out.rearrange("(k p) n -> p k n", p=128), [], [wod.r], wod.r)
        pm = [ps("pm%d" % i) for i in range(2)]
        pT = [ps("pT%d" % i) for i in range(2)]
        pU = ps("pU", [128, 1024]); pV = ps("pV", [128, 1024])
        mc = [0]
        for which, (cs, Mx) in enumerate(((ccs, M1), (scs, M2))):
            for g in range(4):
                for cch in range(2):
                    for nh in range(2):
                        p = pm[mc[0] % 2]; mc[0] += 1
                        for c2 in range(2):
                            P.op('pe', lambda e, p=p, cs=cs, c2=c2, cch=cch, g=g, nh=nh: e.matmul(p[:, :], lhsT=cs[:, c2, cch * 128:(cch + 1) * 128], rhs=wod[:, g * 2 + c2, nh * 512:(nh + 1) * 512], start=(c2 == 0), stop=(c2 == 1)),
                                 reads=[cs.r, wod.r], writes=[p.r])
                        P.op('act', lambda e, p=p, Mx=Mx, g=g, cch=cch, nh=nh, which=which: e.activation(out=Mx[:, g * 2 + cch, nh * 512:(nh + 1) * 512], in_=p[:, :], func=AF.Identity, scale=(1.0 if which == 0 else -1.0)),
                             reads=[p.r], writes=[Mx.r])
        xt = [sb("xt%d" % i, [128, D]) for i in range(NB)]; xn = [sb("xn%d" % i, [128, D]) for i in range(NB)]
        hT = [sb("hT%d" % i, [128, 8, 128], BF16) for i in range(NB)]
        ou = [sb("ou%d" % i, [128, D], BF16) for i in range(NB)]; ov = [sb("ov%d" % i, [128, D], BF16) for i in range(NB)]
        lnts = [(sb("stats%d" % i, [128, 12]), sb("mv%d" % i, [128, 2]), sb("rstd%d" % i, [128, 1]), sb("nmr%d" % i, [128, 1])) for i in range(NB)]
        for ti in range(NT):
            conv_issue(1, 1)
            x = xt[ti % NB]; n = xn[ti % NB]; h = hT[ti % NB]; lnt = lnts[ti % NB]
            dma('sp', x[:, :], X2.ap[ti * 128:(ti + 1) * 128, :], [X2.r], [x.r], x.r)
            ln_stats(x, lnt[0], lnt[1], lnt[2], lnt[3], [x.r])
            P.op('act', lambda e, x=x, n=n, lnt=lnt: e.activation(out=n[:, :], in_=x[:, :], func=AF.Identity, scale=lnt[2][:, 0:1], bias=lnt[3][:, 0:1]), reads=[x.r, lnt[2].r, lnt[3].r], writes=[n.r])
            for kc in range(8):
                p = pT[kc // 4]
                P.op('pe', lambda e, p=p, kc=kc, n=n: e.transpose(p[:, (kc % 4) * 128:(kc % 4 + 1) * 128], n[:, kc * 128:(kc + 1) * 128], ident[:, :]), reads=[n.r, ident.r], writes=[p.r])
            for kc in range(8):
                p = pT[kc // 4]
                P.op('act', lambda e, p=p, kc=kc, h=h: e.activation(out=h[:, kc, :], in_=p[:, (kc % 4) * 128:(kc % 4 + 1) * 128], func=AF.Identity, scale=adac[:, 1, 1, kc:kc + 1], bias=adac[:, 1, 0, kc:kc + 1]),
                     reads=[p.r, adac.r], writes=[h.r])
            for (pp, Mx, oo_, DD) in ((pU, M1, ou[ti % NB], UU), (pV, M2, ov[ti % NB], VW)):
                for nh in range(2):
                    for kc in range(8):
                        P.op('pe', lambda e, pp=pp, Mx=Mx, nh=nh, kc=kc, h=h: e.matmul(pp[:, nh * 512:(nh + 1) * 512], lhsT=h[:, kc, :], rhs=Mx[:, kc, nh * 512:(nh + 1) * 512], start=(kc == 0), stop=(kc == 7)),
                             reads=[h.r, Mx.r], writes=[pp.r])
                P.op('act', lambda e, pp=pp, oo_=oo_: e.copy(out=oo_[:, :], in_=pp[:, :]), reads=[pp.r], writes=[oo_.r])
                dma('act', DD.ap[ti * 128:(ti + 1) * 128, :], oo_[:, :], [oo_.r], [], oo_.r, accum=[DD.r])

    phase(ph_f1)

    def ph_f2(sb, ps):
        Uh = sb("Uh", [128, NT, 512], BF16); Vh = sb("Vh", [128, NT, 512], BF16)
        cn = [sb("cn%d" % i, [128, NT * 128], BF16) for i in range(2)]; sn = [sb("sn%d" % i, [128, NT * 128], BF16) for i in range(2)]
        yo = [sb("yo%d" % i, [128, 512]) for i in range(2)]
        py = [ps("py%d" % i) for i in range(2)]
        it = 0
        for nh in range(2):
            dma('sp', Uh[:, :, :], UU.ap[:, nh * 512:(nh + 1) * 512].rearrange("(t p) n -> p t n", p=128), [UU.r], [Uh.r], Uh.r)
            dma('sp', Vh[:, :, :], VW.ap[:, nh * 512:(nh + 1) * 512].rearrange("(t p) n -> p t n", p=128), [VW.r], [Vh.r], Vh.r)
            for kt in range(NT):
                c = cn[it % 2]; s_ = sn[it % 2]; y = yo[it % 2]; p = py[it % 2]; it += 1
                dma('sp', c[:, :], k_cn[kt], [], [c.r], c.r)
                dma('sp', s_[:, :], k_sn[kt], [], [s_.r], s_.r)
                for tt in range(NT):
                    P.op('pe', lambda e, p=p, c=c, tt=tt: e.matmul(p[:, :], lhsT=c[:, tt * 128:(tt + 1) * 128], rhs=Uh[:, tt, :], start=(tt == 0), stop=False), reads=[c.r, Uh.r], writes=[p.r])
                    P.op('pe', lambda e, p=p, s_=s_, tt=tt: e.matmul(p[:, :], lhsT=s_[:, tt * 128:(tt + 1) * 128], rhs=Vh[:, tt, :], start=False, stop=(tt == NT - 1)), reads=[s_.r, Vh.r], writes=[p.r])
                P.op('act', lambda e, p=p, y=y: e.copy(out=y[:, :], in_=p[:, :]), reads=[p.r], writes=[y.r])
                dma('act', Y1.ap[kt * 128:(kt + 1) * 128, nh * 512:(nh + 1) * 512], y[:, :], [y.r], [], y.r, accum=[Y1.r])

    phase(ph_f2)

    def ph_f3(sb, ps):
        lg, lb = load_bc(sb, 1, 0)
        bb = sb("bb", [128, D])
        dma('sp', bb[:, :], od_b_out.partition_broadcast(128), [], [bb.r], bb.r)
        xt = [sb("xt%d" % i, [128, D]) for i in range(NB)]; tt = [sb("tt%d" % i, [128, D]) for i in range(NB)]
        zz = [sb("zz%d" % i, [128, D]) for i in range(NB)]; oo = [sb("oo%d" % i, [128, D]) for i in range(NB)]
        lnts = [(sb("stats%d" % i, [128, 12]), sb("mv%d" % i, [128, 2]), sb("rstd%d" % i, [128, 1]), sb("nmr%d" % i, [128, 1])) for i in range(NB)]
        for it in range(NT + 2):
            if it < NT:
                ti = it
                x = xt[ti % NB]; t = tt[ti % NB]; z = zz[ti % NB]; lnt = lnts[ti % NB]
                dma('sp', x[:, :], X2.ap[ti * 128:(ti + 1) * 128, :], [X2.r], [x.r], x.r)
                dma('sp', t[:, :], Y1.ap[ti * 128:(ti + 1) * 128, :], [Y1.r], [t.r], t.r)
                P.op('pool', lambda e, t=t: e.tensor_tensor(out=t[:, :], in0=t[:, :], in1=bb[:, :], op=ALU.add), reads=[t.r, bb.r], writes=[t.r])
                resid_A(x, t, z, lnt, 1, 0)
            if 1 <= it <= NT:
                ti = it - 1
                resid_B(zz[ti % NB], lnts[ti % NB], lg, lb, oo[ti % NB])
            if it >= 2:
                ti = it - 2
                dma('pool', X3.ap[ti * 128:(ti + 1) * 128, :], oo[ti % NB][:, :], [oo[ti % NB].r], [], oo[ti % NB].r, accum=[X3.r])

    phase(ph_f3)
    if 'f3' in dbg:
        return nc, gst
    moe(1, X3, out_d)
    return nc, gst


def _consts():
    t = np.arange(SEQ)
    row = (t // 64).astype(np.float32); col = (t % 64).astype(np.float32)
    nf = 16
    freqs = (10000.0 ** (-np.arange(nf, dtype=np.float32) / nf)).astype(np.float32)
    ropec = np.zeros((128, SEQ), np.float32); ropes = np.zeros((128, SEQ), np.float32)
    for i in range(2):
        for d in range(64):
            pos = row if d < 32 else col
            ang = (pos * freqs[d % 16]).astype(np.float32)
            ropec[i * 64 + d] = np.cos(ang)
            sgn = -1.0 if (d % 32) < 16 else 1.0
            ropes[i * 64 + d] = sgn * np.sin(ang)
    k = np.arange(256)
    ang = 2 * np.pi * np.outer(k, k) / 256.0
    cc = (np.cos(ang) / 16.0).astype(np.float32); sc = (np.sin(ang) / 16.0).astype(np.float32)
    n = np.arange(SEQ)
    kt = (np.outer(n, n) % SEQ).astype(np.float64) * (2 * np.pi / SEQ)
    cn = (np.cos(kt) / 64.0); sn = (np.sin(kt) / 64.0)

    def lay(m):
        m4 = m.reshape(32, 128, 32, 128)
        return np.ascontiguousarray(m4.transpose(2, 1, 0, 3)).reshape(32, 128, 32 * 128).astype(ml_dtypes.bfloat16)
    return dict(k_ropec=ropec, k_ropes=ropes, k_cc=cc, k_sc=sc, k_cn=lay(cn), k_sn=lay(sn))


_CACHE = {}


def kernel(**inputs):
    dbg = tuple(os.environ.get("KDBG", "").split(",")) if os.environ.get("KDBG") else ()
    nc, gst = build(dbg)
    gst.close()
    if 'consts' not in _CACHE:
        _CACHE['consts'] = _consts()
    cst = _CACHE['consts']
    f = lambda a: np.ascontiguousarray(np.asarray(a, dtype=np.float32))
    shared = {k: f(inputs[k]) for k in ['c_ctx', 'ada_w', 'ada_b', 'ln_g', 'ln_b', 'od_b_out', 'moe_wg', 'moe_bg', 'moe_wf', 'moe_bf', 'moe_w1', 'moe_w3', 'moe_w2']}
    for k in ['ev_w_in', 'ev_conv_w', 'ev_conv_b', 'ev_gate_a_w', 'ev_gate_a_b', 'ev_gate_x_w', 'ev_gate_x_b', 'ev_lru_lambda', 'ev_da_lambda', 'ev_da_subln', 'ev_w_out', 'od_w_out']:
        shared[k] = f(inputs[k])[0]
    shared.update(cst)
    x = f(inputs['x']); c = f(inputs['c']); ctx = f(inputs['ctx'])
    in_maps = []
    for b in range(8):
        m = dict(shared)
        m['x'] = x[b]; m['c'] = c[b]; m['ctx'] = ctx[b]
        in_maps.append(m)
    ncores = int(os.environ.get('KCORES', '8'))
    res = run_bass_kernel_spmd(nc, in_maps[:ncores], core_ids=list(range(ncores)))
    if dbg:
        return res
    return np.stack([np.asarray(r['out'], dtype=np.float32) for r in res.results], axis=0)
```

```python
import contextlib
import math
import os
import numpy as np
import ml_dtypes
import concourse.bass as bass
import concourse.mybir as mybir
from concourse.bass_utils import run_bass_kernel_spmd

F32 = mybir.dt.float32
BF16 = mybir.dt.bfloat16
I32 = mybir.dt.int32
AF = mybir.ActivationFunctionType
ALU = mybir.AluOpType
AX = mybir.AxisListType
ENGS = ['pe', 'act', 'dve', 'pool', 'sp']
NPOOL = 86
NBG = 4

D = 1024
SEQ = 4096
NCTX = 256
NTOK = SEQ + NCTX
ALPHA = (2.0 * 2) ** 0.25
EPS = 1e-6
BLK = 256
NBLK = 2 * SEQ // BLK + 32
NSLOT = NBLK * BLK
NB = 5
NT = SEQ // 128


class Res:
    __slots__ = ('name', 'writers', 'readers')

    def __init__(self, name):
        self.name = name
        self.writers = {}
        self.readers = {}


class Prog:
    def __init__(self, nc, st):
        self.nc = nc
        self.esem = {e: st.enter_context(nc.semaphore('se_' + e)) for e in ENGS}
        self.bar = st.enter_context(nc.semaphore('sbar'))
        self.psem = [st.enter_context(nc.semaphore('sp%d' % i)) for i in range(NPOOL + NBG)]
        self.bg = {}
        self.cnt = {('e', e): 0 for e in ENGS}
        for i in range(NPOOL + NBG):
            self.cnt[('p', i)] = 0
        self.bar_cnt = 0
        self.nres = 0
        self._reset_phase()
        self.waited = {e: {} for e in ENGS}

    def _reset_phase(self):
        self.ops = {e: [] for e in ENGS}
        self.res_sem = {}
        self.free = list(range(NPOOL))
        self.touched = set()

    def sem(self, k):
        return self.esem[k[1]] if k[0] == 'e' else self.psem[k[1]]

    def res(self, name=None):
        self.nres += 1
        return Res('%s#%d' % (name or 'r', self.nres))

    def op(self, eng, fn, reads=(), writes=(), dma=None, accum=(), bg=False):
        waits = {}
        isdma = dma is not None

        def need(sk, tok):
            waits[sk] = max(waits.get(sk, 0), tok[0])

        for r in reads:
            for sk, tok in r.writers.items():
                need(sk, tok)
        for w in list(writes) + list(accum):
            is_acc = any(w is a for a in accum)
            if not is_acc:
                for sk, tok in w.writers.items():
                    if (not isdma) and tok[2] == 'c' and tok[1] == eng:
                        continue
                    need(sk, tok)
            for sk, tok in w.readers.items():
                if (not isdma) and tok[2] == 'c' and tok[1] == eng:
                    continue
                need(sk, tok)
        if isdma and bg:
            if dma.name not in self.bg:
                self.bg[dma.name] = NPOOL + len(self.bg)
            sk = ('p', self.bg[dma.name])
        elif isdma:
            if dma.name not in self.res_sem:
                self.res_sem[dma.name] = self.free.pop(0)
            sk = ('p', self.res_sem[dma.name])
        if isdma:
            self.cnt[sk] += 16
            tok = (self.cnt[sk], eng, 'd')
            inc = 16
        else:
            sk = ('e', eng)
            self.cnt[sk] += 1
            tok = (self.cnt[sk], eng, 'c')
            inc = 1
        if not bg:
            self.touched.add(sk)
        wl = []
        wd = self.waited[eng]
        for k, v in waits.items():
            if wd.get(k, 0) >= v:
                continue
            wd[k] = v
            wl.append((k, v))
        self.ops[eng].append((fn, wl, sk, inc))
        for r in reads:
            r.readers[sk] = tok
        for w in writes:
            if any(w is a for a in accum):
                continue
            w.writers = {sk: tok}
            w.readers = {}
        for w in accum:
            w.writers[sk] = tok
        return tok

    def flush(self):
        nc = self.nc
        self.bar_cnt += 1
        barv = self.bar_cnt
        finals = [(k, self.cnt[k]) for k in sorted(self.touched)]
        ops = self.ops
        P = self

        def replay(e, key):
            for fn, wl, sk, inc in ops[key]:
                for k, v in wl:
                    e.wait_ge(P.sem(k), v)
                ins = fn(e)
                ins.then_inc(P.sem(sk), inc)
            if key == 'sp':
                for k, v in finals:
                    e.wait_ge(P.sem(k), v)
                e.sem_inc(P.bar, 1)
            else:
                e.wait_ge(P.bar, barv)

        with nc.Block() as block:
            @block.tensor
            def _(e):
                replay(e, 'pe')

            @block.scalar
            def _(e):
                replay(e, 'act')

            @block.vector
            def _(e):
                replay(e, 'dve')

            @block.gpsimd
            def _(e):
                replay(e, 'pool')

            @block.sync
            def _(e):
                replay(e, 'sp')
        bgk = set(('p', i) for i in self.bg.values())
        for e in ENGS:
            for k in self.cnt:
                if k in bgk:
                    continue
                self.waited[e][k] = self.cnt[k]
        self._reset_phase()


class T:
    def __init__(self, P, t, name):
        self.t = t
        self.r = P.res(name)

    def __getitem__(self, k):
        return self.t[k]


class Ctx:
    pass


def build(dbg=()):
    nc = bass.Bass("TRN2", target_bir_lowering=False)
    gst = contextlib.ExitStack()
    P = Prog(nc, gst)

    def din(name, shape, dt=F32):
        return nc.dram_tensor(name, list(shape), dt, kind="ExternalInput").ap()

    class DR:
        def __init__(self, name, shape, dt=F32, out=False):
            kind = "ExternalOutput" if (out or name in dbg) else "Internal"
            self.ap = nc.dram_tensor(name, list(shape), dt, kind=kind).ap()
            self.r = P.res(name)

    x_in = din("x", [SEQ, D]); c_in = din("c", [D]); ctx_in = din("ctx", [NCTX, D]); cctx_in = din("c_ctx", [D])
    ada_w = din("ada_w", [2, D, 6 * D]); ada_b = din("ada_b", [2, 6 * D])
    ln_g = din("ln_g", [2, 2, D]); ln_b = din("ln_b", [2, 2, D])
    w_in = din("ev_w_in", [D, 2560]); conv_w = din("ev_conv_w", [4, 512]); conv_b = din("ev_conv_b", [512])
    ga_w = din("ev_gate_a_w", [2, 8, 64, 64]); ga_b = din("ev_gate_a_b", [2, 512])
    gx_w = din("ev_gate_x_w", [2, 8, 64, 64]); gx_b = din("ev_gate_x_b", [2, 512])
    lru_lam = din("ev_lru_lambda", [2, 512]); da_lam = din("ev_da_lambda", [4, 64]); da_sub = din("ev_da_subln", [128])
    ev_w_out = din("ev_w_out", [D, D]); od_w_out = din("od_w_out", [D, D]); od_b_out = din("od_b_out", [D])
    moe_wg = din("moe_wg", [2, D, 4]); moe_bg = din("moe_bg", [2, 4]); moe_wf = din("moe_wf", [2, D, 32]); moe_bf = din("moe_bf", [2, 32])
    moe_w1 = din("moe_w1", [2, 32, D, 512]); moe_w3 = din("moe_w3", [2, 32, D, 512]); moe_w2 = din("moe_w2", [2, 32, 512, D])
    ropec = din("k_ropec", [128, SEQ]); ropes = din("k_ropes", [128, SEQ])
    k_cc = din("k_cc", [256, 256]); k_sc = din("k_sc", [256, 256])
    k_cn = din("k_cn", [32, 128, 32 * 128], BF16); k_sn = din("k_sn", [32, 128, 32 * 128], BF16)
    out_d = DR("out", [SEQ, D], F32, out=True)

    GG = DR("GG", [512, SEQ], BF16); XR = DR("XR", [512, NTOK]); QT = DR("QT", [512, SEQ], BF16)
    KT = DR("KT", [512, NTOK], BF16); VV = DR("VV", [NTOK, 512], BF16); MT = DR("MT", [D, SEQ], BF16)
    X1 = DR("X1", [SEQ, D]); X2 = DR("X2", [SEQ, D]); X3 = DR("X3", [SEQ, D])
    XG = DR("XG", [NSLOT, D], BF16); YG = DR("YG", [NSLOT, D])
    W1B = DR("W1B", [2 * 32 * 128, 4096], BF16); W3B = DR("W3B", [2 * 32 * 128, 4096], BF16); W2B = DR("W2B", [2 * 32 * 128, 4096], BF16)
    UU = DR("UU", [SEQ, D], BF16); VW = DR("VW", [SEQ, D], BF16); Y1 = DR("Y1", [SEQ, D])
    ADAS = DR("ADAS", [2, 4, D])

    def sbp(name, shape, dt=F32):
        return T(P, gst.enter_context(nc.sbuf_tensor(name, list(shape), dt)), name)

    ident = sbp("ident", [128, 128]); ones_f = sbp("ones_f", [128, 128]); ones_b = sbp("ones_b", [128, 128], BF16)
    ustrict = sbp("ustrict", [128, 128]); ustrict_b = sbp("ustrict_b", [128, 128], BF16); ident_b = sbp("ident_b", [128, 128], BF16)
    iota_p = sbp("iota_p", [128, 1])
    adac = sbp("adac", [128, 2, 4, 8])
    adap = sbp("adap", [128, 2, 2, 8])
    adacx = sbp("adacx", [128, 2, 8])
    gbc = sbp("gbc", [128, 2, 2, D])
    lamc = sbp("lamc", [128, 2])
    eps_c = sbp("eps_c", [128, 1])

    K = Ctx()

    def dma(q, out, in_, reads, writes, key, accum=(), slow=False):
        P.op(q, lambda e: e.dma_start(out=out, in_=in_, allow_slow_non_contiguous=slow), reads=reads, writes=writes, dma=key, accum=accum)

    conv_res = [P.res("conv0"), P.res("conv1")]
    w1v_ = moe_w1.rearrange("l e (p r) n -> (l e p) (r n)", p=128)
    w3v_ = moe_w3.rearrange("l e (p r) n -> (l e p) (r n)", p=128)
    w2v_ = moe_w2.rearrange("l e (p r) n -> (l e p) (r n)", p=128)
    conv_list = []
    for l_ in range(2):
        for g in range(8):
            for (src, dst) in ((w1v_, W1B), (w3v_, W3B), (w2v_, W2B)):
                conv_list.append((l_, g, src, dst))
    conv_pos = [0, 0]

    def conv_issue(l_, n):
        for _ in range(n):
            items = [c for c in conv_list if c[0] == l_]
            if conv_pos[l_] >= len(items):
                return
            (_, g, src, dst) = items[conv_pos[l_]]; conv_pos[l_] += 1
            r0 = l_ * 4096 + g * 512
            P.op('pool', lambda e, src=src, dst=dst, r0=r0: e.dma_start(out=dst.ap[r0:r0 + 512, :], in_=src[r0:r0 + 512, :]), reads=[], writes=[], dma=conv_res[l_], accum=[dst.r], bg=True)

    phno = [0]

    def phase(fn):
        phno[0] += 1
        pfx = "p%d_" % phno[0]
        with contextlib.ExitStack() as st:
            def sb(name, shape, dt=F32):
                return T(P, st.enter_context(nc.sbuf_tensor(pfx + name, list(shape), dt)), name)

            def ps(name, shape=(128, 512), dt=F32):
                return T(P, st.enter_context(nc.psum_tensor(pfx + name, list(shape), dt)), name)
            fn(sb, ps)
            P.flush()

    def ln_stats(xt_ap, stats, mv, rstd, nmr, reads):
        P.op('dve', lambda e: e.bn_stats(out=stats[:, 0:6], in_=xt_ap[:, 0:512]), reads=reads, writes=[stats.r])
        P.op('dve', lambda e: e.bn_stats(out=stats[:, 6:12], in_=xt_ap[:, 512:1024]), reads=reads, writes=[stats.r])
        P.op('dve', lambda e: e.bn_aggr(out=mv[:, :], in_=stats[:, :]), reads=[stats.r], writes=[mv.r])
        P.op('act', lambda e: e.activation(out=rstd[:, :], in_=mv[:, 1:2], func=AF.Sqrt, bias=eps_c[:, 0:1], scale=1.0), reads=[mv.r, eps_c.r], writes=[rstd.r])
        P.op('dve', lambda e: e.reciprocal(out=rstd[:, :], in_=rstd[:, :]), reads=[rstd.r], writes=[rstd.r])
        P.op('dve', lambda e: e.scalar_tensor_tensor(out=nmr[:, :], in0=mv[:, 0:1], scalar=-1.0, in1=rstd[:, :], op0=ALU.mult, op1=ALU.mult), reads=[mv.r, rstd.r], writes=[nmr.r])

    def ph_prologue(sb, ps):
        P.op('pool', lambda e: e.memset(ident[:, :], 0.0), writes=[ident.r])
        P.op('pool', lambda e: e.affine_select(out=ident[:, :], in_=ident[:, :], pattern=[[-1, 128]], compare_op=ALU.not_equal, fill=1.0, base=0, channel_multiplier=1), reads=[ident.r], writes=[ident.r])
        P.op('pool', lambda e: e.memset(ones_f[:, :], 1.0), writes=[ones_f.r])
        P.op('pool', lambda e: e.memset(ones_b[:, :], 1.0), writes=[ones_b.r])
        P.op('pool', lambda e: e.memset(eps_c[:, :], EPS), writes=[eps_c.r])
        P.op('pool', lambda e: e.memset(ustrict[:, :], 1.0), writes=[ustrict.r])
        P.op('pool', lambda e: e.affine_select(out=ustrict[:, :], in_=ustrict[:, :], pattern=[[1, 128]], compare_op=ALU.is_gt, fill=0.0, base=0, channel_multiplier=-1), reads=[ustrict.r], writes=[ustrict.r])
        P.op('pool', lambda e: e.tensor_copy(out=ustrict_b[:, :], in_=ustrict[:, :]), reads=[ustrict.r], writes=[ustrict_b.r])
        P.op('pool', lambda e: e.tensor_copy(out=ident_b[:, :], in_=ident[:, :]), reads=[ident.r], writes=[ident_b.r])
        P.op('pool', lambda e: e.iota(iota_p[:, :], pattern=[[0, 1]], base=0, channel_multiplier=1, allow_small_or_imprecise_dtypes=True), writes=[iota_p.r])
        cc = sb("cc", [128, 2, 8]); sc = sb("sc", [128, 8, 2]); srep = sb("srep", [128, 8, 128])
        dma('sp', cc[:, 0, :], c_in.rearrange("(k p) -> p k", p=128), [], [cc.r], cc.r, slow=True)
        dma('sp', cc[:, 1, :], cctx_in.rearrange("(k p) -> p k", p=128), [], [cc.r], cc.r, slow=True)
        P.op('act', lambda e: e.activation(out=sc[:, :, :].rearrange("p k t -> p t k"), in_=cc[:, :, :], func=AF.Silu), reads=[cc.r], writes=[sc.r])
        for kc in range(8):
            P.op('dve', lambda e, kc=kc: e.tensor_scalar(out=srep[:, kc, :], in0=ones_f[:, :], scalar1=sc[:, kc, 0:1], scalar2=None, op0=ALU.mult), reads=[sc.r, ones_f.r], writes=[srep.r])
        wsl = [sb("wsl%d" % i, [128, 8, D]) for i in range(2)]
        badd = sb("badd", [128, 2, 6, 8]); bbc = sb("bbc", [128, D])
        dma('sp', badd[:, :, :, :], ada_b.rearrange("l (t n p) -> p l t n", p=128, n=8), [], [badd.r], badd.r, slow=True)
        pc = ps("pc", [128, 512]); pb = [ps("pb%d" % i, [128, 512]) for i in range(2)]
        colmap = {0: 0, 1: 1, 3: 2, 4: 3}
        i = 0
        for l in range(2):
            for term in range(6):
                w = wsl[i % 2]; i += 1
                dma('sp', w[:, :, :], ada_w[l, :, term * D:(term + 1) * D].rearrange("(k p) n -> p k n", p=128), [], [w.r], w.r)
                if term in colmap:
                    ci = colmap[term]
                    for n in range(8):
                        for kc in range(8):
                            P.op('pe', lambda e, w=w, n=n, kc=kc: e.matmul(pc[:, n * 2:n * 2 + 2], lhsT=w[:, kc, n * 128:(n + 1) * 128], rhs=sc[:, kc, :], start=(kc == 0), stop=(kc == 7)),
                                 reads=[w.r, sc.r], writes=[pc.r])
                    addc = 1.0 if term in (1, 4) else 0.0
                    P.op('dve', lambda e, l=l, term=term, ci=ci, addc=addc: e.scalar_tensor_tensor(out=adac[:, l, ci, :], in0=pc[:, 0:16:2], scalar=addc, in1=badd[:, l, term, :], op0=ALU.add, op1=ALU.add),
                         reads=[pc.r, badd.r], writes=[adac.r])
                    if l == 0 and term in (0, 1):
                        P.op('dve', lambda e, term=term, addc=addc: e.scalar_tensor_tensor(out=adacx[:, term, :], in0=pc[:, 1:16:2], scalar=addc, in1=badd[:, 0, term, :], op0=ALU.add, op1=ALU.add),
                             reads=[pc.r, badd.r], writes=[adacx.r])
                else:
                    gi = 0 if term == 2 else 1
                    dma('sp', bbc[:, :], ada_b[l, term * D:(term + 1) * D].partition_broadcast(128), [], [bbc.r], bbc.r)
                    for h in range(2):
                        for kc in range(8):
                            P.op('pe', lambda e, w=w, h=h, kc=kc: e.matmul(pb[h][:, :], lhsT=srep[:, kc, :], rhs=w[:, kc, h * 512:(h + 1) * 512], start=(kc == 0), stop=(kc == 7)),
                                 reads=[w.r, srep.r], writes=[pb[h].r])
                        P.op('dve', lambda e, l=l, gi=gi, h=h: e.tensor_tensor(out=gbc[:, l, gi, h * 512:(h + 1) * 512], in0=pb[h][:, :], in1=bbc[:, h * 512:(h + 1) * 512], op=ALU.add),
                             reads=[pb[h].r, bbc.r], writes=[gbc.r])
        for l in range(2):
            dma('sp', ADAS.ap[l].rearrange("t (n p) -> p t n", p=128), adac[:, l, :, :], [adac.r], [], adac.r, accum=[ADAS.r], slow=True)
        for l in range(2):
            dma('sp', adap[:, l, :, :], ADAS.ap[l, 2:4, :].rearrange("t (p n) -> p t n", n=8), [ADAS.r], [adap.r], adap.r, slow=True)
        dl = sb("dl", [1, 4, 64]); dp = sb("dp", [1, 2, 64]); dsum = sb("dsum", [1, 2]); lv = sb("lv", [1, 2])
        dma('sp', dl[:, :, :], da_lam.rearrange("(o a) d -> o a d", o=1), [], [dl.r], dl.r)
        P.op('dve', lambda e: e.tensor_tensor(out=dp[:, :, :], in0=dl[:, 0:4:2, :], in1=dl[:, 1:4:2, :], op=ALU.mult), reads=[dl.r], writes=[dp.r])
        P.op('dve', lambda e: e.reduce_sum(out=dsum[:, :], in_=dp[:, :, :], axis=AX.X), reads=[dp.r], writes=[dsum.r])
        P.op('act', lambda e: e.activation(out=dsum[:, :], in_=dsum[:, :], func=AF.Exp), reads=[dsum.r], writes=[dsum.r])
        P.op('dve', lambda e: e.scalar_tensor_tensor(out=lv[:, 1:2], in0=dsum[:, 0:1], scalar=0.2, in1=dsum[:, 1:2], op0=ALU.add, op1=ALU.subtract), reads=[dsum.r], writes=[lv.r])
        P.op('dve', lambda e: e.tensor_scalar(out=lv[:, 0:1], in0=lv[:, 1:2], scalar1=-1.0, scalar2=None, op0=ALU.mult), reads=[lv.r], writes=[lv.r])
        P.op('pe', lambda e: e.matmul(pc[:, 32:34], lhsT=ones_f[0:1, :], rhs=lv[:, :], start=True, stop=True), reads=[ones_f.r, lv.r], writes=[pc.r])
        P.op('dve', lambda e: e.tensor_copy(out=lamc[:, :], in_=pc[:, 32:34]), reads=[pc.r], writes=[lamc.r])

    phase(ph_prologue)

    def ph_s1(sb, ps):
        wb = sb("wb", [128, 8, 2560], BF16); wp = sb("wp", [128, 8, 1024], BF16)
        wst = [sb("wst%d" % i, [128, 8, 512]) for i in range(2)]
        for ch in range(5):
            w = wst[ch % 2]
            dma('sp', w[:, :, :], w_in[:, ch * 512:(ch + 1) * 512].rearrange("(k p) n -> p k n", p=128), [], [w.r], w.r)
            P.op('pool', lambda e, w=w, ch=ch: e.tensor_copy(out=wb[:, :, ch * 512:(ch + 1) * 512], in_=w[:, :, :]), reads=[w.r], writes=[wb.r])
        for kc in range(8):
            src = wb[:, kc, 1024:2048].rearrange("p (b h f) -> p b h f", h=2, f=16)
            dst = wp[:, kc, :].rearrange("p (b h f) -> p b h f", h=2, f=16)
            P.op('pool', lambda e, src=src, dst=dst: e.tensor_copy(out=dst[:, :, 0, :], in_=src[:, :, 1, :]), reads=[wb.r], writes=[wp.r])
            P.op('pool', lambda e, src=src, dst=dst: e.tensor_copy(out=dst[:, :, 1, :], in_=src[:, :, 0, :]), reads=[wb.r], writes=[wp.r])
        xt = [sb("xt%d" % i, [128, D]) for i in range(3)]
        xn = sb("xn", [128, 4, D])
        hT = [sb("hT%d" % i, [128, 8, 512], BF16) for i in range(2)]
        rc = [sb("rc%d" % i, [128, 512]) for i in range(2)]; rs = [sb("rs%d" % i, [128, 512]) for i in range(2)]
        stats = sb("stats", [128, 12]); mv = sb("mv", [128, 2]); rstd = sb("rstd", [128, 1]); nmr = sb("nmr", [128, 1])
        ogg = [sb("ogg%d" % i, [128, 512], BF16) for i in range(2)]
        oxr = [sb("oxr%d" % i, [128, 512]) for i in range(2)]
        oqk = [sb("oqk%d" % i, [128, 512], BF16) for i in range(2)]
        t1 = sb("t1", [128, 512]); t2 = sb("t2", [128, 512])
        ov = [sb("ov%d" % i, [128, 512], BF16) for i in range(2)]
        pT = [ps("pT%d" % i) for i in range(2)]
        pM = [ps("pM%d" % i) for i in range(4)]
        cnt = {'x': 0, 'm': 0, 'gg': 0, 'xr': 0, 'qk': 0, 'v': 0}

        def nxt(k, n):
            v = cnt[k]; cnt[k] += 1
            return v % n

        for s in range(9):
            isctx = (s == 0)
            ntile = 2 if isctx else 4
            ntk = ntile * 128
            tok0 = 0 if isctx else NCTX + (s - 1) * 512
            lat0 = (s - 1) * 512
            h = hT[s % 2]
            for j in range(ntile):
                x = xt[nxt('x', 3)]
                src = ctx_in[j * 128:(j + 1) * 128, :] if isctx else x_in[lat0 + j * 128: lat0 + (j + 1) * 128, :]
                dma('sp', x[:, :], src, [], [x.r], x.r)
                ln_stats(x, stats, mv, rstd, nmr, [x.r])
                P.op('act', lambda e, x=x, j=j: e.activation(out=xn[:, j, :], in_=x[:, :], func=AF.Identity, scale=rstd[:, 0:1], bias=nmr[:, 0:1]), reads=[x.r, rstd.r, nmr.r], writes=[xn.r])
            for kc in range(8):
                p = pT[kc % 2]
                for j in range(ntile):
                    P.op('pe', lambda e, p=p, j=j, kc=kc: e.transpose(p[:, j * 128:(j + 1) * 128], xn[:, j, kc * 128:(kc + 1) * 128], ident[:, :]), reads=[xn.r, ident.r], writes=[p.r])
                scl = adacx[:, 1, kc:kc + 1] if isctx else adac[:, 0, 1, kc:kc + 1]
                bia = adacx[:, 0, kc:kc + 1] if isctx else adac[:, 0, 0, kc:kc + 1]
                P.op('act', lambda e, p=p, kc=kc, scl=scl, bia=bia, h=h, ntk=ntk: e.activation(out=h[:, kc, 0:ntk], in_=p[:, 0:ntk], func=AF.Identity, scale=scl, bias=bia),
                     reads=[p.r, adac.r, adacx.r], writes=[h.r])
            if not isctx:
                rcb = rc[s % 2]; rsb = rs[s % 2]
                dma('sp', rcb[:, :], ropec[:, lat0:lat0 + 512], [], [rcb.r], rcb.r)
                dma('sp', rsb[:, :], ropes[:, lat0:lat0 + 512], [], [rsb.r], rsb.r)

            def mm_feat(pm, wt, c0, h=h, ntk=ntk):
                for kc in range(8):
                    P.op('pe', lambda e, kc=kc: e.matmul(pm[:, 0:ntk], lhsT=wt[:, kc, c0:c0 + 128], rhs=h[:, kc, 0:ntk], start=(kc == 0), stop=(kc == 7)), reads=[wt.r, h.r], writes=[pm.r])

            if not isctx:
                for c4 in range(4):
                    pm = pM[nxt('m', 4)]
                    mm_feat(pm, wb, c4 * 128)
                    o = ogg[nxt('gg', 2)]
                    P.op('act', lambda e, pm=pm, o=o: e.activation(out=o[:, :], in_=pm[:, :], func=AF.Gelu_apprx_tanh), reads=[pm.r], writes=[o.r])
                    dma('act', GG.ap[c4 * 128:(c4 + 1) * 128, lat0:lat0 + 512], o[:, :], [o.r], [], o.r, accum=[GG.r])
            for c4 in range(4):
                pm = pM[nxt('m', 4)]
                mm_feat(pm, wb, 512 + c4 * 128)
                o = oxr[nxt('xr', 2)]
                P.op('act', lambda e, pm=pm, o=o, ntk=ntk: e.copy(out=o[:, 0:ntk], in_=pm[:, 0:ntk]), reads=[pm.r], writes=[o.r])
                dma('act', XR.ap[c4 * 128:(c4 + 1) * 128, tok0:tok0 + ntk], o[:, 0:ntk], [o.r], [], o.r, accum=[XR.r])
            for which in range(2):
                if which == 0 and isctx:
                    continue
                for hd in range(4):
                    c0 = 1024 + which * 512 + hd * 128
                    pm = pM[nxt('m', 4)]
                    mm_feat(pm, wb, c0)
                    o = oqk[nxt('qk', 2)]
                    if isctx:
                        P.op('dve', lambda e, pm=pm, o=o, ntk=ntk: e.tensor_copy(out=o[:, 0:ntk], in_=pm[:, 0:ntk]), reads=[pm.r], writes=[o.r])
                    else:
                        pm2 = pM[nxt('m', 4)]
                        mm_feat(pm2, wp, which * 512 + hd * 128)
                        P.op('dve', lambda e, pm=pm, rcb=rcb: e.tensor_tensor(out=t1[:, :], in0=pm[:, :], in1=rcb[:, :], op=ALU.mult), reads=[pm.r, rcb.r], writes=[t1.r])
                        P.op('dve', lambda e, pm2=pm2, rsb=rsb: e.tensor_tensor(out=t2[:, :], in0=pm2[:, :], in1=rsb[:, :], op=ALU.mult), reads=[pm2.r, rsb.r], writes=[t2.r])
                        P.op('pool', lambda e, o=o: e.tensor_tensor(out=o[:, :], in0=t1[:, :], in1=t2[:, :], op=ALU.add), reads=[t1.r, t2.r], writes=[o.r])
                    if which == 0:
                        dma('pool', QT.ap[hd * 128:(hd + 1) * 128, lat0:lat0 + 512], o[:, :], [o.r], [], o.r, accum=[QT.r])
                    else:
                        dma('pool', KT.ap[hd * 128:(hd + 1) * 128, tok0:tok0 + ntk], o[:, 0:ntk], [o.r], [], o.r, accum=[KT.r])
            for j in range(ntile):
                pm = pM[nxt('m', 4)]
                for kc in range(8):
                    P.op('pe', lambda e, pm=pm, kc=kc, j=j, h=h: e.matmul(pm[:, :], lhsT=h[:, kc, j * 128:(j + 1) * 128], rhs=wb[:, kc, 2048:2560], start=(kc == 0), stop=(kc == 7)), reads=[wb.r, h.r], writes=[pm.r])
                o = ov[nxt('v', 2)]
                P.op('act', lambda e, pm=pm, o=o: e.copy(out=o[:, :], in_=pm[:, :]), reads=[pm.r], writes=[o.r])
                dma('act', VV.ap[tok0 + j * 128: tok0 + (j + 1) * 128, :], o[:, :], [o.r], [], o.r, accum=[VV.r])

    phase(ph_s1)
    if 's1' in dbg:
        return nc, gst

    def ph_s2(sb, ps):
        LB = NTOK + 6
        xr = sb("xr", [128, LB]); xc = sb("xc", [128, NTOK]); rr = sb("rr", [128, NTOK]); ii = sb("ii", [128, NTOK])
        a2 = sb("a2", [128, NTOK]); hf = sb("hf", [128, NTOK]); hb = sb("hb", [128, NTOK])
        xcb = sb("xcb", [128, NTOK], BF16); gg = sb("gg", [128, SEQ], BF16); yo = sb("yo", [128, SEQ], BF16)
        cw = sb("cw", [128, 4, 4]); cb = sb("cb", [128, 4]); gba = sb("gba", [128, 2, 4]); gbx = sb("gbx", [128, 2, 4])
        lam = sb("lam", [128, 2, 4]); cA = sb("cA", [128, 2, 4]); cA2 = sb("cA2", [128, 2, 4])
        wgs = sb("wgs", [128, 16, 128]); wg = sb("wg", [128, 16, 128], BF16)
        pm = [ps("pm%d" % i) for i in range(4)]
        P.op('pool', lambda e: e.memset(xr[:, :], 0.0), writes=[xr.r])
        P.op('pool', lambda e: e.memset(wgs[:, :, :], 0.0), writes=[wgs.r])
        for k in range(4):
            dma('sp', cw[:, :, k], conv_w[k].rearrange("(c p) -> p c", p=128), [], [], cw.r, accum=[cw.r], slow=True)
        dma('sp', cb[:, :], conv_b.rearrange("(c p) -> p c", p=128), [], [cb.r], cb.r, slow=True)
        dma('sp', gba[:, :, :], ga_b.rearrange("d (c p) -> p d c", p=128), [], [gba.r], gba.r, slow=True)
        dma('sp', gbx[:, :, :], gx_b.rearrange("d (c p) -> p d c", p=128), [], [gbx.r], gbx.r, slow=True)
        dma('sp', lam[:, :, :], lru_lam.rearrange("d (c p) -> p d c", p=128), [], [lam.r], lam.r, slow=True)
        for typ, gw in enumerate((ga_w, gx_w)):
            for d in range(2):
                for cc in range(4):
                    mi = (typ * 2 + d) * 4 + cc
                    for hh in range(2):
                        dma('sp', wgs[hh * 64:(hh + 1) * 64, mi, hh * 64:(hh + 1) * 64], gw[d, 2 * cc + hh], [wgs.r], [], wgs.r, accum=[wgs.r])
        P.op('pool', lambda e: e.tensor_copy(out=wg[:, :, :], in_=wgs[:, :, :]), reads=[wgs.r], writes=[wg.r])
        P.op('act', lambda e: e.activation(out=cA[:, :, :], in_=lam[:, :, :], func=AF.Exp, scale=-1.0), reads=[lam.r], writes=[cA.r])
        P.op('act', lambda e: e.activation(out=cA[:, :, :], in_=cA[:, :, :], func=AF.Ln, bias=ones_f[:, 0:1], scale=1.0), reads=[cA.r, ones_f.r], writes=[cA.r])
        P.op('dve', lambda e: e.tensor_scalar(out=cA2[:, :, :], in0=cA[:, :, :], scalar1=-16.0, scalar2=None, op0=ALU.mult), reads=[cA.r], writes=[cA2.r])
        P.op('dve', lambda e: e.tensor_scalar(out=cA[:, :, :], in0=cA[:, :, :], scalar1=-8.0, scalar2=None, op0=ALU.mult), reads=[cA.r], writes=[cA.r])
        segs = [(0, NCTX, 0), (NCTX, SEQ, 259)]
        chunks = [(c0, min(512, NTOK - c0)) for c0 in range(0, NTOK, 512)]
        mcnt = [0]
        for cc in range(4):
            conv_issue(0, 6)
            dma('sp', xr[:, 2:2 + NCTX], XR.ap[cc * 128:(cc + 1) * 128, 0:NCTX], [XR.r], [xr.r], xr.r)
            dma('sp', xr[:, 261:261 + SEQ], XR.ap[cc * 128:(cc + 1) * 128, NCTX:NTOK], [XR.r], [], xr.r, accum=[xr.r])
            dma('sp', gg[:, :], GG.ap[cc * 128:(cc + 1) * 128, :], [GG.r], [gg.r], gg.r)
            for (o0, ln, b0) in segs:
                P.op('dve', lambda e, o0=o0, ln=ln, b0=b0, cc=cc: e.tensor_scalar(out=xc[:, o0:o0 + ln], in0=xr[:, b0:b0 + ln], scalar1=cw[:, cc, 0:1], scalar2=cb[:, cc:cc + 1], op0=ALU.mult, op1=ALU.add),
                     reads=[xr.r, cw.r, cb.r], writes=[xc.r])
                for k in range(1, 4):
                    P.op('dve', lambda e, o0=o0, ln=ln, b0=b0, cc=cc, k=k: e.scalar_tensor_tensor(out=xc[:, o0:o0 + ln], in0=xr[:, b0 + k:b0 + k + ln], scalar=cw[:, cc, k:k + 1], in1=xc[:, o0:o0 + ln], op0=ALU.mult, op1=ALU.add),
                         reads=[xr.r, cw.r, xc.r], writes=[xc.r])
            P.op('pool', lambda e: e.tensor_copy(out=xcb[:, :], in_=xc[:, :]), reads=[xc.r], writes=[xcb.r])
            for d in range(2):
                for (c0, cl) in chunks:
                    for typ, dst, bb in ((0, rr, gba), (1, ii, gbx)):
                        p = pm[mcnt[0] % 4]; mcnt[0] += 1
                        mi = (typ * 2 + d) * 4 + cc
                        P.op('pe', lambda e, p=p, mi=mi, c0=c0, cl=cl: e.matmul(p[:, 0:cl], lhsT=wg[:, mi, :], rhs=xcb[:, c0:c0 + cl], start=True, stop=True), reads=[wg.r, xcb.r], writes=[p.r])
                        P.op('act', lambda e, p=p, dst=dst, bb=bb, c0=c0, cl=cl, d=d, cc=cc: e.activation(out=dst[:, c0:c0 + cl], in_=p[:, 0:cl], func=AF.Sigmoid, bias=bb[:, d, cc:cc + 1], scale=1.0),
                             reads=[p.r, bb.r], writes=[dst.r])
                P.op('act', lambda e, d=d, cc=cc: e.activation(out=a2[:, :], in_=rr[:, :], func=AF.Exp, scale=cA2[:, d, cc:cc + 1]), reads=[rr.r, cA2.r], writes=[a2.r])
                P.op('act', lambda e, d=d, cc=cc: e.activation(out=rr[:, :], in_=rr[:, :], func=AF.Exp, scale=cA[:, d, cc:cc + 1]), reads=[rr.r, cA.r], writes=[rr.r])
                P.op('dve', lambda e: e.tensor_scalar(out=a2[:, :], in0=a2[:, :], scalar1=-1.0, scalar2=1.0, op0=ALU.mult, op1=ALU.add), reads=[a2.r], writes=[a2.r])
                P.op('act', lambda e: e.activation(out=a2[:, :], in_=a2[:, :], func=AF.Sqrt), reads=[a2.r], writes=[a2.r])
                P.op('dve', lambda e: e.tensor_tensor(out=a2[:, :], in0=a2[:, :], in1=ii[:, :], op=ALU.mult), reads=[a2.r, ii.r], writes=[a2.r])
                P.op('dve', lambda e: e.tensor_tensor(out=a2[:, :], in0=a2[:, :], in1=xc[:, :], op=ALU.mult), reads=[a2.r, xc.r], writes=[a2.r])
                if d == 0:
                    P.op('dve', lambda e: e.tensor_tensor_scan(out=hf[:, :], data0=rr[:, :], data1=a2[:, :], initial=0.0, op0=ALU.mult, op1=ALU.add), reads=[rr.r, a2.r], writes=[hf.r])
                else:
                    P.op('dve', lambda e: e.tensor_tensor_scan(out=hb[:, NCTX - 1::-1], data0=rr[:, NCTX - 1::-1], data1=a2[:, NCTX - 1::-1], initial=0.0, op0=ALU.mult, op1=ALU.add), reads=[rr.r, a2.r], writes=[hb.r])
                    P.op('dve', lambda e: e.tensor_tensor_scan(out=hb[:, NTOK - 1:NCTX - 1:-1], data0=rr[:, NTOK - 1:NCTX - 1:-1], data1=a2[:, NTOK - 1:NCTX - 1:-1], initial=hb[:, 0:1], op0=ALU.mult, op1=ALU.add),
                         reads=[rr.r, a2.r, hb.r], writes=[hb.r])
            P.op('pool', lambda e: e.tensor_tensor(out=hf[:, NCTX:NTOK], in0=hf[:, NCTX:NTOK], in1=hb[:, NCTX:NTOK], op=ALU.add), reads=[hf.r, hb.r], writes=[hf.r])
            P.op('pool', lambda e: e.tensor_tensor(out=yo[:, :], in0=hf[:, NCTX:NTOK], in1=gg[:, :], op=ALU.mult), reads=[hf.r, gg.r], writes=[yo.r])
            dma('pool', MT.ap[cc * 128:(cc + 1) * 128, :], yo[:, :], [yo.r], [], yo.r, accum=[MT.r])

    phase(ph_s2)

    def ph_s3(sb, ps):
        NKT = NTOK // 128
        vt = sb("vt", [128, NKT, 512], BF16)
        kt_ = [sb("kt%d" % i, [128, NTOK], BF16) for i in range(2)]
        qt_ = [sb("qt%d" % i, [128, SEQ], BF16) for i in range(2)]
        pt = [sb("pt%d" % i, [128, 1024], BF16) for i in range(2)]
        r1 = sb("r1", [128, 512]); o1 = sb("o1", [128, 512]); r2 = sb("r2", [128, 512]); o2 = sb("o2", [128, 512])
        sq = sb("sq", [128, 512], BF16); rsd = sb("rsd", [128, 512]); ob = [sb("ob%d" % i, [128, 512], BF16) for i in range(2)]
        g08 = sb("g08", [128, 1])
        psS = [ps("psS%d" % i, [128, 1024]) for i in range(2)]
        psO = [ps("psO%d" % i) for i in range(2)]; psD = [ps("psD%d" % i) for i in range(2)]
        accA = [sb("accA%d" % i, [128, 1024]) for i in range(2)]
        dsb = [sb("dsb%d" % i, [128, 512]) for i in range(2)]
        dma('sp', g08[:, :], da_sub.rearrange("(p o) -> p o", o=1), [], [g08.r], g08.r, slow=True)
        P.op('dve', lambda e: e.tensor_scalar(out=g08[:, :], in0=g08[:, :], scalar1=0.8, scalar2=None, op0=ALU.mult), reads=[g08.r], writes=[g08.r])
        dma('sp', vt[:, :, :], VV.ap.rearrange("(t p) n -> p t n", p=128), [VV.r], [vt.r], vt.r)
        oc = [0]
        for hd in range(4):
            kt = kt_[hd % 2]; qt = qt_[hd % 2]
            dma('sp', kt[:, :], KT.ap[hd * 128:(hd + 1) * 128, :], [KT.r], [kt.r], kt.r)
            dma('sp', qt[:, :], QT.ap[hd * 128:(hd + 1) * 128, :], [QT.r], [qt.r], qt.r)
            for qc in range(8):
                q0 = qc * 512

                def qk(t, kt=kt, qt=qt, q0=q0):
                    S = psS[t % 2]
                    for i in range(2):
                        P.op('pe', lambda e, S=S, i=i, t=t: e.matmul(S[:, i * 512:(i + 1) * 512], lhsT=kt[i * 64:(i + 1) * 64, t * 128:(t + 1) * 128], rhs=qt[i * 64:(i + 1) * 64, q0:q0 + 512], start=True, stop=True),
                             reads=[kt.r, qt.r], writes=[S.r])
                qk(0)
                for t in range(NKT):
                    S = psS[t % 2]; p = pt[t % 2]
                    P.op('act', lambda e, S=S, p=p: e.activation(out=p[:, :], in_=S[:, :], func=AF.Exp, scale=0.125), reads=[S.r], writes=[p.r])
                    if t + 1 < NKT:
                        qk(t + 1)
                    for i in range(2):
                        P.op('pe', lambda e, p=p, i=i, t=t, hd=hd: e.matmul(psO[i][:, :], lhsT=vt[:, t, hd * 128:(hd + 1) * 128], rhs=p[:, i * 512:(i + 1) * 512], start=(t == 0), stop=(t == NKT - 1)),
                             reads=[vt.r, p.r], writes=[psO[i].r])
                    for (eng, ac, c0, c1) in (('dve', accA[qc % 2], 0, 1024),):
                        if t == 0:
                            P.op(eng, lambda e, p=p, ac=ac, c0=c0, c1=c1: e.tensor_copy(out=ac[:, :], in_=p[:, c0:c1]), reads=[p.r], writes=[ac.r])
                        else:
                            P.op(eng, lambda e, p=p, ac=ac, c0=c0, c1=c1: e.tensor_tensor(out=ac[:, :], in0=ac[:, :], in1=p[:, c0:c1], op=ALU.add), reads=[p.r, ac.r], writes=[ac.r])
                aA = accA[qc % 2]
                P.op('pe', lambda e, aA=aA: e.matmul(psD[0][:, :], lhsT=ones_f[:, :], rhs=aA[:, 0:512], start=True, stop=True), reads=[ones_f.r, aA.r], writes=[psD[0].r])
                P.op('pe', lambda e, aA=aA: e.matmul(psD[1][:, :], lhsT=ones_f[:, :], rhs=aA[:, 512:1024], start=True, stop=True), reads=[ones_f.r, aA.r], writes=[psD[1].r])
                for i in range(2):
                    P.op('act', lambda e, i=i: e.activation(out=dsb[i][:, :], in_=psD[i][:, :], func=AF.Ln), reads=[psD[i].r], writes=[dsb[i].r])
                    P.op('act', lambda e, i=i: e.activation(out=dsb[i][:, :], in_=dsb[i][:, :], func=AF.Exp, scale=-1.0), reads=[dsb[i].r], writes=[dsb[i].r])
                P.op('dve', lambda e: e.tensor_tensor(out=o1[:, :], in0=psO[0][:, :], in1=dsb[0][:, :], op=ALU.mult), reads=[psO[0].r, dsb[0].r], writes=[o1.r])
                P.op('dve', lambda e: e.tensor_tensor(out=o2[:, :], in0=psO[1][:, :], in1=dsb[1][:, :], op=ALU.mult), reads=[psO[1].r, dsb[1].r], writes=[o2.r])
                P.op('dve', lambda e: e.scalar_tensor_tensor(out=o1[:, :], in0=o2[:, :], scalar=lamc[:, 0:1], in1=o1[:, :], op0=ALU.mult, op1=ALU.add), reads=[o2.r, o1.r, lamc.r], writes=[o1.r])
                P.op('pool', lambda e: e.tensor_tensor(out=sq[:, :], in0=o1[:, :], in1=o1[:, :], op=ALU.mult), reads=[o1.r], writes=[sq.r])
                S = psS[0]
                P.op('pe', lambda e, S=S: e.matmul(S[:, 0:512], lhsT=ones_b[:, :], rhs=sq[:, :], start=True, stop=True), reads=[ones_b.r, sq.r], writes=[S.r])
                P.op('act', lambda e, S=S: e.activation(out=rsd[:, :], in_=S[:, 0:512], func=AF.Ln, bias=eps_c[:, 0:1], scale=1.0 / 128.0), reads=[S.r, eps_c.r], writes=[rsd.r])
                P.op('act', lambda e: e.activation(out=rsd[:, :], in_=rsd[:, :], func=AF.Exp, scale=-0.5), reads=[rsd.r], writes=[rsd.r])
                P.op('dve', lambda e: e.tensor_tensor(out=o1[:, :], in0=o1[:, :], in1=rsd[:, :], op=ALU.mult), reads=[o1.r, rsd.r], writes=[o1.r])
                o = ob[oc[0] % 2]; oc[0] += 1
                P.op('act', lambda e, o=o: e.activation(out=o[:, :], in_=o1[:, :], func=AF.Identity, scale=g08[:, 0:1]), reads=[o1.r, g08.r], writes=[o.r])
                dma('act', MT.ap[512 + hd * 128:512 + (hd + 1) * 128, q0:q0 + 512], o[:, :], [o.r], [], o.r, accum=[MT.r])

    phase(ph_s3)

    def load_bc(sb, l, j):
        lg = sb("lgbc", [128, D]); lb = sb("lbbc", [128, D])
        dma('sp', lg[:, :], ln_g[l, j].partition_broadcast(128), [], [lg.r], lg.r)
        dma('sp', lb[:, :], ln_b[l, j].partition_broadcast(128), [], [lb.r], lb.r)
        return lg, lb

    def resid_A(x, t, z, lnt, l, gi):
        stats, mv, rstd, nmr = lnt
        P.op('dve', lambda e: e.tensor_tensor(out=t[:, :], in0=t[:, :], in1=gbc[:, l, gi, :], op=ALU.mult), reads=[t.r, gbc.r], writes=[t.r])
        P.op('dve', lambda e: e.scalar_tensor_tensor(out=z[:, :], in0=x[:, :], scalar=ALPHA, in1=t[:, :], op0=ALU.mult, op1=ALU.add), reads=[x.r, t.r], writes=[z.r])
        ln_stats(z, stats, mv, rstd, nmr, [z.r])

    def resid_B(z, lnt, lg, lb, o):
        stats, mv, rstd, nmr = lnt
        P.op('act', lambda e: e.activation(out=z[:, :], in_=z[:, :], func=AF.Identity, scale=rstd[:, 0:1], bias=nmr[:, 0:1]), reads=[z.r, rstd.r, nmr.r], writes=[z.r])
        P.op('pool', lambda e: e.tensor_tensor(out=z[:, :], in0=z[:, :], in1=lg[:, :], op=ALU.mult), reads=[z.r, lg.r], writes=[z.r])
        P.op('dve', lambda e: e.tensor_tensor(out=o[:, :], in0=z[:, :], in1=lb[:, :], op=ALU.add), reads=[z.r, lb.r], writes=[o.r])

    def ph_s4(sb, ps):
        wo = sb("wo", [128, 8, D], BF16)
        wst = [sb("wst%d" % i, [128, 8, 512]) for i in range(2)]
        for ch in range(2):
            w = wst[ch]
            dma('sp', w[:, :, :], ev_w_out[:, ch * 512:(ch + 1) * 512].rearrange("(k p) n -> p k n", p=128), [], [w.r], w.r)
            P.op('pool', lambda e, w=w, ch=ch: e.tensor_copy(out=wo[:, :, ch * 512:(ch + 1) * 512], in_=w[:, :, :]), reads=[w.r], writes=[wo.r])
        lg, lb = load_bc(sb, 0, 0)
        mt = [sb("mt%d" % i, [128, 8, 512], BF16) for i in range(2)]
        xt = [sb("xt%d" % i, [128, D]) for i in range(NB)]; tt = [sb("tt%d" % i, [128, D]) for i in range(NB)]
        zz = [sb("zz%d" % i, [128, D]) for i in range(NB)]; oo = [sb("oo%d" % i, [128, D]) for i in range(NB)]
        lnts = [(sb("stats%d" % i, [128, 12]), sb("mv%d" % i, [128, 2]), sb("rstd%d" % i, [128, 1]), sb("nmr%d" % i, [128, 1])) for i in range(NB)]
        py = [ps("py%d" % i, [128, 1024]) for i in range(3)]
        def stB(ti):
            z = zz[ti % NB]; o = oo[ti % NB]
            resid_B(z, lnts[ti % NB], lg, lb, o)
            if ti >= 1:
                stS(ti - 1)

        def stS(ti):
            o = oo[ti % NB]
            dma('pool', X1.ap[ti * 128:(ti + 1) * 128, :], o[:, :], [o.r], [], o.r, accum=[X1.r])
        for s in range(8):
            m = mt[s % 2]
            dma('sp', m[:, :, :], MT.ap[:, s * 512:(s + 1) * 512].rearrange("(k p) t -> p k t", p=128), [MT.r], [m.r], m.r)
            for j in range(4):
                ti = s * 4 + j
                x = xt[ti % NB]; t = tt[ti % NB]; z = zz[ti % NB]; y = py[ti % 3]; lnt = lnts[ti % NB]
                dma('sp', x[:, :], x_in[ti * 128:(ti + 1) * 128, :], [], [x.r], x.r)
                for h in range(2):
                    for kc in range(8):
                        P.op('pe', lambda e, y=y, m=m, h=h, kc=kc, j=j: e.matmul(y[:, h * 512:(h + 1) * 512], lhsT=m[:, kc, j * 128:(j + 1) * 128], rhs=wo[:, kc, h * 512:(h + 1) * 512], start=(kc == 0), stop=(kc == 7)),
                             reads=[m.r, wo.r], writes=[y.r])
                P.op('act', lambda e, y=y, t=t: e.copy(out=t[:, :], in_=y[:, :]), reads=[y.r], writes=[t.r])
                resid_A(x, t, z, lnt, 0, 0)
                if ti >= 1:
                    stB(ti - 1)
        stB(NT - 1)
        stS(NT - 1)

    phase(ph_s4)
    if 's4' in dbg:
        return nc, gst

    Am = sbp("Am", [128, NT, 2, 32]); RK = sbp("RK", [128, NT, 2]); WG = sbp("WG", [128, NT, 2])
    SLOT = sbp("SLOT", [128, NT * 2], I32); carry = sbp("carry", [128, 32]); idxW = sbp("idxW", [128, NBLK], I32)
    pstart = sbp("pstart", [128, 32])

    def moe(l, Xin, Xout):
        def ph_r(sb, ps):
            wr = sb("wr", [128, 8, 36]); rb = sb("rb", [128, 36])
            dma('sp', wr[:, :, 0:4], moe_wg[l].rearrange("(k p) g -> p k g", p=128), [], [], wr.r, accum=[wr.r], slow=True)
            dma('sp', wr[:, :, 4:36], moe_wf[l].rearrange("(k p) g -> p k g", p=128), [], [], wr.r, accum=[wr.r], slow=True)
            dma('sp', rb[:, 0:4], moe_bg[l].partition_broadcast(128), [], [], rb.r, accum=[rb.r], slow=True)
            dma('sp', rb[:, 4:36], moe_bf[l].partition_broadcast(128), [], [], rb.r, accum=[rb.r], slow=True)
            LG = sb("LG", [128, NT, 36]); xnb = sb("xnb", [128, NT, D], BF16)
            xt = [sb("xt%d" % i, [128, D]) for i in range(NB)]; xn = [sb("xn%d" % i, [128, D]) for i in range(NB)]
            tokT = [sb("tokT%d" % i, [128, 8, 128]) for i in range(NB)]
            lnts = [(sb("stats%d" % i, [128, 12]), sb("mv%d" % i, [128, 2]), sb("rstd%d" % i, [128, 1]), sb("nmr%d" % i, [128, 1])) for i in range(NB)]
            pT = [ps("pT%d" % i) for i in range(4)]; plg = [ps("plg%d" % i) for i in range(2)]
            for ti in range(NT):
                x = xt[ti % NB]; n = xn[ti % NB]; tk = tokT[ti % NB]; lnt = lnts[ti % NB]; pl = plg[ti % 2]
                dma('sp', x[:, :], Xin.ap[ti * 128:(ti + 1) * 128, :], [Xin.r], [x.r], x.r)
                ln_stats(x, lnt[0], lnt[1], lnt[2], lnt[3], [x.r])
                P.op('act', lambda e, x=x, n=n, lnt=lnt: e.activation(out=n[:, :], in_=x[:, :], func=AF.Identity, scale=lnt[2][:, 0:1], bias=lnt[3][:, 0:1]), reads=[x.r, lnt[2].r, lnt[3].r], writes=[n.r])
                P.op('pool', lambda e, n=n, ti=ti: e.tensor_copy(out=xnb[:, ti, :], in_=n[:, :]), reads=[n.r], writes=[], accum=[xnb.r])
                for kc in range(8):
                    p = pT[(ti % 2) * 2 + kc // 4]
                    P.op('pe', lambda e, p=p, kc=kc, n=n: e.transpose(p[:, (kc % 4) * 128:(kc % 4 + 1) * 128], n[:, kc * 128:(kc + 1) * 128], ident[:, :]), reads=[n.r, ident.r], writes=[p.r])
                for kc in range(8):
                    p = pT[(ti % 2) * 2 + kc // 4]
                    P.op('act', lambda e, p=p, kc=kc, tk=tk: e.activation(out=tk[:, kc, :], in_=p[:, (kc % 4) * 128:(kc % 4 + 1) * 128], func=AF.Identity, scale=adac[:, l, 3, kc:kc + 1], bias=adac[:, l, 2, kc:kc + 1]),
                         reads=[p.r, adac.r], writes=[tk.r])
                for kc in range(8):
                    P.op('pe', lambda e, kc=kc, tk=tk, pl=pl: e.matmul(pl[:, 0:36], lhsT=tk[:, kc, :], rhs=wr[:, kc, :], start=(kc == 0), stop=(kc == 7)), reads=[tk.r, wr.r], writes=[pl.r])
                P.op('dve', lambda e, pl=pl, ti=ti: e.tensor_tensor(out=LG[:, ti, :], in0=pl[:, 0:36], in1=rb[:, :], op=ALU.add), reads=[pl.r, rb.r], writes=[], accum=[LG.r])
            gmax = sb("gmax", [128, NT]); dlt = sb("dlt", [128, NT, 4]); gmask = sb("gmask", [128, NT, 4]); sg = sb("sg", [128, NT])
            pen = sb("pen", [128, NT, 4]); fm = sb("fm", [128, NT, 32]); T8 = sb("T8", [128, NT, 8]); dd = sb("dd", [128, NT]); s0 = sb("s0", [128, NT])
            Asb = sb("Asb", [128, NT, 32], BF16); tmp3 = sb("tmp3", [128, NT, 32]); slk = sb("slk", [128, NT])
            ppf = ps("ppf", [128, 1024]); pcnt = plg[0]
            P.op('dve', lambda e: e.reduce_max(out=gmax[:, :], in_=LG[:, :, 0:4], axis=AX.X), reads=[LG.r], writes=[gmax.r])
            P.op('dve', lambda e: e.tensor_tensor(out=dlt[:, :, :], in0=LG[:, :, 0:4], in1=gmax[:, :].unsqueeze(2).to_broadcast([128, NT, 4]), op=ALU.subtract), reads=[LG.r, gmax.r], writes=[dlt.r])
            P.op('dve', lambda e: e.tensor_scalar(out=gmask[:, :, :], in0=dlt[:, :, :], scalar1=0.0, scalar2=None, op0=ALU.is_equal), reads=[dlt.r], writes=[gmask.r])
            P.op('act', lambda e: e.activation(out=dlt[:, :, :], in_=dlt[:, :, :], func=AF.Exp), reads=[dlt.r, gmask.r], writes=[dlt.r])
            P.op('dve', lambda e: e.reduce_sum(out=sg[:, :], in_=dlt[:, :, :], axis=AX.X), reads=[dlt.r], writes=[sg.r])
            P.op('dve', lambda e: e.reciprocal(out=sg[:, :], in_=sg[:, :]), reads=[sg.r], writes=[sg.r])
            P.op('dve', lambda e: e.tensor_scalar(out=pen[:, :, :], in0=gmask[:, :, :], scalar1=-1.0, scalar2=1e30, op0=ALU.add, op1=ALU.mult), reads=[gmask.r], writes=[pen.r])
            P.op('dve', lambda e: e.tensor_tensor(out=fm[:, :, :].rearrange("p t (g j) -> p t g j", g=4), in0=LG[:, :, 4:36].rearrange("p t (g j) -> p t g j", g=4), in1=pen[:, :, :].unsqueeze(3).to_broadcast([128, NT, 4, 8]), op=ALU.add),
                 reads=[LG.r, pen.r], writes=[fm.r])
            for ti in range(NT):
                P.op('dve', lambda e, ti=ti: e.max(out=T8[:, ti, :], in_=fm[:, ti, :]), reads=[fm.r], writes=[], accum=[T8.r])
            for k in range(2):
                P.op('dve', lambda e, k=k: e.tensor_tensor(out=Am[:, :, k, :], in0=fm[:, :, :], in1=T8[:, :, k:k + 1].to_broadcast([128, NT, 32]), op=ALU.is_equal), reads=[fm.r, T8.r], writes=[], accum=[Am.r])
            P.op('dve', lambda e: e.tensor_tensor(out=dd[:, :], in0=T8[:, :, 0], in1=T8[:, :, 1], op=ALU.subtract), reads=[T8.r], writes=[dd.r])
            P.op('act', lambda e: e.activation(out=s0[:, :], in_=dd[:, :], func=AF.Sigmoid), reads=[dd.r], writes=[s0.r])
            P.op('dve', lambda e: e.tensor_tensor(out=WG[:, :, 0], in0=sg[:, :], in1=s0[:, :], op=ALU.mult), reads=[sg.r, s0.r], writes=[WG.r])
            P.op('dve', lambda e: e.tensor_tensor(out=WG[:, :, 1], in0=sg[:, :], in1=WG[:, :, 0], op=ALU.subtract), reads=[sg.r, WG.r], writes=[WG.r])
            P.op('dve', lambda e: e.tensor_tensor(out=Asb[:, :, :], in0=Am[:, :, 0, :], in1=Am[:, :, 1, :], op=ALU.add), reads=[Am.r], writes=[Asb.r])
            for ti in range(NT):
                P.op('pe', lambda e, ti=ti: e.matmul(ppf[:, ti * 32:(ti + 1) * 32], lhsT=ustrict_b[:, :], rhs=Asb[:, ti, :], start=True, stop=(ti == 0)), reads=[ustrict_b.r, Asb.r], writes=[ppf.r])
                for tj in range(ti):
                    P.op('pe', lambda e, ti=ti, tj=tj: e.matmul(ppf[:, ti * 32:(ti + 1) * 32], lhsT=ones_b[:, :], rhs=Asb[:, tj, :], start=False, stop=(tj == ti - 1)), reads=[ones_b.r, Asb.r], writes=[ppf.r])
            for tj in range(NT):
                P.op('pe', lambda e, tj=tj: e.matmul(pcnt[:, 0:32], lhsT=ones_b[:, :], rhs=Asb[:, tj, :], start=(tj == 0), stop=(tj == NT - 1)), reads=[ones_b.r, Asb.r], writes=[pcnt.r])
            P.op('dve', lambda e: e.tensor_copy(out=carry[:, :], in_=pcnt[:, 0:32]), reads=[pcnt.r], writes=[carry.r])
            for k in range(2):
                P.op('dve', lambda e, k=k: e.tensor_tensor(out=tmp3[:, :, :], in0=Am[:, :, k, :], in1=ppf[:, :].rearrange("p (t e) -> p t e", e=32), op=ALU.mult), reads=[Am.r, ppf.r], writes=[tmp3.r])
                P.op('dve', lambda e, k=k: e.reduce_sum(out=RK[:, :, k], in_=tmp3[:, :, :], axis=AX.X), reads=[tmp3.r], writes=[RK.r])
            pad = sb("pad", [128, 32]); mm_ = sb("mm_", [128, 32]); pend = sb("pend", [128, 32]); thr = sb("thr", [128, NBLK])
            cmp = sb("cmp", [128, NBLK, 32]); be = sb("be", [128, NBLK]); idxf = sb("idxf", [128, NBLK])
            P.op('pool', lambda e: e.iota(thr[:, :], pattern=[[BLK, NBLK]], base=0, channel_multiplier=0, allow_small_or_imprecise_dtypes=True), writes=[thr.r])
            P.op('dve', lambda e: e.tensor_tensor(out=cmp[:, 0:32, :], in0=carry[:, :].unsqueeze(2).to_broadcast([128, 32, 32]), in1=thr[:, 0:32].unsqueeze(1).to_broadcast([128, 32, 32]), op=ALU.is_gt), reads=[carry.r, thr.r], writes=[cmp.r])
            P.op('dve', lambda e: e.reduce_sum(out=mm_[:, :], in_=cmp[:, 0:32, :], axis=AX.X), reads=[cmp.r], writes=[mm_.r])
            P.op('dve', lambda e: e.tensor_scalar(out=pad[:, :], in0=mm_[:, :], scalar1=float(BLK), scalar2=None, op0=ALU.mult), reads=[mm_.r], writes=[pad.r])
            P.op('dve', lambda e: e.tensor_tensor_scan(out=pend[:, :], data0=ones_f[:, 0:32], data1=pad[:, :], initial=0.0, op0=ALU.mult, op1=ALU.add), reads=[ones_f.r, pad.r], writes=[pend.r])
            P.op('dve', lambda e: e.tensor_tensor(out=pstart[:, :], in0=pend[:, :], in1=pad[:, :], op=ALU.subtract), reads=[pend.r, pad.r], writes=[pstart.r])
            P.op('dve', lambda e: e.tensor_tensor(out=cmp[:, :, :], in0=pend[:, :].unsqueeze(1).to_broadcast([128, NBLK, 32]), in1=thr[:, :].unsqueeze(2).to_broadcast([128, NBLK, 32]), op=ALU.is_le), reads=[pend.r, thr.r], writes=[cmp.r])
            P.op('dve', lambda e: e.reduce_sum(out=be[:, :], in_=cmp[:, :, :], axis=AX.X), reads=[cmp.r], writes=[be.r])
            P.op('dve', lambda e: e.tensor_scalar(out=be[:, :], in0=be[:, :], scalar1=31.0, scalar2=128.0, op0=ALU.min, op1=ALU.mult), reads=[be.r], writes=[be.r])
            P.op('dve', lambda e: e.tensor_scalar(out=idxf[:, :], in0=be[:, :], scalar1=iota_p[:, 0:1], scalar2=float(l * 4096), op0=ALU.add, op1=ALU.add), reads=[be.r, iota_p.r], writes=[idxf.r])
            P.op('dve', lambda e: e.tensor_copy(out=idxW[:, :], in_=idxf[:, :]), reads=[idxf.r], writes=[idxW.r])
            for k in range(2):
                P.op('dve', lambda e, k=k: e.tensor_tensor(out=tmp3[:, :, :], in0=Am[:, :, k, :], in1=pstart[:, :].unsqueeze(1).to_broadcast([128, NT, 32]), op=ALU.mult), reads=[Am.r, pstart.r], writes=[tmp3.r])
                P.op('dve', lambda e, k=k: e.reduce_sum(out=slk[:, :], in_=tmp3[:, :, :], axis=AX.X), reads=[tmp3.r], writes=[slk.r])
                P.op('dve', lambda e, k=k: e.tensor_tensor(out=slk[:, :], in0=slk[:, :], in1=RK[:, :, k], op=ALU.add), reads=[slk.r, RK.r], writes=[slk.r])
                P.op('dve', lambda e, k=k: e.tensor_copy(out=SLOT[:, k::2], in_=slk[:, :]), reads=[slk.r], writes=[], accum=[SLOT.r])
            for ti in range(NT):
                for k in range(2):
                    P.op('pool', lambda e, ti=ti, k=k: e.indirect_dma_start(out=XG.ap, out_offset=bass.IndirectOffsetOnAxis(ap=SLOT[:, ti * 2 + k:ti * 2 + k + 1], axis=0), in_=xnb[:, ti, :], in_offset=None),
                         reads=[xnb.r, SLOT.r], writes=[], dma=xnb.r, accum=[XG.r])

        phase(ph_r)

        def ph_e(sb, ps):
            w1b = [sb("w1b%d" % i, [128, 8, 512], BF16) for i in range(2)]
            w3b = [sb("w3b%d" % i, [128, 8, 512], BF16) for i in range(2)]
            w2b = [sb("w2b%d" % i, [128, 4, D], BF16) for i in range(2)]
            xb = [sb("xb%d" % i, [128, 2, D], BF16) for i in range(2)]
            XT = [sb("XT%d" % i, [128, 8, BLK], BF16) for i in range(2)]
            hid = [sb("hid%d" % i, [128, 4, BLK], BF16) for i in range(2)]
            sa = [sb("sa%d" % i, [128, BLK]) for i in range(2)]
            ysb = [sb("ysb%d" % i, [128, D]) for i in range(2)]
            pT = [ps("pT%d" % i, [128, 512], BF16) for i in range(2)]; pa = [ps("pa%d" % i) for i in range(2)]; pb = [ps("pb%d" % i) for i in range(2)]
            py = ps("py", [128, 1024])
            w1v = W1B; w3v = W3B; w2v = W2B
            yc = [0]

            def prep(i):
                b = i % 2
                for wt, wv in ((w1b[b], w1v), (w3b[b], w3v), (w2b[b], w2v)):
                    P.op('pool', lambda e, wt=wt, wv=wv, i=i: e.indirect_dma_start(out=wt[:, :, :].rearrange("p a b -> p (a b)"), out_offset=None, in_=wv.ap, in_offset=bass.IndirectOffsetOnAxis(ap=idxW[:, i:i + 1], axis=0)),
                         reads=[idxW.r, wv.r], writes=[wt.r], dma=wt.r)
                x = xb[b]
                dma('sp', x[:, :, :], XG.ap[i * BLK:(i + 1) * BLK, :].rearrange("(j p) d -> p j d", p=128), [XG.r], [x.r], x.r)
                xT = XT[b]
                for kc in range(8):
                    p = pT[kc % 2]
                    for j in range(2):
                        P.op('pe', lambda e, p=p, j=j, kc=kc, x=x: e.transpose(p[:, j * 128:(j + 1) * 128], x[:, j, kc::8], ident_b[:, :]), reads=[x.r, ident_b.r], writes=[p.r])
                    P.op('act', lambda e, p=p, kc=kc, xT=xT: e.activation(out=xT[:, kc, :], in_=p[:, 0:BLK], func=AF.Identity, scale=adap[:, l, 1, kc:kc + 1], bias=adap[:, l, 0, kc:kc + 1]),
                         reads=[p.r, adap.r], writes=[xT.r])

            def compute(i):
                b = i % 2
                xT = XT[b]; h = hid[b]
                for fc in range(4):
                    A_ = pa[fc % 2]; B_ = pb[fc % 2]; s_ = sa[fc % 2]
                    for (pp, wt) in ((A_, w1b[b]), (B_, w3b[b])):
                        for kc in range(8):
                            P.op('pe', lambda e, pp=pp, wt=wt, kc=kc, fc=fc, xT=xT: e.matmul(pp[:, 0:BLK], lhsT=wt[:, kc, fc::4], rhs=xT[:, kc, :], start=(kc == 0), stop=(kc == 7)), reads=[wt.r, xT.r], writes=[pp.r])
                    P.op('act', lambda e, A_=A_, s_=s_: e.activation(out=s_[:, :], in_=A_[:, 0:BLK], func=AF.Silu), reads=[A_.r], writes=[s_.r])
                    P.op('dve', lambda e, B_=B_, s_=s_, h=h, fc=fc: e.tensor_tensor(out=h[:, fc, :], in0=s_[:, :], in1=B_[:, 0:BLK], op=ALU.mult), reads=[s_.r, B_.r], writes=[h.r])
                for j in range(2):
                    for hh in range(2):
                        for fc in range(4):
                            P.op('pe', lambda e, j=j, hh=hh, fc=fc, h=h, b=b: e.matmul(py[:, hh * 512:(hh + 1) * 512], lhsT=h[:, fc, j * 128:(j + 1) * 128], rhs=w2b[b][:, fc, hh * 512:(hh + 1) * 512], start=(fc == 0), stop=(fc == 3)),
                                 reads=[h.r, w2b[b].r], writes=[py.r])
                    y = ysb[yc[0] % 2]; yc[0] += 1
                    if j == 0:
                        P.op('act', lambda e, y=y: e.copy(out=y[:, :], in_=py[:, :]), reads=[py.r], writes=[y.r])
                    else:
                        P.op('dve', lambda e, y=y: e.tensor_copy(out=y[:, :], in_=py[:, :]), reads=[py.r], writes=[y.r])
                    dma('act', YG.ap[i * BLK + j * 128: i * BLK + (j + 1) * 128, :], y[:, :], [y.r], [], y.r, accum=[YG.r])

            prep(0)
            for i in range(NBLK):
                if i + 1 < NBLK:
                    prep(i + 1)
                compute(i)

        phase(ph_e)

        def ph_c(sb, ps):
            lg, lb = load_bc(sb, l, 1)
            y0 = [sb("y0%d" % i, [128, D]) for i in range(NB)]; y1 = [sb("y1%d" % i, [128, D]) for i in range(NB)]
            xt = [sb("xt%d" % i, [128, D]) for i in range(NB)]; tt = [sb("tt%d" % i, [128, D]) for i in range(NB)]
            zz = [sb("zz%d" % i, [128, D]) for i in range(NB)]; oo = [sb("oo%d" % i, [128, D]) for i in range(NB)]
            lnts = [(sb("stats%d" % i, [128, 12]), sb("mv%d" % i, [128, 2]), sb("rstd%d" % i, [128, 1]), sb("nmr%d" % i, [128, 1])) for i in range(NB)]
            for it in range(NT + 3):
                if it < NT:
                    ti = it; b = ti % NB
                    for k, yy in ((0, y0[b]), (1, y1[b])):
                        P.op('pool', lambda e, yy=yy, ti=ti, k=k: e.indirect_dma_start(out=yy[:, :], out_offset=None, in_=YG.ap, in_offset=bass.IndirectOffsetOnAxis(ap=SLOT[:, ti * 2 + k:ti * 2 + k + 1], axis=0)),
                             reads=[YG.r, SLOT.r], writes=[yy.r], dma=yy.r)
                    dma('sp', xt[b][:, :], Xin.ap[ti * 128:(ti + 1) * 128, :], [Xin.r], [xt[b].r], xt[b].r)
                if 1 <= it <= NT:
                    ti = it - 1; b = ti % NB
                    t = tt[b]
                    P.op('act', lambda e, t=t, b=b, ti=ti: e.activation(out=t[:, :], in_=y0[b][:, :], func=AF.Identity, scale=WG[:, ti, 0:1]), reads=[y0[b].r, WG.r], writes=[t.r])
                    P.op('dve', lambda e, t=t, b=b, ti=ti: e.scalar_tensor_tensor(out=t[:, :], in0=y1[b][:, :], scalar=WG[:, ti, 1:2], in1=t[:, :], op0=ALU.mult, op1=ALU.add), reads=[y1[b].r, WG.r, t.r], writes=[t.r])
                    resid_A(xt[b], t, zz[b], lnts[b], l, 1)
                if 2 <= it <= NT + 1:
                    ti = it - 2; b = ti % NB
                    resid_B(zz[b], lnts[b], lg, lb, oo[b])
                if it >= 3:
                    ti = it - 3; b = ti % NB
                    dma('pool', Xout.ap[ti * 128:(ti + 1) * 128, :], oo[b][:, :], [oo[b].r], [], oo[b].r, accum=[Xout.r])

        phase(ph_c)

    moe(0, X1, X2)
    if 'm0' in dbg:
        return nc, gst

    def ph_f1(sb, ps):
        ccs = sb("ccs", [128, 2, 256]); scs = sb("scs", [128, 2, 256]); wod = sb("wod", [128, 8, D])
        M1 = sb("M1", [128, 8, D], BF16); M2 = sb("M2", [128, 8, D], BF16)
        dma('sp', ccs[:, :, :], k_cc.rearrange("(a p) n -> p a n", p=128), [], [ccs.r], ccs.r)
        dma('sp', scs[:, :, :], k_sc.rearrange("(a p) n -> p a n", p=128), [], [scs.r], scs.r)
        dma('sp', wod[:, :, :], od_w_out.rearrange("(k p) n -> p k n", p=128), [], [wod.r], wod.r)
        pm = [ps("pm%d" % i) for i in range(2)]
        pT = [ps("pT%d" % i) for i in range(2)]
        pU = ps("pU", [128, 1024]); pV = ps("pV", [128, 1024])
        mc = [0]
        for which, (cs, Mx) in enumerate(((ccs, M1), (scs, M2))):
            for g in range(4):
                for cch in range(2):
                    for nh in range(2):
                        p = pm[mc[0] % 2]; mc[0] += 1
                        for c2 in range(2):
                            P.op('pe', lambda e, p=p, cs=cs, c2=c2, cch=cch, g=g, nh=nh: e.matmul(p[:, :], lhsT=cs[:, c2, cch * 128:(cch + 1) * 128], rhs=wod[:, g * 2 + c2, nh * 512:(nh + 1) * 512], start=(c2 == 0), stop=(c2 == 1)),
                                 reads=[cs.r, wod.r], writes=[p.r])
                        P.op('act', lambda e, p=p, Mx=Mx, g=g, cch=cch, nh=nh, which=which: e.activation(out=Mx[:, g * 2 + cch, nh * 512:(nh + 1) * 512], in_=p[:, :], func=AF.Identity, scale=(1.0 if which == 0 else -1.0)),
                             reads=[p.r], writes=[Mx.r])
        xt = [sb("xt%d" % i, [128, D]) for i in range(NB)]; xn = [sb("xn%d" % i, [128, D]) for i in range(NB)]
        hT = [sb("hT%d" % i, [128, 8, 128], BF16) for i in range(NB)]
        ou = [sb("ou%d" % i, [128, D], BF16) for i in range(NB)]; ov = [sb("ov%d" % i, [128, D], BF16) for i in range(NB)]
        lnts = [(sb("stats%d" % i, [128, 12]), sb("mv%d" % i, [128, 2]), sb("rstd%d" % i, [128, 1]), sb("nmr%d" % i, [128, 1])) for i in range(NB)]
        for ti in range(NT):
            conv_issue(1, 1)
            x = xt[ti % NB]; n = xn[ti % NB]; h = hT[ti % NB]; lnt = lnts[ti % NB]
            dma('sp', x[:, :], X2.ap[ti * 128:(ti + 1) * 128, :], [X2.r], [x.r], x.r)
            ln_stats(x, lnt[0], lnt[1], lnt[2], lnt[3], [x.r])
            P.op('act', lambda e, x=x, n=n, lnt=lnt: e.activation(out=n[:, :], in_=x[:, :], func=AF.Identity, scale=lnt[2][:, 0:1], bias=lnt[3][:, 0:1]), reads=[x.r, lnt[2].r, lnt[3].r], writes=[n.r])
            for kc in range(8):
                p = pT[kc // 4]
                P.op('pe', lambda e, p=p, kc=kc, n=n: e.transpose(p[:, (kc % 4) * 128:(kc % 4 + 1) * 128], n[:, kc * 128:(kc + 1) * 128], ident[:, :]), reads=[n.r, ident.r], writes=[p.r])
            for kc in range(8):
                p = pT[kc // 4]
                P.op('act', lambda e, p=p, kc=kc, h=h: e.activation(out=h[:, kc, :], in_=p[:, (kc % 4) * 128:(kc % 4 + 1) * 128], func=AF.Identity, scale=adac[:, 1, 1, kc:kc + 1], bias=adac[:, 1, 0, kc:kc + 1]),
                     reads=[p.r, adac.r], writes=[h.r])
            for (pp, Mx, oo_, DD) in ((pU, M1, ou[ti % NB], UU), (pV, M2, ov[ti % NB], VW)):
                for nh in range(2):
                    for kc in range(8):
                        P.op('pe', lambda e, pp=pp, Mx=Mx, nh=nh, kc=kc, h=h: e.matmul(pp[:, nh * 512:(nh + 1) * 512], lhsT=h[:, kc, :], rhs=Mx[:, kc, nh * 512:(nh + 1) * 512], start=(kc == 0), stop=(kc == 7)),
                             reads=[h.r, Mx.r], writes=[pp.r])
                P.op('act', lambda e, pp=pp, oo_=oo_: e.copy(out=oo_[:, :], in_=pp[:, :]), reads=[pp.r], writes=[oo_.r])
                dma('act', DD.ap[ti * 128:(ti + 1) * 128, :], oo_[:, :], [oo_.r], [], oo_.r, accum=[DD.r])

    phase(ph_f1)

    def ph_f2(sb, ps):
        Uh = sb("Uh", [128, NT, 512], BF16); Vh = sb("Vh", [128, NT, 512], BF16)
        cn = [sb("cn%d" % i, [128, NT * 128], BF16) for i in range(2)]; sn = [sb("sn%d" % i, [128, NT * 128], BF16) for i in range(2)]
        yo = [sb("yo%d" % i, [128, 512]) for i in range(2)]
        py = [ps("py%d" % i) for i in range(2)]
        it = 0
        for nh in range(2):
            dma('sp', Uh[:, :, :], UU.ap[:, nh * 512:(nh + 1) * 512].rearrange("(t p) n -> p t n", p=128), [UU.r], [Uh.r], Uh.r)
            dma('sp', Vh[:, :, :], VW.ap[:, nh * 512:(nh + 1) * 512].rearrange("(t p) n -> p t n", p=128), [VW.r], [Vh.r], Vh.r)
            for kt in range(NT):
                c = cn[it % 2]; s_ = sn[it % 2]; y = yo[it % 2]; p = py[it % 2]; it += 1
                dma('sp', c[:, :], k_cn[kt], [], [c.r], c.r)
                dma('sp', s_[:, :], k_sn[kt], [], [s_.r], s_.r)
                for tt in range(NT):
                    P.op('pe', lambda e, p=p, c=c, tt=tt: e.matmul(p[:, :], lhsT=c[:, tt * 128:(tt + 1) * 128], rhs=Uh[:, tt, :], start=(tt == 0), stop=False), reads=[c.r, Uh.r], writes=[p.r])
                    P.op('pe', lambda e, p=p, s_=s_, tt=tt: e.matmul(p[:, :], lhsT=s_[:, tt * 128:(tt + 1) * 128], rhs=Vh[:, tt, :], start=False, stop=(tt == NT - 1)), reads=[s_.r, Vh.r], writes=[p.r])
                P.op('act', lambda e, p=p, y=y: e.copy(out=y[:, :], in_=p[:, :]), reads=[p.r], writes=[y.r])
                dma('act', Y1.ap[kt * 128:(kt + 1) * 128, nh * 512:(nh + 1) * 512], y[:, :], [y.r], [], y.r, accum=[Y1.r])

    phase(ph_f2)

    def ph_f3(sb, ps):
        lg, lb = load_bc(sb, 1, 0)
        bb = sb("bb", [128, D])
        dma('sp', bb[:, :], od_b_out.partition_broadcast(128), [], [bb.r], bb.r)
        xt = [sb("xt%d" % i, [128, D]) for i in range(NB)]; tt = [sb("tt%d" % i, [128, D]) for i in range(NB)]
        zz = [sb("zz%d" % i, [128, D]) for i in range(NB)]; oo = [sb("oo%d" % i, [128, D]) for i in range(NB)]
        lnts = [(sb("stats%d" % i, [128, 12]), sb("mv%d" % i, [128, 2]), sb("rstd%d" % i, [128, 1]), sb("nmr%d" % i, [128, 1])) for i in range(NB)]
        for it in range(NT + 2):
            if it < NT:
                ti = it
                x = xt[ti % NB]; t = tt[ti % NB]; z = zz[ti % NB]; lnt = lnts[ti % NB]
                dma('sp', x[:, :], X2.ap[ti * 128:(ti + 1) * 128, :], [X2.r], [x.r], x.r)
                dma('sp', t[:, :], Y1.ap[ti * 128:(ti + 1) * 128, :], [Y1.r], [t.r], t.r)
                P.op('pool', lambda e, t=t: e.tensor_tensor(out=t[:, :], in0=t[:, :], in1=bb[:, :], op=ALU.add), reads=[t.r, bb.r], writes=[t.r])
                resid_A(x, t, z, lnt, 1, 0)
            if 1 <= it <= NT:
                ti = it - 1
                resid_B(zz[ti % NB], lnts[ti % NB], lg, lb, oo[ti % NB])
            if it >= 2:
                ti = it - 2
                dma('pool', X3.ap[ti * 128:(ti + 1) * 128, :], oo[ti % NB][:, :], [oo[ti % NB].r], [], oo[ti % NB].r, accum=[X3.r])

    phase(ph_f3)
    if 'f3' in dbg:
        return nc, gst
    moe(1, X3, out_d)
    return nc, gst


def _consts():
    t = np.arange(SEQ)
    row = (t // 64).astype(np.float32); col = (t % 64).astype(np.float32)
    nf = 16
    freqs = (10000.0 ** (-np.arange(nf, dtype=np.float32) / nf)).astype(np.float32)
    ropec = np.zeros((128, SEQ), np.float32); ropes = np.zeros((128, SEQ), np.float32)
    for i in range(2):
        for d in range(64):
            pos = row if d < 32 else col
            ang = (pos * freqs[d % 16]).astype(np.float32)
            ropec[i * 64 + d] = np.cos(ang)
            sgn = -1.0 if (d % 32) < 16 else 1.0
            ropes[i * 64 + d] = sgn * np.sin(ang)
    k = np.arange(256)
    ang = 2 * np.pi * np.outer(k, k) / 256.0
    cc = (np.cos(ang) / 16.0).astype(np.float32); sc = (np.sin(ang) / 16.0).astype(np.float32)
    n = np.arange(SEQ)
    kt = (np.outer(n, n) % SEQ).astype(np.float64) * (2 * np.pi / SEQ)
    cn = (np.cos(kt) / 64.0); sn = (np.sin(kt) / 64.0)

    def lay(m):
        m4 = m.reshape(32, 128, 32, 128)
        return np.ascontiguousarray(m4.transpose(2, 1, 0, 3)).reshape(32, 128, 32 * 128).astype(ml_dtypes.bfloat16)
    return dict(k_ropec=ropec, k_ropes=ropes, k_cc=cc, k_sc=sc, k_cn=lay(cn), k_sn=lay(sn))


_CACHE = {}


def kernel(**inputs):
    dbg = tuple(os.environ.get("KDBG", "").split(",")) if os.environ.get("KDBG") else ()
    nc, gst = build(dbg)
    gst.close()
    if 'consts' not in _CACHE:
        _CACHE['consts'] = _consts()
    cst = _CACHE['consts']
    f = lambda a: np.ascontiguousarray(np.asarray(a, dtype=np.float32))
    shared = {k: f(inputs[k]) for k in ['c_ctx', 'ada_w', 'ada_b', 'ln_g', 'ln_b', 'od_b_out', 'moe_wg', 'moe_bg', 'moe_wf', 'moe_bf', 'moe_w1', 'moe_w3', 'moe_w2']}
    for k in ['ev_w_in', 'ev_conv_w', 'ev_conv_b', 'ev_gate_a_w', 'ev_gate_a_b', 'ev_gate_x_w', 'ev_gate_x_b', 'ev_lru_lambda', 'ev_da_lambda', 'ev_da_subln', 'ev_w_out', 'od_w_out']:
        shared[k] = f(inputs[k])[0]
    shared.update(cst)
    x = f(inputs['x']); c = f(inputs['c']); ctx = f(inputs['ctx'])
    in_maps = []
    for b in range(8):
        m = dict(shared)
        m['x'] = x[b]; m['c'] = c[b]; m['ctx'] = ctx[b]
        in_maps.append(m)
    ncores = int(os.environ.get('KCORES', '8'))
    res = run_bass_kernel_spmd(nc, in_maps[:ncores], core_ids=list(range(ncores)))
    if dbg:
        return res
    return np.stack([np.asarray(r['out'], dtype=np.float32) for r in res.results], axis=0)
```

```python
import contextlib
import math
import os
import numpy as np
import ml_dtypes
import concourse.bass as bass
import concourse.mybir as mybir
from concourse.bass_utils import run_bass_kernel_spmd

F32 = mybir.dt.float32
BF16 = mybir.dt.bfloat16
I32 = mybir.dt.int32
AF = mybir.ActivationFunctionType
ALU = mybir.AluOpType
AX = mybir.AxisListType
ENGS = ['pe', 'act', 'dve', 'pool', 'sp']
NPOOL = 86
NBG = 4

D = 1024
SEQ = 4096
NCTX = 256
NTOK = SEQ + NCTX
ALPHA = (2.0 * 2) ** 0.25
EPS = 1e-6
BLK = 256
NBLK = 2 * SEQ // BLK + 32
NSLOT = NBLK * BLK
NB = 5
NT = SEQ // 128


class Res:
    __slots__ = ('name', 'writers', 'readers')

    def __init__(self, name):
        self.name = name
        self.writers = {}
        self.readers = {}


class Prog:
    def __init__(self, nc, st):
        self.nc = nc
        self.esem = {e: st.enter_context(nc.semaphore('se_' + e)) for e in ENGS}
        self.bar = st.enter_context(nc.semaphore('sbar'))
        self.psem = [st.enter_context(nc.semaphore('sp%d' % i)) for i in range(NPOOL + NBG)]
        self.bg = {}
        self.cnt = {('e', e): 0 for e in ENGS}
        for i in range(NPOOL + NBG):
            self.cnt[('p', i)] = 0
        self.bar_cnt = 0
        self.nres = 0
        self._reset_phase()
        self.waited = {e: {} for e in ENGS}

    def _reset_phase(self):
        self.ops = {e: [] for e in ENGS}
        self.res_sem = {}
        self.free = list(range(NPOOL))
        self.touched = set()

    def sem(self, k):
        return self.esem[k[1]] if k[0] == 'e' else self.psem[k[1]]

    def res(self, name=None):
        self.nres += 1
        return Res('%s#%d' % (name or 'r', self.nres))

    def op(self, eng, fn, reads=(), writes=(), dma=None, accum=(), bg=False):
        waits = {}
        isdma = dma is not None

        def need(sk, tok):
            waits[sk] = max(waits.get(sk, 0), tok[0])

        for r in reads:
            for sk, tok in r.writers.items():
                need(sk, tok)
        for w in list(writes) + list(accum):
            is_acc = any(w is a for a in accum)
            if not is_acc:
                for sk, tok in w.writers.items():
                    if (not isdma) and tok[2] == 'c' and tok[1] == eng:
                        continue
                    need(sk, tok)
            for sk, tok in w.readers.items():
                if (not isdma) and tok[2] == 'c' and tok[1] == eng:
                    continue
                need(sk, tok)
        if isdma and bg:
            if dma.name not in self.bg:
                self.bg[dma.name] = NPOOL + len(self.bg)
            sk = ('p', self.bg[dma.name])
        elif isdma:
            if dma.name not in self.res_sem:
                self.res_sem[dma.name] = self.free.pop(0)
            sk = ('p', self.res_sem[dma.name])
        if isdma:
            self.cnt[sk] += 16
            tok = (self.cnt[sk], eng, 'd')
            inc = 16
        else:
            sk = ('e', eng)
            self.cnt[sk] += 1
            tok = (self.cnt[sk], eng, 'c')
            inc = 1
        if not bg:
            self.touched.add(sk)
        wl = []
        wd = self.waited[eng]
        for k, v in waits.items():
            if wd.get(k, 0) >= v:
                continue
            wd[k] = v
            wl.append((k, v))
        self.ops[eng].append((fn, wl, sk, inc))
        for r in reads:
            r.readers[sk] = tok
        for w in writes:
            if any(w is a for a in accum):
                continue
            w.writers = {sk: tok}
            w.readers = {}
        for w in accum:
            w.writers[sk] = tok
        return tok

    def flush(self):
        nc = self.nc
        self.bar_cnt += 1
        barv = self.bar_cnt
        finals = [(k, self.cnt[k]) for k in sorted(self.touched)]
        ops = self.ops
        P = self

        def replay(e, key):
            for fn, wl, sk, inc in ops[key]:
                for k, v in wl:
                    e.wait_ge(P.sem(k), v)
                ins = fn(e)
                ins.then_inc(P.sem(sk), inc)
            if key == 'sp':
                for k, v in finals:
                    e.wait_ge(P.sem(k), v)
                e.sem_inc(P.bar, 1)
            else:
                e.wait_ge(P.bar, barv)

        with nc.Block() as block:
            @block.tensor
            def _(e):
                replay(e, 'pe')

            @block.scalar
            def _(e):
                replay(e, 'act')

            @block.vector
            def _(e):
                replay(e, 'dve')

            @block.gpsimd
            def _(e):
                replay(e, 'pool')

            @block.sync
            def _(e):
                replay(e, 'sp')
        bgk = set(('p', i) for i in self.bg.values())
        for e in ENGS:
            for k in self.cnt:
                if k in bgk:
                    continue
                self.waited[e][k] = self.cnt[k]
        self._reset_phase()


class T:
    def __init__(self, P, t, name):
        self.t = t
        self.r = P.res(name)

    def __getitem__(self, k):
        return self.t[k]


class Ctx:
    pass


def build(dbg=()):
    nc = bass.Bass("TRN2", target_bir_lowering=False)
    gst = contextlib.ExitStack()
    P = Prog(nc, gst)

    def din(name, shape, dt=F32):
        return nc.dram_tensor(name, list(shape), dt, kind="ExternalInput").ap()

    class DR:
        def __init__(self, name, shape, dt=F32, out=False):
            kind = "ExternalOutput" if (out or name in dbg) else "Internal"
            self.ap = nc.dram_tensor(name, list(shape), dt, kind=kind).ap()
            self.r = P.res(name)

    x_in = din("x", [SEQ, D]); c_in = din("c", [D]); ctx_in = din("ctx", [NCTX, D]); cctx_in = din("c_ctx", [D])
    ada_w = din("ada_w", [2, D, 6 * D]); ada_b = din("ada_b", [2, 6 * D])
    ln_g = din("ln_g", [2, 2, D]); ln_b = din("ln_b", [2, 2, D])
    w_in = din("ev_w_in", [D, 2560]); conv_w = din("ev_conv_w", [4, 512]); conv_b = din("ev_conv_b", [512])
    ga_w = din("ev_gate_a_w", [2, 8, 64, 64]); ga_b = din("ev_gate_a_b", [2, 512])
    gx_w = din("ev_gate_x_w", [2, 8, 64, 64]); gx_b = din("ev_gate_x_b", [2, 512])
    lru_lam = din("ev_lru_lambda", [2, 512]); da_lam = din("ev_da_lambda", [4, 64]); da_sub = din("ev_da_subln", [128])
    ev_w_out = din("ev_w_out", [D, D]); od_w_out = din("od_w_out", [D, D]); od_b_out = din("od_b_out", [D])
    moe_wg = din("moe_wg", [2, D, 4]); moe_bg = din("moe_bg", [2, 4]); moe_wf = din("moe_wf", [2, D, 32]); moe_bf = din("moe_bf", [2, 32])
    moe_w1 = din("moe_w1", [2, 32, D, 512]); moe_w3 = din("moe_w3", [2, 32, D, 512]); moe_w2 = din("moe_w2", [2, 32, 512, D])
    ropec = din("k_ropec", [128, SEQ]); ropes = din("k_ropes", [128, SEQ])
    k_cc = din("k_cc", [256, 256]); k_sc = din("k_sc", [256, 256])
    k_cn = din("k_cn", [32, 128, 32 * 128], BF16); k_sn = din("k_sn", [32, 128, 32 * 128], BF16)
    out_d = DR("out", [SEQ, D], F32, out=True)

    GG = DR("GG", [512, SEQ], BF16); XR = DR("XR", [512, NTOK]); QT = DR("QT", [512, SEQ], BF16)
    KT = DR("KT", [512, NTOK], BF16); VV = DR("VV", [NTOK, 512], BF16); MT = DR("MT", [D, SEQ], BF16)
    X1 = DR("X1", [SEQ, D]); X2 = DR("X2", [SEQ, D]); X3 = DR("X3", [SEQ, D])
    XG = DR("XG", [NSLOT, D], BF16); YG = DR("YG", [NSLOT, D])
    W1B = DR("W1B", [2 * 32 * 128, 4096], BF16); W3B = DR("W3B", [2 * 32 * 128, 4096], BF16); W2B = DR("W2B", [2 * 32 * 128, 4096], BF16)
    UU = DR("UU", [SEQ, D], BF16); VW = DR("VW", [SEQ, D], BF16); Y1 = DR("Y1", [SEQ, D])
    ADAS = DR("ADAS", [2, 4, D]); DBGI = DR("DBGI", [128, NBLK * 5], I32)

    def sbp(name, shape, dt=F32):
        return T(P, gst.enter_context(nc.sbuf_tensor(name, list(shape), dt)), name)

    ident = sbp("ident", [128, 128]); ones_f = sbp("ones_f", [128, 128]); ones_b = sbp("ones_b", [128, 128], BF16)
    ustrict = sbp("ustrict", [128, 128]); ustrict_b = sbp("ustrict_b", [128, 128], BF16); ident_b = sbp("ident_b", [128, 128], BF16)
    iota_p = sbp("iota_p", [128, 1])
    adac = sbp("adac", [128, 2, 4, 8])
    adap = sbp("adap", [128, 2, 2, 8])
    adacx = sbp("adacx", [128, 2, 8])
    gbc = sbp("gbc", [128, 2, 2, D])
    lamc = sbp("lamc", [128, 2])
    eps_c = sbp("eps_c", [128, 1])

    K = Ctx()

    def dma(q, out, in_, reads, writes, key, accum=(), slow=False):
        P.op(q, lambda e: e.dma_start(out=out, in_=in_, allow_slow_non_contiguous=slow), reads=reads, writes=writes, dma=key, accum=accum)

    conv_res = [P.res("conv0"), P.res("conv1")]
    w1v_ = moe_w1.rearrange("l e (p r) n -> (l e p) (r n)", p=128)
    w3v_ = moe_w3.rearrange("l e (p r) n -> (l e p) (r n)", p=128)
    w2v_ = moe_w2.rearrange("l e (p r) n -> (l e p) (r n)", p=128)
    conv_list = []
    for l_ in range(2):
        for g in range(8):
            for (src, dst) in ((w1v_, W1B), (w3v_, W3B), (w2v_, W2B)):
                conv_list.append((l_, g, src, dst))
    conv_pos = [0, 0]

    def conv_issue(l_, n):
        for _ in range(n):
            items = [c for c in conv_list if c[0] == l_]
            if conv_pos[l_] >= len(items):
                return
            (_, g, src, dst) = items[conv_pos[l_]]; conv_pos[l_] += 1
            r0 = l_ * 4096 + g * 512
            P.op('pool', lambda e, src=src, dst=dst, r0=r0: e.dma_start(out=dst.ap[r0:r0 + 512, :], in_=src[r0:r0 + 512, :]), reads=[], writes=[], dma=conv_res[l_], accum=[dst.r], bg=True)

    phno = [0]

    def phase(fn):
        phno[0] += 1
        pfx = "p%d_" % phno[0]
        with contextlib.ExitStack() as st:
            def sb(name, shape, dt=F32):
                return T(P, st.enter_context(nc.sbuf_tensor(pfx + name, list(shape), dt)), name)

            def ps(name, shape=(128, 512), dt=F32):
                return T(P, st.enter_context(nc.psum_tensor(pfx + name, list(shape), dt)), name)
            fn(sb, ps)
            P.flush()

    def ln_stats(xt_ap, stats, mv, rstd, nmr, reads):
        P.op('dve', lambda e: e.bn_stats(out=stats[:, 0:6], in_=xt_ap[:, 0:512]), reads=reads, writes=[stats.r])
        P.op('dve', lambda e: e.bn_stats(out=stats[:, 6:12], in_=xt_ap[:, 512:1024]), reads=reads, writes=[stats.r])
        P.op('dve', lambda e: e.bn_aggr(out=mv[:, :], in_=stats[:, :]), reads=[stats.r], writes=[mv.r])
        P.op('act', lambda e: e.activation(out=rstd[:, :], in_=mv[:, 1:2], func=AF.Sqrt, bias=eps_c[:, 0:1], scale=1.0), reads=[mv.r, eps_c.r], writes=[rstd.r])
        P.op('dve', lambda e: e.reciprocal(out=rstd[:, :], in_=rstd[:, :]), reads=[rstd.r], writes=[rstd.r])
        P.op('dve', lambda e: e.scalar_tensor_tensor(out=nmr[:, :], in0=mv[:, 0:1], scalar=-1.0, in1=rstd[:, :], op0=ALU.mult, op1=ALU.mult), reads=[mv.r, rstd.r], writes=[nmr.r])

    def ph_prologue(sb, ps):
        P.op('pool', lambda e: e.memset(ident[:, :], 0.0), writes=[ident.r])
        P.op('pool', lambda e: e.affine_select(out=ident[:, :], in_=ident[:, :], pattern=[[-1, 128]], compare_op=ALU.not_equal, fill=1.0, base=0, channel_multiplier=1), reads=[ident.r], writes=[ident.r])
        P.op('pool', lambda e: e.memset(ones_f[:, :], 1.0), writes=[ones_f.r])
        P.op('pool', lambda e: e.memset(ones_b[:, :], 1.0), writes=[ones_b.r])
        P.op('pool', lambda e: e.memset(eps_c[:, :], EPS), writes=[eps_c.r])
        P.op('pool', lambda e: e.memset(ustrict[:, :], 1.0), writes=[ustrict.r])
        P.op('pool', lambda e: e.affine_select(out=ustrict[:, :], in_=ustrict[:, :], pattern=[[1, 128]], compare_op=ALU.is_gt, fill=0.0, base=0, channel_multiplier=-1), reads=[ustrict.r], writes=[ustrict.r])
        P.op('pool', lambda e: e.tensor_copy(out=ustrict_b[:, :], in_=ustrict[:, :]), reads=[ustrict.r], writes=[ustrict_b.r])
        P.op('pool', lambda e: e.tensor_copy(out=ident_b[:, :], in_=ident[:, :]), reads=[ident.r], writes=[ident_b.r])
        P.op('pool', lambda e: e.iota(iota_p[:, :], pattern=[[0, 1]], base=0, channel_multiplier=1, allow_small_or_imprecise_dtypes=True), writes=[iota_p.r])
        cc = sb("cc", [128, 2, 8]); sc = sb("sc", [128, 8, 2]); srep = sb("srep", [128, 8, 128])
        dma('sp', cc[:, 0, :], c_in.rearrange("(k p) -> p k", p=128), [], [cc.r], cc.r, slow=True)
        dma('sp', cc[:, 1, :], cctx_in.rearrange("(k p) -> p k", p=128), [], [cc.r], cc.r, slow=True)
        P.op('act', lambda e: e.activation(out=sc[:, :, :].rearrange("p k t -> p t k"), in_=cc[:, :, :], func=AF.Silu), reads=[cc.r], writes=[sc.r])
        for kc in range(8):
            P.op('dve', lambda e, kc=kc: e.tensor_scalar(out=srep[:, kc, :], in0=ones_f[:, :], scalar1=sc[:, kc, 0:1], scalar2=None, op0=ALU.mult), reads=[sc.r, ones_f.r], writes=[srep.r])
        wsl = [sb("wsl%d" % i, [128, 8, D]) for i in range(2)]
        badd = sb("badd", [128, 2, 6, 8]); bbc = sb("bbc", [128, D])
        dma('sp', badd[:, :, :, :], ada_b.rearrange("l (t n p) -> p l t n", p=128, n=8), [], [badd.r], badd.r, slow=True)
        pc = ps("pc", [128, 512]); pb = [ps("pb%d" % i, [128, 512]) for i in range(2)]
        colmap = {0: 0, 1: 1, 3: 2, 4: 3}
        i = 0
        for l in range(2):
            for term in range(6):
                w = wsl[i % 2]; i += 1
                dma('sp', w[:, :, :], ada_w[l, :, term * D:(term + 1) * D].rearrange("(k p) n -> p k n", p=128), [], [w.r], w.r)
                if term in colmap:
                    ci = colmap[term]
                    for n in range(8):
                        for kc in range(8):
                            P.op('pe', lambda e, w=w, n=n, kc=kc: e.matmul(pc[:, n * 2:n * 2 + 2], lhsT=w[:, kc, n * 128:(n + 1) * 128], rhs=sc[:, kc, :], start=(kc == 0), stop=(kc == 7)),
                                 reads=[w.r, sc.r], writes=[pc.r])
                    addc = 1.0 if term in (1, 4) else 0.0
                    P.op('dve', lambda e, l=l, term=term, ci=ci, addc=addc: e.scalar_tensor_tensor(out=adac[:, l, ci, :], in0=pc[:, 0:16:2], scalar=addc, in1=badd[:, l, term, :], op0=ALU.add, op1=ALU.add),
                         reads=[pc.r, badd.r], writes=[adac.r])
                    if l == 0 and term in (0, 1):
                        P.op('dve', lambda e, term=term, addc=addc: e.scalar_tensor_tensor(out=adacx[:, term, :], in0=pc[:, 1:16:2], scalar=addc, in1=badd[:, 0, term, :], op0=ALU.add, op1=ALU.add),
                             reads=[pc.r, badd.r], writes=[adacx.r])
                else:
                    gi = 0 if term == 2 else 1
                    dma('sp', bbc[:, :], ada_b[l, term * D:(term + 1) * D].partition_broadcast(128), [], [bbc.r], bbc.r)
                    for h in range(2):
                        for kc in range(8):
                            P.op('pe', lambda e, w=w, h=h, kc=kc: e.matmul(pb[h][:, :], lhsT=srep[:, kc, :], rhs=w[:, kc, h * 512:(h + 1) * 512], start=(kc == 0), stop=(kc == 7)),
                                 reads=[w.r, srep.r], writes=[pb[h].r])
                        P.op('dve', lambda e, l=l, gi=gi, h=h: e.tensor_tensor(out=gbc[:, l, gi, h * 512:(h + 1) * 512], in0=pb[h][:, :], in1=bbc[:, h * 512:(h + 1) * 512], op=ALU.add),
                             reads=[pb[h].r, bbc.r], writes=[gbc.r])
        for l in range(2):
            dma('sp', ADAS.ap[l].rearrange("t (n p) -> p t n", p=128), adac[:, l, :, :], [adac.r], [], adac.r, accum=[ADAS.r], slow=True)
        for l in range(2):
            dma('sp', adap[:, l, :, :], ADAS.ap[l, 2:4, :].rearrange("t (p n) -> p t n", n=8), [ADAS.r], [adap.r], adap.r, slow=True)
        dl = sb("dl", [1, 4, 64]); dp = sb("dp", [1, 2, 64]); dsum = sb("dsum", [1, 2]); lv = sb("lv", [1, 2])
        dma('sp', dl[:, :, :], da_lam.rearrange("(o a) d -> o a d", o=1), [], [dl.r], dl.r)
        P.op('dve', lambda e: e.tensor_tensor(out=dp[:, :, :], in0=dl[:, 0:4:2, :], in1=dl[:, 1:4:2, :], op=ALU.mult), reads=[dl.r], writes=[dp.r])
        P.op('dve', lambda e: e.reduce_sum(out=dsum[:, :], in_=dp[:, :, :], axis=AX.X), reads=[dp.r], writes=[dsum.r])
        P.op('act', lambda e: e.activation(out=dsum[:, :], in_=dsum[:, :], func=AF.Exp), reads=[dsum.r], writes=[dsum.r])
        P.op('dve', lambda e: e.scalar_tensor_tensor(out=lv[:, 1:2], in0=dsum[:, 0:1], scalar=0.2, in1=dsum[:, 1:2], op0=ALU.add, op1=ALU.subtract), reads=[dsum.r], writes=[lv.r])
        P.op('dve', lambda e: e.tensor_scalar(out=lv[:, 0:1], in0=lv[:, 1:2], scalar1=-1.0, scalar2=None, op0=ALU.mult), reads=[lv.r], writes=[lv.r])
        P.op('pe', lambda e: e.matmul(pc[:, 32:34], lhsT=ones_f[0:1, :], rhs=lv[:, :], start=True, stop=True), reads=[ones_f.r, lv.r], writes=[pc.r])
        P.op('dve', lambda e: e.tensor_copy(out=lamc[:, :], in_=pc[:, 32:34]), reads=[pc.r], writes=[lamc.r])

    phase(ph_prologue)

    def ph_s1(sb, ps):
        wb = sb("wb", [128, 8, 2560], BF16); wp = sb("wp", [128, 8, 1024], BF16)
        wst = [sb("wst%d" % i, [128, 8, 512]) for i in range(2)]
        for ch in range(5):
            w = wst[ch % 2]
            dma('sp', w[:, :, :], w_in[:, ch * 512:(ch + 1) * 512].rearrange("(k p) n -> p k n", p=128), [], [w.r], w.r)
            P.op('pool', lambda e, w=w, ch=ch: e.tensor_copy(out=wb[:, :, ch * 512:(ch + 1) * 512], in_=w[:, :, :]), reads=[w.r], writes=[wb.r])
        for kc in range(8):
            src = wb[:, kc, 1024:2048].rearrange("p (b h f) -> p b h f", h=2, f=16)
            dst = wp[:, kc, :].rearrange("p (b h f) -> p b h f", h=2, f=16)
            P.op('pool', lambda e, src=src, dst=dst: e.tensor_copy(out=dst[:, :, 0, :], in_=src[:, :, 1, :]), reads=[wb.r], writes=[wp.r])
            P.op('pool', lambda e, src=src, dst=dst: e.tensor_copy(out=dst[:, :, 1, :], in_=src[:, :, 0, :]), reads=[wb.r], writes=[wp.r])
        xt = [sb("xt%d" % i, [128, D]) for i in range(3)]
        xn = sb("xn", [128, 4, D])
        hT = [sb("hT%d" % i, [128, 8, 512], BF16) for i in range(2)]
        rc = [sb("rc%d" % i, [128, 512]) for i in range(2)]; rs = [sb("rs%d" % i, [128, 512]) for i in range(2)]
        stats = sb("stats", [128, 12]); mv = sb("mv", [128, 2]); rstd = sb("rstd", [128, 1]); nmr = sb("nmr", [128, 1])
        ogg = [sb("ogg%d" % i, [128, 512], BF16) for i in range(2)]
        oxr = [sb("oxr%d" % i, [128, 512]) for i in range(2)]
        oqk = [sb("oqk%d" % i, [128, 512], BF16) for i in range(2)]
        t1 = sb("t1", [128, 512]); t2 = sb("t2", [128, 512])
        ov = [sb("ov%d" % i, [128, 512], BF16) for i in range(2)]
        pT = [ps("pT%d" % i) for i in range(2)]
        pM = [ps("pM%d" % i) for i in range(4)]
        cnt = {'x': 0, 'm': 0, 'gg': 0, 'xr': 0, 'qk': 0, 'v': 0}

        def nxt(k, n):
            v = cnt[k]; cnt[k] += 1
            return v % n

        for s in range(9):
            isctx = (s == 0)
            ntile = 2 if isctx else 4
            ntk = ntile * 128
            tok0 = 0 if isctx else NCTX + (s - 1) * 512
            lat0 = (s - 1) * 512
            h = hT[s % 2]
            for j in range(ntile):
                x = xt[nxt('x', 3)]
                src = ctx_in[j * 128:(j + 1) * 128, :] if isctx else x_in[lat0 + j * 128: lat0 + (j + 1) * 128, :]
                dma('sp', x[:, :], src, [], [x.r], x.r)
                ln_stats(x, stats, mv, rstd, nmr, [x.r])
                P.op('act', lambda e, x=x, j=j: e.activation(out=xn[:, j, :], in_=x[:, :], func=AF.Identity, scale=rstd[:, 0:1], bias=nmr[:, 0:1]), reads=[x.r, rstd.r, nmr.r], writes=[xn.r])
            for kc in range(8):
                p = pT[kc % 2]
                for j in range(ntile):
                    P.op('pe', lambda e, p=p, j=j, kc=kc: e.transpose(p[:, j * 128:(j + 1) * 128], xn[:, j, kc * 128:(kc + 1) * 128], ident[:, :]), reads=[xn.r, ident.r], writes=[p.r])
                scl = adacx[:, 1, kc:kc + 1] if isctx else adac[:, 0, 1, kc:kc + 1]
                bia = adacx[:, 0, kc:kc + 1] if isctx else adac[:, 0, 0, kc:kc + 1]
                P.op('act', lambda e, p=p, kc=kc, scl=scl, bia=bia, h=h, ntk=ntk: e.activation(out=h[:, kc, 0:ntk], in_=p[:, 0:ntk], func=AF.Identity, scale=scl, bias=bia),
                     reads=[p.r, adac.r, adacx.r], writes=[h.r])
            if not isctx:
                rcb = rc[s % 2]; rsb = rs[s % 2]
                dma('sp', rcb[:, :], ropec[:, lat0:lat0 + 512], [], [rcb.r], rcb.r)
                dma('sp', rsb[:, :], ropes[:, lat0:lat0 + 512], [], [rsb.r], rsb.r)

            def mm_feat(pm, wt, c0, h=h, ntk=ntk):
                for kc in range(8):
                    P.op('pe', lambda e, kc=kc: e.matmul(pm[:, 0:ntk], lhsT=wt[:, kc, c0:c0 + 128], rhs=h[:, kc, 0:ntk], start=(kc == 0), stop=(kc == 7)), reads=[wt.r, h.r], writes=[pm.r])

            if not isctx:
                for c4 in range(4):
                    pm = pM[nxt('m', 4)]
                    mm_feat(pm, wb, c4 * 128)
                    o = ogg[nxt('gg', 2)]
                    P.op('act', lambda e, pm=pm, o=o: e.activation(out=o[:, :], in_=pm[:, :], func=AF.Gelu_apprx_tanh), reads=[pm.r], writes=[o.r])
                    dma('act', GG.ap[c4 * 128:(c4 + 1) * 128, lat0:lat0 + 512], o[:, :], [o.r], [], o.r, accum=[GG.r])
            for c4 in range(4):
                pm = pM[nxt('m', 4)]
                mm_feat(pm, wb, 512 + c4 * 128)
                o = oxr[nxt('xr', 2)]
                P.op('act', lambda e, pm=pm, o=o, ntk=ntk: e.copy(out=o[:, 0:ntk], in_=pm[:, 0:ntk]), reads=[pm.r], writes=[o.r])
                dma('act', XR.ap[c4 * 128:(c4 + 1) * 128, tok0:tok0 + ntk], o[:, 0:ntk], [o.r], [], o.r, accum=[XR.r])
            for which in range(2):
                if which == 0 and isctx:
                    continue
                for hd in range(4):
                    c0 = 1024 + which * 512 + hd * 128
                    pm = pM[nxt('m', 4)]
                    mm_feat(pm, wb, c0)
                    o = oqk[nxt('qk', 2)]
                    if isctx:
                        P.op('dve', lambda e, pm=pm, o=o, ntk=ntk: e.tensor_copy(out=o[:, 0:ntk], in_=pm[:, 0:ntk]), reads=[pm.r], writes=[o.r])
                    else:
                        pm2 = pM[nxt('m', 4)]
                        mm_feat(pm2, wp, which * 512 + hd * 128)
                        P.op('dve', lambda e, pm=pm, rcb=rcb: e.tensor_tensor(out=t1[:, :], in0=pm[:, :], in1=rcb[:, :], op=ALU.mult), reads=[pm.r, rcb.r], writes=[t1.r])
                        P.op('dve', lambda e, pm2=pm2, rsb=rsb: e.tensor_tensor(out=t2[:, :], in0=pm2[:, :], in1=rsb[:, :], op=ALU.mult), reads=[pm2.r, rsb.r], writes=[t2.r])
                        P.op('pool', lambda e, o=o: e.tensor_tensor(out=o[:, :], in0=t1[:, :], in1=t2[:, :], op=ALU.add), reads=[t1.r, t2.r], writes=[o.r])
                    if which == 0:
                        dma('pool', QT.ap[hd * 128:(hd + 1) * 128, lat0:lat0 + 512], o[:, :], [o.r], [], o.r, accum=[QT.r])
                    else:
                        dma('pool', KT.ap[hd * 128:(hd + 1) * 128, tok0:tok0 + ntk], o[:, 0:ntk], [o.r], [], o.r, accum=[KT.r])
            for j in range(ntile):
                pm = pM[nxt('m', 4)]
                for kc in range(8):
                    P.op('pe', lambda e, pm=pm, kc=kc, j=j, h=h: e.matmul(pm[:, :], lhsT=h[:, kc, j * 128:(j + 1) * 128], rhs=wb[:, kc, 2048:2560], start=(kc == 0), stop=(kc == 7)), reads=[wb.r, h.r], writes=[pm.r])
                o = ov[nxt('v', 2)]
                P.op('act', lambda e, pm=pm, o=o: e.copy(out=o[:, :], in_=pm[:, :]), reads=[pm.r], writes=[o.r])
                dma('act', VV.ap[tok0 + j * 128: tok0 + (j + 1) * 128, :], o[:, :], [o.r], [], o.r, accum=[VV.r])

    phase(ph_s1)
    if 's1' in dbg:
        return nc, gst

    def ph_s2(sb, ps):
        LB = NTOK + 6
        xr = sb("xr", [128, LB]); xc = sb("xc", [128, NTOK]); rr = sb("rr", [128, NTOK]); ii = sb("ii", [128, NTOK])
        a2 = sb("a2", [128, NTOK]); hf = sb("hf", [128, NTOK]); hb = sb("hb", [128, NTOK])
        xcb = sb("xcb", [128, NTOK], BF16); gg = sb("gg", [128, SEQ], BF16); yo = sb("yo", [128, SEQ], BF16)
        cw = sb("cw", [128, 4, 4]); cb = sb("cb", [128, 4]); gba = sb("gba", [128, 2, 4]); gbx = sb("gbx", [128, 2, 4])
        lam = sb("lam", [128, 2, 4]); cA = sb("cA", [128, 2, 4]); cA2 = sb("cA2", [128, 2, 4])
        wgs = sb("wgs", [128, 16, 128]); wg = sb("wg", [128, 16, 128], BF16)
        pm = [ps("pm%d" % i) for i in range(4)]
        P.op('pool', lambda e: e.memset(xr[:, :], 0.0), writes=[xr.r])
        P.op('pool', lambda e: e.memset(wgs[:, :, :], 0.0), writes=[wgs.r])
        for k in range(4):
            dma('sp', cw[:, :, k], conv_w[k].rearrange("(c p) -> p c", p=128), [], [], cw.r, accum=[cw.r], slow=True)
        dma('sp', cb[:, :], conv_b.rearrange("(c p) -> p c", p=128), [], [cb.r], cb.r, slow=True)
        dma('sp', gba[:, :, :], ga_b.rearrange("d (c p) -> p d c", p=128), [], [gba.r], gba.r, slow=True)
        dma('sp', gbx[:, :, :], gx_b.rearrange("d (c p) -> p d c", p=128), [], [gbx.r], gbx.r, slow=True)
        dma('sp', lam[:, :, :], lru_lam.rearrange("d (c p) -> p d c", p=128), [], [lam.r], lam.r, slow=True)
        for typ, gw in enumerate((ga_w, gx_w)):
            for d in range(2):
                for cc in range(4):
                    mi = (typ * 2 + d) * 4 + cc
                    for hh in range(2):
                        dma('sp', wgs[hh * 64:(hh + 1) * 64, mi, hh * 64:(hh + 1) * 64], gw[d, 2 * cc + hh], [wgs.r], [], wgs.r, accum=[wgs.r])
        P.op('pool', lambda e: e.tensor_copy(out=wg[:, :, :], in_=wgs[:, :, :]), reads=[wgs.r], writes=[wg.r])
        P.op('act', lambda e: e.activation(out=cA[:, :, :], in_=lam[:, :, :], func=AF.Exp, scale=-1.0), reads=[lam.r], writes=[cA.r])
        P.op('act', lambda e: e.activation(out=cA[:, :, :], in_=cA[:, :, :], func=AF.Ln, bias=ones_f[:, 0:1], scale=1.0), reads=[cA.r, ones_f.r], writes=[cA.r])
        P.op('dve', lambda e: e.tensor_scalar(out=cA2[:, :, :], in0=cA[:, :, :], scalar1=-16.0, scalar2=None, op0=ALU.mult), reads=[cA.r], writes=[cA2.r])
        P.op('dve', lambda e: e.tensor_scalar(out=cA[:, :, :], in0=cA[:, :, :], scalar1=-8.0, scalar2=None, op0=ALU.mult), reads=[cA.r], writes=[cA.r])
        segs = [(0, NCTX, 0), (NCTX, SEQ, 259)]
        chunks = [(c0, min(512, NTOK - c0)) for c0 in range(0, NTOK, 512)]
        mcnt = [0]
        for cc in range(4):
            conv_issue(0, 6)
            dma('sp', xr[:, 2:2 + NCTX], XR.ap[cc * 128:(cc + 1) * 128, 0:NCTX], [XR.r], [xr.r], xr.r)
            dma('sp', xr[:, 261:261 + SEQ], XR.ap[cc * 128:(cc + 1) * 128, NCTX:NTOK], [XR.r], [], xr.r, accum=[xr.r])
            dma('sp', gg[:, :], GG.ap[cc * 128:(cc + 1) * 128, :], [GG.r], [gg.r], gg.r)
            for (o0, ln, b0) in segs:
                P.op('dve', lambda e, o0=o0, ln=ln, b0=b0, cc=cc: e.tensor_scalar(out=xc[:, o0:o0 + ln], in0=xr[:, b0:b0 + ln], scalar1=cw[:, cc, 0:1], scalar2=cb[:, cc:cc + 1], op0=ALU.mult, op1=ALU.add),
                     reads=[xr.r, cw.r, cb.r], writes=[xc.r])
                for k in range(1, 4):
                    P.op('dve', lambda e, o0=o0, ln=ln, b0=b0, cc=cc, k=k: e.scalar_tensor_tensor(out=xc[:, o0:o0 + ln], in0=xr[:, b0 + k:b0 + k + ln], scalar=cw[:, cc, k:k + 1], in1=xc[:, o0:o0 + ln], op0=ALU.mult, op1=ALU.add),
                         reads=[xr.r, cw.r, xc.r], writes=[xc.r])
            P.op('pool', lambda e: e.tensor_copy(out=xcb[:, :], in_=xc[:, :]), reads=[xc.r], writes=[xcb.r])
            for d in range(2):
                for (c0, cl) in chunks:
                    for typ, dst, bb in ((0, rr, gba), (1, ii, gbx)):
                        p = pm[mcnt[0] % 4]; mcnt[0] += 1
                        mi = (typ * 2 + d) * 4 + cc
                        P.op('pe', lambda e, p=p, mi=mi, c0=c0, cl=cl: e.matmul(p[:, 0:cl], lhsT=wg[:, mi, :], rhs=xcb[:, c0:c0 + cl], start=True, stop=True), reads=[wg.r, xcb.r], writes=[p.r])
                        P.op('act', lambda e, p=p, dst=dst, bb=bb, c0=c0, cl=cl, d=d, cc=cc: e.activation(out=dst[:, c0:c0 + cl], in_=p[:, 0:cl], func=AF.Sigmoid, bias=bb[:, d, cc:cc + 1], scale=1.0),
                             reads=[p.r, bb.r], writes=[dst.r])
                P.op('act', lambda e, d=d, cc=cc: e.activation(out=a2[:, :], in_=rr[:, :], func=AF.Exp, scale=cA2[:, d, cc:cc + 1]), reads=[rr.r, cA2.r], writes=[a2.r])
                P.op('act', lambda e, d=d, cc=cc: e.activation(out=rr[:, :], in_=rr[:, :], func=AF.Exp, scale=cA[:, d, cc:cc + 1]), reads=[rr.r, cA.r], writes=[rr.r])
                P.op('dve', lambda e: e.tensor_scalar(out=a2[:, :], in0=a2[:, :], scalar1=-1.0, scalar2=1.0, op0=ALU.mult, op1=ALU.add), reads=[a2.r], writes=[a2.r])
                P.op('act', lambda e: e.activation(out=a2[:, :], in_=a2[:, :], func=AF.Sqrt), reads=[a2.r], writes=[a2.r])
                P.op('dve', lambda e: e.tensor_tensor(out=a2[:, :], in0=a2[:, :], in1=ii[:, :], op=ALU.mult), reads=[a2.r, ii.r], writes=[a2.r])
                P.op('dve', lambda e: e.tensor_tensor(out=a2[:, :], in0=a2[:, :], in1=xc[:, :], op=ALU.mult), reads=[a2.r, xc.r], writes=[a2.r])
                if d == 0:
                    P.op('dve', lambda e: e.tensor_tensor_scan(out=hf[:, :], data0=rr[:, :], data1=a2[:, :], initial=0.0, op0=ALU.mult, op1=ALU.add), reads=[rr.r, a2.r], writes=[hf.r])
                else:
                    P.op('dve', lambda e: e.tensor_tensor_scan(out=hb[:, NCTX - 1::-1], data0=rr[:, NCTX - 1::-1], data1=a2[:, NCTX - 1::-1], initial=0.0, op0=ALU.mult, op1=ALU.add), reads=[rr.r, a2.r], writes=[hb.r])
                    P.op('dve', lambda e: e.tensor_tensor_scan(out=hb[:, NTOK - 1:NCTX - 1:-1], data0=rr[:, NTOK - 1:NCTX - 1:-1], data1=a2[:, NTOK - 1:NCTX - 1:-1], initial=hb[:, 0:1], op0=ALU.mult, op1=ALU.add),
                         reads=[rr.r, a2.r, hb.r], writes=[hb.r])
            P.op('pool', lambda e: e.tensor_tensor(out=hf[:, NCTX:NTOK], in0=hf[:, NCTX:NTOK], in1=hb[:, NCTX:NTOK], op=ALU.add), reads=[hf.r, hb.r], writes=[hf.r])
            P.op('pool', lambda e: e.tensor_tensor(out=yo[:, :], in0=hf[:, NCTX:NTOK], in1=gg[:, :], op=ALU.mult), reads=[hf.r, gg.r], writes=[yo.r])
            dma('pool', MT.ap[cc * 128:(cc + 1) * 128, :], yo[:, :], [yo.r], [], yo.r, accum=[MT.r])

    phase(ph_s2)

    def ph_s3(sb, ps):
        NKT = NTOK // 128
        vt = sb("vt", [128, NKT, 512], BF16)
        kt_ = [sb("kt%d" % i, [128, NTOK], BF16) for i in range(2)]
        qt_ = [sb("qt%d" % i, [128, SEQ], BF16) for i in range(2)]
        pt = [sb("pt%d" % i, [128, 1024], BF16) for i in range(2)]
        r1 = sb("r1", [128, 512]); o1 = sb("o1", [128, 512]); r2 = sb("r2", [128, 512]); o2 = sb("o2", [128, 512])
        sq = sb("sq", [128, 512], BF16); rsd = sb("rsd", [128, 512]); ob = [sb("ob%d" % i, [128, 512], BF16) for i in range(2)]
        g08 = sb("g08", [128, 1])
        psS = [ps("psS%d" % i, [128, 1024]) for i in range(2)]
        psO = [ps("psO%d" % i) for i in range(2)]; psD = [ps("psD%d" % i) for i in range(2)]
        accA = [sb("accA%d" % i, [128, 512]) for i in range(2)]
        dsb = [sb("dsb%d" % i, [128, 512]) for i in range(2)]
        dma('sp', g08[:, :], da_sub.rearrange("(p o) -> p o", o=1), [], [g08.r], g08.r, slow=True)
        P.op('dve', lambda e: e.tensor_scalar(out=g08[:, :], in0=g08[:, :], scalar1=0.8, scalar2=None, op0=ALU.mult), reads=[g08.r], writes=[g08.r])
        dma('sp', vt[:, :, :], VV.ap.rearrange("(t p) n -> p t n", p=128), [VV.r], [vt.r], vt.r)
        oc = [0]
        for hd in range(4):
            kt = kt_[hd % 2]; qt = qt_[hd % 2]
            dma('sp', kt[:, :], KT.ap[hd * 128:(hd + 1) * 128, :], [KT.r], [kt.r], kt.r)
            dma('sp', qt[:, :], QT.ap[hd * 128:(hd + 1) * 128, :], [QT.r], [qt.r], qt.r)
            for qc in range(8):
                q0 = qc * 512

                def qk(t, kt=kt, qt=qt, q0=q0):
                    S = psS[t % 2]
                    for i in range(2):
                        P.op('pe', lambda e, S=S, i=i, t=t: e.matmul(S[:, i * 512:(i + 1) * 512], lhsT=kt[i * 64:(i + 1) * 64, t * 128:(t + 1) * 128], rhs=qt[i * 64:(i + 1) * 64, q0:q0 + 512], start=True, stop=True),
                             reads=[kt.r, qt.r], writes=[S.r])
                qk(0)
                for t in range(NKT):
                    S = psS[t % 2]; p = pt[t % 2]
                    P.op('act', lambda e, S=S, p=p: e.activation(out=p[:, :], in_=S[:, :], func=AF.Exp, scale=0.125), reads=[S.r], writes=[p.r])
                    if t + 1 < NKT:
                        qk(t + 1)
                    for i in range(2):
                        P.op('pe', lambda e, p=p, i=i, t=t, hd=hd: e.matmul(psO[i][:, :], lhsT=vt[:, t, hd * 128:(hd + 1) * 128], rhs=p[:, i * 512:(i + 1) * 512], start=(t == 0), stop=(t == NKT - 1)),
                             reads=[vt.r, p.r], writes=[psO[i].r])
                    P.op('pe', lambda e, p=p, t=t: e.matmul(psD[1][:, :], lhsT=ones_b[:, :], rhs=p[:, 512:1024], start=(t == 0), stop=(t == NKT - 1)), reads=[ones_b.r, p.r], writes=[psD[1].r])
                    for (eng, ac, c0, c1) in (('dve', accA[qc % 2], 0, 512),):
                        if t == 0:
                            P.op(eng, lambda e, p=p, ac=ac, c0=c0, c1=c1: e.tensor_copy(out=ac[:, :], in_=p[:, c0:c1]), reads=[p.r], writes=[ac.r])
                        else:
                            P.op(eng, lambda e, p=p, ac=ac, c0=c0, c1=c1: e.tensor_tensor(out=ac[:, :], in0=ac[:, :], in1=p[:, c0:c1], op=ALU.add), reads=[p.r, ac.r], writes=[ac.r])
                aA = accA[qc % 2]
                P.op('pe', lambda e, aA=aA: e.matmul(psD[0][:, :], lhsT=ones_f[:, :], rhs=aA[:, 0:512], start=True, stop=True), reads=[ones_f.r, aA.r], writes=[psD[0].r])
                for i in range(2):
                    P.op('act', lambda e, i=i: e.activation(out=dsb[i][:, :], in_=psD[i][:, :], func=AF.Ln), reads=[psD[i].r], writes=[dsb[i].r])
                    P.op('act', lambda e, i=i: e.activation(out=dsb[i][:, :], in_=dsb[i][:, :], func=AF.Exp, scale=-1.0), reads=[dsb[i].r], writes=[dsb[i].r])
                P.op('dve', lambda e: e.tensor_tensor(out=o1[:, :], in0=psO[0][:, :], in1=dsb[0][:, :], op=ALU.mult), reads=[psO[0].r, dsb[0].r], writes=[o1.r])
                P.op('dve', lambda e: e.tensor_tensor(out=o2[:, :], in0=psO[1][:, :], in1=dsb[1][:, :], op=ALU.mult), reads=[psO[1].r, dsb[1].r], writes=[o2.r])
                P.op('dve', lambda e: e.scalar_tensor_tensor(out=o1[:, :], in0=o2[:, :], scalar=lamc[:, 0:1], in1=o1[:, :], op0=ALU.mult, op1=ALU.add), reads=[o2.r, o1.r, lamc.r], writes=[o1.r])
                P.op('pool', lambda e: e.tensor_tensor(out=sq[:, :], in0=o1[:, :], in1=o1[:, :], op=ALU.mult), reads=[o1.r], writes=[sq.r])
                S = psS[0]
                P.op('pe', lambda e, S=S: e.matmul(S[:, 0:512], lhsT=ones_b[:, :], rhs=sq[:, :], start=True, stop=True), reads=[ones_b.r, sq.r], writes=[S.r])
                P.op('act', lambda e, S=S: e.activation(out=rsd[:, :], in_=S[:, 0:512], func=AF.Ln, bias=eps_c[:, 0:1], scale=1.0 / 128.0), reads=[S.r, eps_c.r], writes=[rsd.r])
                P.op('act', lambda e: e.activation(out=rsd[:, :], in_=rsd[:, :], func=AF.Exp, scale=-0.5), reads=[rsd.r], writes=[rsd.r])
                P.op('dve', lambda e: e.tensor_tensor(out=o1[:, :], in0=o1[:, :], in1=rsd[:, :], op=ALU.mult), reads=[o1.r, rsd.r], writes=[o1.r])
                o = ob[oc[0] % 2]; oc[0] += 1
                P.op('act', lambda e, o=o: e.activation(out=o[:, :], in_=o1[:, :], func=AF.Identity, scale=g08[:, 0:1]), reads=[o1.r, g08.r], writes=[o.r])
                dma('act', MT.ap[512 + hd * 128:512 + (hd + 1) * 128, q0:q0 + 512], o[:, :], [o.r], [], o.r, accum=[MT.r])

    phase(ph_s3)

    def load_bc(sb, l, j):
        lg = sb("lgbc", [128, D]); lb = sb("lbbc", [128, D])
        dma('sp', lg[:, :], ln_g[l, j].partition_broadcast(128), [], [lg.r], lg.r)
        dma('sp', lb[:, :], ln_b[l, j].partition_broadcast(128), [], [lb.r], lb.r)
        return lg, lb

    def resid_A(x, t, z, lnt, l, gi):
        stats, mv, rstd, nmr = lnt
        P.op('dve', lambda e: e.tensor_tensor(out=t[:, :], in0=t[:, :], in1=gbc[:, l, gi, :], op=ALU.mult), reads=[t.r, gbc.r], writes=[t.r])
        P.op('dve', lambda e: e.scalar_tensor_tensor(out=z[:, :], in0=x[:, :], scalar=ALPHA, in1=t[:, :], op0=ALU.mult, op1=ALU.add), reads=[x.r, t.r], writes=[z.r])
        ln_stats(z, stats, mv, rstd, nmr, [z.r])

    def resid_B(z, lnt, lg, lb, o):
        stats, mv, rstd, nmr = lnt
        P.op('act', lambda e: e.activation(out=z[:, :], in_=z[:, :], func=AF.Identity, scale=rstd[:, 0:1], bias=nmr[:, 0:1]), reads=[z.r, rstd.r, nmr.r], writes=[z.r])
        P.op('pool', lambda e: e.tensor_tensor(out=z[:, :], in0=z[:, :], in1=lg[:, :], op=ALU.mult), reads=[z.r, lg.r], writes=[z.r])
        P.op('dve', lambda e: e.tensor_tensor(out=o[:, :], in0=z[:, :], in1=lb[:, :], op=ALU.add), reads=[z.r, lb.r], writes=[o.r])

    def ph_s4(sb, ps):
        wo = sb("wo", [128, 8, D], BF16)
        wst = [sb("wst%d" % i, [128, 8, 512]) for i in range(2)]
        for ch in range(2):
            w = wst[ch]
            dma('sp', w[:, :, :], ev_w_out[:, ch * 512:(ch + 1) * 512].rearrange("(k p) n -> p k n", p=128), [], [w.r], w.r)
            P.op('pool', lambda e, w=w, ch=ch: e.tensor_copy(out=wo[:, :, ch * 512:(ch + 1) * 512], in_=w[:, :, :]), reads=[w.r], writes=[wo.r])
        lg, lb = load_bc(sb, 0, 0)
        mt = [sb("mt%d" % i, [128, 8, 512], BF16) for i in range(2)]
        xt = [sb("xt%d" % i, [128, D]) for i in range(NB)]; tt = [sb("tt%d" % i, [128, D]) for i in range(NB)]
        zz = [sb("zz%d" % i, [128, D]) for i in range(NB)]; oo = [sb("oo%d" % i, [128, D]) for i in range(NB)]
        lnts = [(sb("stats%d" % i, [128, 12]), sb("mv%d" % i, [128, 2]), sb("rstd%d" % i, [128, 1]), sb("nmr%d" % i, [128, 1])) for i in range(NB)]
        py = [ps("py%d" % i, [128, 1024]) for i in range(3)]
        def stB(ti):
            z = zz[ti % NB]; o = oo[ti % NB]
            resid_B(z, lnts[ti % NB], lg, lb, o)
            if ti >= 1:
                stS(ti - 1)

        def stS(ti):
            o = oo[ti % NB]
            dma('pool', X1.ap[ti * 128:(ti + 1) * 128, :], o[:, :], [o.r], [], o.r, accum=[X1.r])
        for s in range(8):
            m = mt[s % 2]
            dma('sp', m[:, :, :], MT.ap[:, s * 512:(s + 1) * 512].rearrange("(k p) t -> p k t", p=128), [MT.r], [m.r], m.r)
            for j in range(4):
                ti = s * 4 + j
                x = xt[ti % NB]; t = tt[ti % NB]; z = zz[ti % NB]; y = py[ti % 3]; lnt = lnts[ti % NB]
                dma('sp', x[:, :], x_in[ti * 128:(ti + 1) * 128, :], [], [x.r], x.r)
                for h in range(2):
                    for kc in range(8):
                        P.op('pe', lambda e, y=y, m=m, h=h, kc=kc, j=j: e.matmul(y[:, h * 512:(h + 1) * 512], lhsT=m[:, kc, j * 128:(j + 1) * 128], rhs=wo[:, kc, h * 512:(h + 1) * 512], start=(kc == 0), stop=(kc == 7)),
                             reads=[m.r, wo.r], writes=[y.r])
                P.op('act', lambda e, y=y, t=t: e.copy(out=t[:, :], in_=y[:, :]), reads=[y.r], writes=[t.r])
                resid_A(x, t, z, lnt, 0, 0)
                if ti >= 1:
                    stB(ti - 1)
        stB(NT - 1)
        stS(NT - 1)

    phase(ph_s4)
    if 's4' in dbg:
        return nc, gst

    Am = sbp("Am", [128, NT, 2, 32]); RK = sbp("RK", [128, NT, 2]); WG = sbp("WG", [128, NT, 2])
    SLOT = sbp("SLOT", [128, NT * 2], I32); carry = sbp("carry", [128, 32]); idxW = sbp("idxW", [128, NBLK], I32)
    pstart = sbp("pstart", [128, 32]); idxW2 = sbp("idxW2", [128, NBLK * 4], I32); m3p = sbp("m3p", [128, 4])

    def moe(l, Xin, Xout):
        def ph_r(sb, ps):
            wr = sb("wr", [128, 8, 36]); rb = sb("rb", [128, 36])
            dma('sp', wr[:, :, 0:4], moe_wg[l].rearrange("(k p) g -> p k g", p=128), [], [], wr.r, accum=[wr.r], slow=True)
            dma('sp', wr[:, :, 4:36], moe_wf[l].rearrange("(k p) g -> p k g", p=128), [], [], wr.r, accum=[wr.r], slow=True)
            dma('sp', rb[:, 0:4], moe_bg[l].partition_broadcast(128), [], [], rb.r, accum=[rb.r], slow=True)
            dma('sp', rb[:, 4:36], moe_bf[l].partition_broadcast(128), [], [], rb.r, accum=[rb.r], slow=True)
            LG = sb("LG", [128, NT, 36]); xnb = sb("xnb", [128, NT, D], BF16)
            xt = [sb("xt%d" % i, [128, D]) for i in range(NB)]; xn = [sb("xn%d" % i, [128, D]) for i in range(NB)]
            tokT = [sb("tokT%d" % i, [128, 8, 128]) for i in range(NB)]
            lnts = [(sb("stats%d" % i, [128, 12]), sb("mv%d" % i, [128, 2]), sb("rstd%d" % i, [128, 1]), sb("nmr%d" % i, [128, 1])) for i in range(NB)]
            pT = [ps("pT%d" % i) for i in range(4)]; plg = [ps("plg%d" % i) for i in range(2)]
            for ti in range(NT):
                x = xt[ti % NB]; n = xn[ti % NB]; tk = tokT[ti % NB]; lnt = lnts[ti % NB]; pl = plg[ti % 2]
                dma('sp', x[:, :], Xin.ap[ti * 128:(ti + 1) * 128, :], [Xin.r], [x.r], x.r)
                ln_stats(x, lnt[0], lnt[1], lnt[2], lnt[3], [x.r])
                P.op('act', lambda e, x=x, n=n, lnt=lnt: e.activation(out=n[:, :], in_=x[:, :], func=AF.Identity, scale=lnt[2][:, 0:1], bias=lnt[3][:, 0:1]), reads=[x.r, lnt[2].r, lnt[3].r], writes=[n.r])
                P.op('pool', lambda e, n=n, ti=ti: e.tensor_copy(out=xnb[:, ti, :], in_=n[:, :]), reads=[n.r], writes=[], accum=[xnb.r])
                for kc in range(8):
                    p = pT[(ti % 2) * 2 + kc // 4]
                    P.op('pe', lambda e, p=p, kc=kc, n=n: e.transpose(p[:, (kc % 4) * 128:(kc % 4 + 1) * 128], n[:, kc * 128:(kc + 1) * 128], ident[:, :]), reads=[n.r, ident.r], writes=[p.r])
                for kc in range(8):
                    p = pT[(ti % 2) * 2 + kc // 4]
                    P.op('act', lambda e, p=p, kc=kc, tk=tk: e.activation(out=tk[:, kc, :], in_=p[:, (kc % 4) * 128:(kc % 4 + 1) * 128], func=AF.Identity, scale=adac[:, l, 3, kc:kc + 1], bias=adac[:, l, 2, kc:kc + 1]),
                         reads=[p.r, adac.r], writes=[tk.r])
                for kc in range(8):
                    P.op('pe', lambda e, kc=kc, tk=tk, pl=pl: e.matmul(pl[:, 0:36], lhsT=tk[:, kc, :], rhs=wr[:, kc, :], start=(kc == 0), stop=(kc == 7)), reads=[tk.r, wr.r], writes=[pl.r])
                P.op('dve', lambda e, pl=pl, ti=ti: e.tensor_tensor(out=LG[:, ti, :], in0=pl[:, 0:36], in1=rb[:, :], op=ALU.add), reads=[pl.r, rb.r], writes=[], accum=[LG.r])
            gmax = sb("gmax", [128, NT]); dlt = sb("dlt", [128, NT, 4]); gmask = sb("gmask", [128, NT, 4]); sg = sb("sg", [128, NT])
            pen = sb("pen", [128, NT, 4]); fm = sb("fm", [128, NT, 32]); T8 = sb("T8", [128, NT, 8]); dd = sb("dd", [128, NT]); s0 = sb("s0", [128, NT])
            Asb = sb("Asb", [128, NT, 32], BF16); tmp3 = sb("tmp3", [128, NT, 32]); slk = sb("slk", [128, NT])
            ppf = ps("ppf", [128, 1024]); pcnt = plg[0]
            P.op('dve', lambda e: e.reduce_max(out=gmax[:, :], in_=LG[:, :, 0:4], axis=AX.X), reads=[LG.r], writes=[gmax.r])
            P.op('dve', lambda e: e.tensor_tensor(out=dlt[:, :, :], in0=LG[:, :, 0:4], in1=gmax[:, :].unsqueeze(2).to_broadcast([128, NT, 4]), op=ALU.subtract), reads=[LG.r, gmax.r], writes=[dlt.r])
            P.op('dve', lambda e: e.tensor_scalar(out=gmask[:, :, :], in0=dlt[:, :, :], scalar1=0.0, scalar2=None, op0=ALU.is_equal), reads=[dlt.r], writes=[gmask.r])
            P.op('act', lambda e: e.activation(out=dlt[:, :, :], in_=dlt[:, :, :], func=AF.Exp), reads=[dlt.r, gmask.r], writes=[dlt.r])
            P.op('dve', lambda e: e.reduce_sum(out=sg[:, :], in_=dlt[:, :, :], axis=AX.X), reads=[dlt.r], writes=[sg.r])
            P.op('dve', lambda e: e.reciprocal(out=sg[:, :], in_=sg[:, :]), reads=[sg.r], writes=[sg.r])
            P.op('dve', lambda e: e.tensor_scalar(out=pen[:, :, :], in0=gmask[:, :, :], scalar1=-1.0, scalar2=1e30, op0=ALU.add, op1=ALU.mult), reads=[gmask.r], writes=[pen.r])
            P.op('dve', lambda e: e.tensor_tensor(out=fm[:, :, :].rearrange("p t (g j) -> p t g j", g=4), in0=LG[:, :, 4:36].rearrange("p t (g j) -> p t g j", g=4), in1=pen[:, :, :].unsqueeze(3).to_broadcast([128, NT, 4, 8]), op=ALU.add),
                 reads=[LG.r, pen.r], writes=[fm.r])
            for ti in range(NT):
                P.op('dve', lambda e, ti=ti: e.max(out=T8[:, ti, :], in_=fm[:, ti, :]), reads=[fm.r], writes=[], accum=[T8.r])
            for k in range(2):
                P.op('dve', lambda e, k=k: e.tensor_tensor(out=Am[:, :, k, :], in0=fm[:, :, :], in1=T8[:, :, k:k + 1].to_broadcast([128, NT, 32]), op=ALU.is_equal), reads=[fm.r, T8.r], writes=[], accum=[Am.r])
            P.op('dve', lambda e: e.tensor_tensor(out=dd[:, :], in0=T8[:, :, 0], in1=T8[:, :, 1], op=ALU.subtract), reads=[T8.r], writes=[dd.r])
            P.op('act', lambda e: e.activation(out=s0[:, :], in_=dd[:, :], func=AF.Sigmoid), reads=[dd.r], writes=[s0.r])
            P.op('dve', lambda e: e.tensor_tensor(out=WG[:, :, 0], in0=sg[:, :], in1=s0[:, :], op=ALU.mult), reads=[sg.r, s0.r], writes=[WG.r])
            P.op('dve', lambda e: e.tensor_tensor(out=WG[:, :, 1], in0=sg[:, :], in1=WG[:, :, 0], op=ALU.subtract), reads=[sg.r, WG.r], writes=[WG.r])
            P.op('dve', lambda e: e.tensor_tensor(out=Asb[:, :, :], in0=Am[:, :, 0, :], in1=Am[:, :, 1, :], op=ALU.add), reads=[Am.r], writes=[Asb.r])
            for ti in range(NT):
                P.op('pe', lambda e, ti=ti: e.matmul(ppf[:, ti * 32:(ti + 1) * 32], lhsT=ustrict_b[:, :], rhs=Asb[:, ti, :], start=True, stop=(ti == 0)), reads=[ustrict_b.r, Asb.r], writes=[ppf.r])
                for tj in range(ti):
                    P.op('pe', lambda e, ti=ti, tj=tj: e.matmul(ppf[:, ti * 32:(ti + 1) * 32], lhsT=ones_b[:, :], rhs=Asb[:, tj, :], start=False, stop=(tj == ti - 1)), reads=[ones_b.r, Asb.r], writes=[ppf.r])
            for tj in range(NT):
                P.op('pe', lambda e, tj=tj: e.matmul(pcnt[:, 0:32], lhsT=ones_b[:, :], rhs=Asb[:, tj, :], start=(tj == 0), stop=(tj == NT - 1)), reads=[ones_b.r, Asb.r], writes=[pcnt.r])
            P.op('dve', lambda e: e.tensor_copy(out=carry[:, :], in_=pcnt[:, 0:32]), reads=[pcnt.r], writes=[carry.r])
            for k in range(2):
                P.op('dve', lambda e, k=k: e.tensor_tensor(out=tmp3[:, :, :], in0=Am[:, :, k, :], in1=ppf[:, :].rearrange("p (t e) -> p t e", e=32), op=ALU.mult), reads=[Am.r, ppf.r], writes=[tmp3.r])
                P.op('dve', lambda e, k=k: e.reduce_sum(out=RK[:, :, k], in_=tmp3[:, :, :], axis=AX.X), reads=[tmp3.r], writes=[RK.r])
            pad = sb("pad", [128, 32]); mm_ = sb("mm_", [128, 32]); pend = sb("pend", [128, 32]); thr = sb("thr", [128, NBLK])
            cmp = sb("cmp", [128, NBLK, 32]); be = sb("be", [128, NBLK]); idxf = sb("idxf", [128, NBLK])
            P.op('pool', lambda e: e.iota(thr[:, :], pattern=[[BLK, NBLK]], base=0, channel_multiplier=0, allow_small_or_imprecise_dtypes=True), writes=[thr.r])
            P.op('dve', lambda e: e.tensor_tensor(out=cmp[:, 0:32, :], in0=carry[:, :].unsqueeze(2).to_broadcast([128, 32, 32]), in1=thr[:, 0:32].unsqueeze(1).to_broadcast([128, 32, 32]), op=ALU.is_gt), reads=[carry.r, thr.r], writes=[cmp.r])
            P.op('dve', lambda e: e.reduce_sum(out=mm_[:, :], in_=cmp[:, 0:32, :], axis=AX.X), reads=[cmp.r], writes=[mm_.r])
            P.op('dve', lambda e: e.tensor_scalar(out=pad[:, :], in0=mm_[:, :], scalar1=float(BLK), scalar2=None, op0=ALU.mult), reads=[mm_.r], writes=[pad.r])
            P.op('dve', lambda e: e.tensor_tensor_scan(out=pend[:, :], data0=ones_f[:, 0:32], data1=pad[:, :], initial=0.0, op0=ALU.mult, op1=ALU.add), reads=[ones_f.r, pad.r], writes=[pend.r])
            P.op('dve', lambda e: e.tensor_tensor(out=pstart[:, :], in0=pend[:, :], in1=pad[:, :], op=ALU.subtract), reads=[pend.r, pad.r], writes=[pstart.r])
            P.op('dve', lambda e: e.tensor_tensor(out=cmp[:, :, :], in0=pend[:, :].unsqueeze(1).to_broadcast([128, NBLK, 32]), in1=thr[:, :].unsqueeze(2).to_broadcast([128, NBLK, 32]), op=ALU.is_le), reads=[pend.r, thr.r], writes=[cmp.r])
            P.op('dve', lambda e: e.reduce_sum(out=be[:, :], in_=cmp[:, :, :], axis=AX.X), reads=[cmp.r], writes=[be.r])
            P.op('dve', lambda e: e.tensor_scalar(out=be[:, :], in0=be[:, :], scalar1=31.0, scalar2=128.0, op0=ALU.min, op1=ALU.mult), reads=[be.r], writes=[be.r])
            P.op('dve', lambda e: e.tensor_scalar(out=idxf[:, :], in0=be[:, :], scalar1=iota_p[:, 0:1], scalar2=float(l * 4096), op0=ALU.add, op1=ALU.add), reads=[be.r, iota_p.r], writes=[idxf.r])
            P.op('dve', lambda e: e.tensor_copy(out=idxW[:, :], in_=idxf[:, :]), reads=[idxf.r], writes=[idxW.r])
            idx4 = sb("idx4", [128, NBLK]); idxf2 = sb("idxf2", [128, NBLK, 4]); np3 = sb("np3", [128, 1])
            P.op('pool', lambda e: e.iota(m3p[:, :], pattern=[[128, 4]], base=0, channel_multiplier=0, allow_small_or_imprecise_dtypes=True), writes=[m3p.r])
            P.op('dve', lambda e: e.tensor_scalar(out=np3[:, :], in0=iota_p[:, :], scalar1=-3.0, scalar2=None, op0=ALU.mult), reads=[iota_p.r], writes=[np3.r])
            P.op('dve', lambda e: e.tensor_scalar(out=m3p[:, :], in0=m3p[:, :], scalar1=np3[:, 0:1], scalar2=None, op0=ALU.add), reads=[m3p.r, np3.r], writes=[m3p.r])
            P.op('dve', lambda e: e.tensor_scalar(out=idx4[:, :], in0=idxf[:, :], scalar1=4.0, scalar2=None, op0=ALU.mult), reads=[idxf.r], writes=[idx4.r])
            P.op('dve', lambda e: e.tensor_tensor(out=idxf2[:, :, :], in0=idx4[:, :].unsqueeze(2).to_broadcast([128, NBLK, 4]), in1=m3p[:, :].unsqueeze(1).to_broadcast([128, NBLK, 4]), op=ALU.add), reads=[idx4.r, m3p.r], writes=[idxf2.r])
            P.op('dve', lambda e: e.tensor_copy(out=idxW2[:, :], in_=idxf2[:, :, :].rearrange("p a b -> p (a b)")), reads=[idxf2.r], writes=[idxW2.r])
            for k in range(2):
                P.op('dve', lambda e, k=k: e.tensor_tensor(out=tmp3[:, :, :], in0=Am[:, :, k, :], in1=pstart[:, :].unsqueeze(1).to_broadcast([128, NT, 32]), op=ALU.mult), reads=[Am.r, pstart.r], writes=[tmp3.r])
                P.op('dve', lambda e, k=k: e.reduce_sum(out=slk[:, :], in_=tmp3[:, :, :], axis=AX.X), reads=[tmp3.r], writes=[slk.r])
                P.op('dve', lambda e, k=k: e.tensor_tensor(out=slk[:, :], in0=slk[:, :], in1=RK[:, :, k], op=ALU.add), reads=[slk.r, RK.r], writes=[slk.r])
                P.op('dve', lambda e, k=k: e.tensor_copy(out=SLOT[:, k::2], in_=slk[:, :]), reads=[slk.r], writes=[], accum=[SLOT.r])
            for ti in range(NT):
                for k in range(2):
                    P.op('pool', lambda e, ti=ti, k=k: e.indirect_dma_start(out=XG.ap, out_offset=bass.IndirectOffsetOnAxis(ap=SLOT[:, ti * 2 + k:ti * 2 + k + 1], axis=0), in_=xnb[:, ti, :], in_offset=None),
                         reads=[xnb.r, SLOT.r], writes=[], dma=xnb.r, accum=[XG.r])

        if 'DBGI' in dbg and l == 0:
            def ph_dbg(sb, ps):
                dma('sp', DBGI.ap[:, 0:NBLK], idxW[:, :], [idxW.r], [], idxW.r, accum=[DBGI.r])
                dma('sp', DBGI.ap[:, NBLK:NBLK * 5], idxW2[:, :], [idxW2.r], [], idxW2.r, accum=[DBGI.r])
        phase(ph_r)
        if 'DBGI' in dbg and l == 0:
            phase(ph_dbg)

        def ph_e(sb, ps):
            w1b = [sb("w1b%d" % i, [128, 8, 512], BF16) for i in range(2)]
            w3b = [sb("w3b%d" % i, [128, 8, 512], BF16) for i in range(2)]
            w2b = [sb("w2b%d" % i, [128, 4, D], BF16) for i in range(2)]
            xb = [sb("xb%d" % i, [128, 2, D], BF16) for i in range(2)]
            XT = [sb("XT%d" % i, [128, 8, BLK], BF16) for i in range(2)]
            hid = [sb("hid%d" % i, [128, 4, BLK], BF16) for i in range(2)]
            sa = [sb("sa%d" % i, [128, BLK]) for i in range(2)]
            ysb = [sb("ysb%d" % i, [128, D]) for i in range(2)]
            pT = [ps("pT%d" % i, [128, 512], BF16) for i in range(2)]; pa = [ps("pa%d" % i) for i in range(2)]; pb = [ps("pb%d" % i) for i in range(2)]
            py = ps("py", [128, 1024])
            w1v = W1B; w3v = W3B; w2v = W2B
            yc = [0]
            w2rows = W2B.ap.rearrange("a (r n) -> (a r) n", r=4)

            def prep(i):
                b = i % 2
                for wt, wv in ((w1b[b], w1v), (w3b[b], w3v)):
                    P.op('pool', lambda e, wt=wt, wv=wv, i=i: e.indirect_dma_start(out=wt[:, :, :].rearrange("p a b -> p (a b)"), out_offset=None, in_=wv.ap, in_offset=bass.IndirectOffsetOnAxis(ap=idxW[:, i:i + 1], axis=0)),
                         reads=[idxW.r, wv.r], writes=[wt.r], dma=wt.r)
                for fc in range(4):
                    P.op('pool', lambda e, b=b, i=i, fc=fc: e.indirect_dma_start(out=w2b[b][:, fc, :], out_offset=None, in_=w2rows, in_offset=bass.IndirectOffsetOnAxis(ap=idxW2[:, i * 4 + fc:i * 4 + fc + 1], axis=0)),
                         reads=[idxW2.r, W2B.r], writes=([w2b[b].r] if fc == 0 else []), dma=w2b[b].r, accum=([] if fc == 0 else [w2b[b].r]))
                x = xb[b]
                dma('sp', x[:, :, :], XG.ap[i * BLK:(i + 1) * BLK, :].rearrange("(j p) d -> p j d", p=128), [XG.r], [x.r], x.r)
                xT = XT[b]
                for kc in range(8):
                    p = pT[kc % 2]
                    for j in range(2):
                        P.op('pe', lambda e, p=p, j=j, kc=kc, x=x: e.transpose(p[:, j * 128:(j + 1) * 128], x[:, j, kc::8], ident_b[:, :]), reads=[x.r, ident_b.r], writes=[p.r])
                    P.op('act', lambda e, p=p, kc=kc, xT=xT: e.activation(out=xT[:, kc, :], in_=p[:, 0:BLK], func=AF.Identity, scale=adap[:, l, 1, kc:kc + 1], bias=adap[:, l, 0, kc:kc + 1]),
                         reads=[p.r, adap.r], writes=[xT.r])

            def compute(i):
                b = i % 2
                xT = XT[b]; h = hid[b]
                for fc in range(4):
                    A_ = pa[fc % 2]; B_ = pb[fc % 2]; s_ = sa[fc % 2]
                    for (pp, wt) in ((A_, w1b[b]), (B_, w3b[b])):
                        for kc in range(8):
                            P.op('pe', lambda e, pp=pp, wt=wt, kc=kc, fc=fc, xT=xT: e.matmul(pp[:, 0:BLK], lhsT=wt[:, kc, fc * 128:(fc + 1) * 128], rhs=xT[:, kc, :], start=(kc == 0), stop=(kc == 7)), reads=[wt.r, xT.r], writes=[pp.r])
                    P.op('act', lambda e, A_=A_, s_=s_: e.activation(out=s_[:, :], in_=A_[:, 0:BLK], func=AF.Silu), reads=[A_.r], writes=[s_.r])
                    P.op('dve', lambda e, B_=B_, s_=s_, h=h, fc=fc: e.tensor_tensor(out=h[:, fc, :], in0=s_[:, :], in1=B_[:, 0:BLK], op=ALU.mult), reads=[s_.r, B_.r], writes=[h.r])
                for j in range(2):
                    for hh in range(2):
                        for fc in range(4):
                            P.op('pe', lambda e, j=j, hh=hh, fc=fc, h=h, b=b: e.matmul(py[:, hh * 512:(hh + 1) * 512], lhsT=h[:, fc, j * 128:(j + 1) * 128], rhs=w2b[b][:, fc, hh * 512:(hh + 1) * 512], start=(fc == 0), stop=(fc == 3)),
                                 reads=[h.r, w2b[b].r], writes=[py.r])
                    y = ysb[yc[0] % 2]; yc[0] += 1
                    if j == 0:
                        P.op('act', lambda e, y=y: e.copy(out=y[:, :], in_=py[:, :]), reads=[py.r], writes=[y.r])
                    else:
                        P.op('dve', lambda e, y=y: e.tensor_copy(out=y[:, :], in_=py[:, :]), reads=[py.r], writes=[y.r])
                    dma('act', YG.ap[i * BLK + j * 128: i * BLK + (j + 1) * 128, :], y[:, :], [y.r], [], y.r, accum=[YG.r])

            prep(0)
            for i in range(NBLK):
                if i + 1 < NBLK:
                    prep(i + 1)
                compute(i)

        phase(ph_e)

        def ph_c(sb, ps):
            lg, lb = load_bc(sb, l, 1)
            y0 = [sb("y0%d" % i, [128, D]) for i in range(NB)]; y1 = [sb("y1%d" % i, [128, D]) for i in range(NB)]
            xt = [sb("xt%d" % i, [128, D]) for i in range(NB)]; tt = [sb("tt%d" % i, [128, D]) for i in range(NB)]
            zz = [sb("zz%d" % i, [128, D]) for i in range(NB)]; oo = [sb("oo%d" % i, [128, D]) for i in range(NB)]
            lnts = [(sb("stats%d" % i, [128, 12]), sb("mv%d" % i, [128, 2]), sb("rstd%d" % i, [128, 1]), sb("nmr%d" % i, [128, 1])) for i in range(NB)]
            for it in range(NT + 3):
                if it < NT:
                    ti = it; b = ti % NB
                    for k, yy in ((0, y0[b]), (1, y1[b])):
                        P.op('pool', lambda e, yy=yy, ti=ti, k=k: e.indirect_dma_start(out=yy[:, :], out_offset=None, in_=YG.ap, in_offset=bass.IndirectOffsetOnAxis(ap=SLOT[:, ti * 2 + k:ti * 2 + k + 1], axis=0)),
                             reads=[YG.r, SLOT.r], writes=[yy.r], dma=yy.r)
                    dma('sp', xt[b][:, :], Xin.ap[ti * 128:(ti + 1) * 128, :], [Xin.r], [xt[b].r], xt[b].r)
                if 1 <= it <= NT:
                    ti = it - 1; b = ti % NB
                    t = tt[b]
                    P.op('act', lambda e, t=t, b=b, ti=ti: e.activation(out=t[:, :], in_=y0[b][:, :], func=AF.Identity, scale=WG[:, ti, 0:1]), reads=[y0[b].r, WG.r], writes=[t.r])
                    P.op('dve', lambda e, t=t, b=b, ti=ti: e.scalar_tensor_tensor(out=t[:, :], in0=y1[b][:, :], scalar=WG[:, ti, 1:2], in1=t[:, :], op0=ALU.mult, op1=ALU.add), reads=[y1[b].r, WG.r, t.r], writes=[t.r])
                    resid_A(xt[b], t, zz[b], lnts[b], l, 1)
                if 2 <= it <= NT + 1:
                    ti = it - 2; b = ti % NB
                    resid_B(zz[b], lnts[b], lg, lb, oo[b])
                if it >= 3:
                    ti = it - 3; b = ti % NB
                    dma('pool', Xout.ap[ti * 128:(ti + 1) * 128, :], oo[b][:, :], [oo[b].r], [], oo[b].r, accum=[Xout.r])

        phase(ph_c)

    moe(0, X1, X2)
    if 'm0' in dbg:
        return nc, gst

    def ph_f1(sb, ps):
        ccs = sb("ccs", [128, 2, 256]); scs = sb("scs", [128, 2, 256]); wod = sb("wod", [128, 8, D])
        M1 = sb("M1", [128, 8, D], BF16); M2 = sb("M2", [128, 8, D], BF16)
        dma('sp', ccs[:, :, :], k_cc.rearrange("(a p) n -> p a n", p=128), [], [ccs.r], ccs.r)
        dma('sp', scs[:, :, :], k_sc.rearrange("(a p) n -> p a n", p=128), [], [scs.r], scs.r)
        dma('sp', wod[:, :, :], od_w_out.rearrange("(k p) n -> p k n", p=128), [], [wod.r], wod.r)
        pm = [ps("pm%d" % i) for i in range(2)]
        pT = [ps("pT%d" % i) for i in range(2)]
        pU = ps("pU", [128, 1024]); pV = ps("pV", [128, 1024])
        mc = [0]
        for which, (cs, Mx) in enumerate(((ccs, M1), (scs, M2))):
            for g in range(4):
                for cch in range(2):
                    for nh in range(2):
                        p = pm[mc[0] % 2]; mc[0] += 1
                        for c2 in range(2):
                            P.op('pe', lambda e, p=p, cs=cs, c2=c2, cch=cch, g=g, nh=nh: e.matmul(p[:, :], lhsT=cs[:, c2, cch * 128:(cch + 1) * 128], rhs=wod[:, g * 2 + c2, nh * 512:(nh + 1) * 512], start=(c2 == 0), stop=(c2 == 1)),
                                 reads=[cs.r, wod.r], writes=[p.r])
                        P.op('act', lambda e, p=p, Mx=Mx, g=g, cch=cch, nh=nh, which=which: e.activation(out=Mx[:, g * 2 + cch, nh * 512:(nh + 1) * 512], in_=p[:, :], func=AF.Identity, scale=(1.0 if which == 0 else -1.0)),
                             reads=[p.r], writes=[Mx.r])
        xt = [sb("xt%d" % i, [128, D]) for i in range(NB)]; xn = [sb("xn%d" % i, [128, D]) for i in range(NB)]
        hT = [sb("hT%d" % i, [128, 8, 128], BF16) for i in range(NB)]
        ou = [sb("ou%d" % i, [128, D], BF16) for i in range(NB)]; ov = [sb("ov%d" % i, [128, D], BF16) for i in range(NB)]
        lnts = [(sb("stats%d" % i, [128, 12]), sb("mv%d" % i, [128, 2]), sb("rstd%d" % i, [128, 1]), sb("nmr%d" % i, [128, 1])) for i in range(NB)]
        for ti in range(NT):
            conv_issue(1, 1)
            x = xt[ti % NB]; n = xn[ti % NB]; h = hT[ti % NB]; lnt = lnts[ti % NB]
            dma('sp', x[:, :], X2.ap[ti * 128:(ti + 1) * 128, :], [X2.r], [x.r], x.r)
            ln_stats(x, lnt[0], lnt[1], lnt[2], lnt[3], [x.r])
            P.op('act', lambda e, x=x, n=n, lnt=lnt: e.activation(out=n[:, :], in_=x[:, :], func=AF.Identity, scale=lnt[2][:, 0:1], bias=lnt[3][:, 0:1]), reads=[x.r, lnt[2].r, lnt[3].r], writes=[n.r])
            for kc in range(8):
                p = pT[kc // 4]
                P.op('pe', lambda e, p=p, kc=kc, n=n: e.transpose(p[:, (kc % 4) * 128:(kc % 4 + 1) * 128], n[:, kc * 128:(kc + 1) * 128], ident[:, :]), reads=[n.r, ident.r], writes=[p.r])
            for kc in range(8):
                p = pT[kc // 4]
                P.op('act', lambda e, p=p, kc=kc, h=h: e.activation(out=h[:, kc, :], in_=p[:, (kc % 4) * 128:(kc % 4 + 1) * 128], func=AF.Identity, scale=adac[:, 1, 1, kc:kc + 1], bias=adac[:, 1, 0, kc:kc + 1]),
                     reads=[p.r, adac.r], writes=[h.r])
            for (pp, Mx, oo_, DD) in ((pU, M1, ou[ti % NB], UU), (pV, M2, ov[ti % NB], VW)):
                for nh in range(2):
                    for kc in range(8):
                        P.op('pe', lambda e, pp=pp, Mx=Mx, nh=nh, kc=kc, h=h: e.matmul(pp[:, nh * 512:(nh + 1) * 512], lhsT=h[:, kc, :], rhs=Mx[:, kc, nh * 512:(nh + 1) * 512], start=(kc == 0), stop=(kc == 7)),
                             reads=[h.r, Mx.r], writes=[pp.r])
                P.op('act', lambda e, pp=pp, oo_=oo_: e.copy(out=oo_[:, :], in_=pp[:, :]), reads=[pp.r], writes=[oo_.r])
                dma('act', DD.ap[ti * 128:(ti + 1) * 128, :], oo_[:, :], [oo_.r], [], oo_.r, accum=[DD.r])

    phase(ph_f1)

    def ph_f2(sb, ps):
        Uh = sb("Uh", [128, NT, 512], BF16); Vh = sb("Vh", [128, NT, 512], BF16)
        cn = [sb("cn%d" % i, [128, NT * 128], BF16) for i in range(2)]; sn = [sb("sn%d" % i, [128, NT * 128], BF16) for i in range(2)]
        yo = [sb("yo%d" % i, [128, 512]) for i in range(2)]
        py = [ps("py%d" % i) for i in range(2)]
        it = 0
        for nh in range(2):
            dma('sp', Uh[:, :, :], UU.ap[:, nh * 512:(nh + 1) * 512].rearrange("(t p) n -> p t n", p=128), [UU.r], [Uh.r], Uh.r)
            dma('sp', Vh[:, :, :], VW.ap[:, nh * 512:(nh + 1) * 512].rearrange("(t p) n -> p t n", p=128), [VW.r], [Vh.r], Vh.r)
            for kt in range(NT):
                c = cn[it % 2]; s_ = sn[it % 2]; y = yo[it % 2]; p = py[it % 2]; it += 1
                dma('sp', c[:, :], k_cn[kt], [], [c.r], c.r)
                dma('sp', s_[:, :], k_sn[kt], [], [s_.r], s_.r)
                for tt in range(NT):
                    P.op('pe', lambda e, p=p, c=c, tt=tt: e.matmul(p[:, :], lhsT=c[:, tt * 128:(tt + 1) * 128], rhs=Uh[:, tt, :], start=(tt == 0), stop=False), reads=[c.r, Uh.r], writes=[p.r])
                    P.op('pe', lambda e, p=p, s_=s_, tt=tt: e.matmul(p[:, :], lhsT=s_[:, tt * 128:(tt + 1) * 128], rhs=Vh[:, tt, :], start=False, stop=(tt == NT - 1)), reads=[s_.r, Vh.r], writes=[p.r])
                P.op('act', lambda e, p=p, y=y: e.copy(out=y[:, :], in_=p[:, :]), reads=[p.r], writes=[y.r])
                dma('act', Y1.ap[kt * 128:(kt + 1) * 128, nh * 512:(nh + 1) * 512], y[:, :], [y.r], [], y.r, accum=[Y1.r])

    phase(ph_f2)

    def ph_f3(sb, ps):
        lg, lb = load_bc(sb, 1, 0)
        bb = sb("bb", [128, D])
        dma('sp', bb[:, :], od_b_out.partition_broadcast(128), [], [bb.r], bb.r)
        xt = [sb("xt%d" % i, [128, D]) for i in range(NB)]; tt = [sb("tt%d" % i, [128, D]) for i in range(NB)]
        zz = [sb("zz%d" % i, [128, D]) for i in range(NB)]; oo = [sb("oo%d" % i, [128, D]) for i in range(NB)]
        lnts = [(sb("stats%d" % i, [128, 12]), sb("mv%d" % i, [128, 2]), sb("rstd%d" % i, [128, 1]), sb("nmr%d" % i, [128, 1])) for i in range(NB)]
        for it in range(NT + 2):
            if it < NT:
                ti = it
                x = xt[ti % NB]; t = tt[ti % NB]; z = zz[ti % NB]; lnt = lnts[ti % NB]
                dma('sp', x[:, :], X2.ap[ti * 128:(ti + 1) * 128, :], [X2.r], [x.r], x.r)
                dma('sp', t[:, :], Y1.ap[ti * 128:(ti + 1) * 128, :], [Y1.r], [t.r], t.r)
                P.op('pool', lambda e, t=t: e.tensor_tensor(out=t[:, :], in0=t[:, :], in1=bb[:, :], op=ALU.add), reads=[t.r, bb.r], writes=[t.r])
                resid_A(x, t, z, lnt, 1, 0)
            if 1 <= it <= NT:
                ti = it - 1
                resid_B(zz[ti % NB], lnts[ti % NB], lg, lb, oo[ti % NB])
            if it >= 2:
                ti = it - 2
                dma('pool', X3.ap[ti * 128:(ti + 1) * 128, :], oo[ti % NB][:, :], [oo[ti % NB].r], [], oo[ti % NB].r, accum=[X3.r])

    phase(ph_f3)
    if 'f3' in dbg:
        return nc, gst
    moe(1, X3, out_d)
    return nc, gst


def _consts():
    t = np.arange(SEQ)
    row = (t // 64).astype(np.float32); col = (t % 64).astype(np.float32)
    nf = 16
    freqs = (10000.0 ** (-np.arange(nf, dtype=np.float32) / nf)).astype(np.float32)
    ropec = np.zeros((128, SEQ), np.float32); ropes = np.zeros((128, SEQ), np.float32)
    for i in range(2):
        for d in range(64):
            pos = row if d < 32 else col
            ang = (pos * freqs[d % 16]).astype(np.float32)
            ropec[i * 64 + d] = np.cos(ang)
            sgn = -1.0 if (d % 32) < 16 else 1.0
            ropes[i * 64 + d] = sgn * np.sin(ang)
    k = np.arange(256)
    ang = 2 * np.pi * np.outer(k, k) / 256.0
    cc = (np.cos(ang) / 16.0).astype(np.float32); sc = (np.sin(ang) / 16.0).astype(np.float32)
    n = np.arange(SEQ)
    kt = (np.outer(n, n) % SEQ).astype(np.float64) * (2 * np.pi / SEQ)
    cn = (np.cos(kt) / 64.0); sn = (np.sin(kt) / 64.0)

    def lay(m):
        m4 = m.reshape(32, 128, 32, 128)
        return np.ascontiguousarray(m4.transpose(2, 1, 0, 3)).reshape(32, 128, 32 * 128).astype(ml_dtypes.bfloat16)
    return dict(k_ropec=ropec, k_ropes=ropes, k_cc=cc, k_sc=sc, k_cn=lay(cn), k_sn=lay(sn))


_CACHE = {}


def kernel(**inputs):
    dbg = tuple(os.environ.get("KDBG", "").split(",")) if os.environ.get("KDBG") else ()
    nc, gst = build(dbg)
    gst.close()
    if 'consts' not in _CACHE:
        _CACHE['consts'] = _consts()
    cst = _CACHE['consts']
    f = lambda a: np.ascontiguousarray(np.asarray(a, dtype=np.float32))
    shared = {k: f(inputs[k]) for k in ['c_ctx', 'ada_w', 'ada_b', 'ln_g', 'ln_b', 'od_b_out', 'moe_wg', 'moe_bg', 'moe_wf', 'moe_bf', 'moe_w1', 'moe_w3', 'moe_w2']}
    for k in ['ev_w_in', 'ev_conv_w', 'ev_conv_b', 'ev_gate_a_w', 'ev_gate_a_b', 'ev_gate_x_w', 'ev_gate_x_b', 'ev_lru_lambda', 'ev_da_lambda', 'ev_da_subln', 'ev_w_out', 'od_w_out']:
        shared[k] = f(inputs[k])[0]
    shared.update(cst)
    x = f(inputs['x']); c = f(inputs['c']); ctx = f(inputs['ctx'])
    in_maps = []
    for b in range(8):
        m = dict(shared)
        m['x'] = x[b]; m['c'] = c[b]; m['ctx'] = ctx[b]
        in_maps.append(m)
    ncores = int(os.environ.get('KCORES', '8'))
    res = run_bass_kernel_spmd(nc, in_maps[:ncores], core_ids=list(range(ncores)))
    if dbg:
        return res
    return np.stack([np.asarray(r['out'], dtype=np.float32) for r in res.results], axis=0)
```
